# Optimizing a Trainium2 kernel written in Bass

```python
import math
import jax, jax.numpy as jnp
from jax import lax
import numpy as np

D_MODEL = 1024
BATCH = 8
SEQ = 8192
DEPTH = 2

PLE_DIM = 256

GRID_W = 64
NA_HEADS = 8
NA_HEAD_DIM = 64
NA_ROWS = 8
NA_COLS = 16
NA_BAND = 2 * NA_COLS
NA_NCB = GRID_W // NA_COLS
NA_WIDTH = NA_HEADS * NA_HEAD_DIM

MLA_HEADS = 8
MLA_NOPE = 64
MLA_ROPE = 32
MLA_V = 64
MLA_Q_LORA = 768
MLA_KV_LORA = 256
MLA_WIDTH = MLA_HEADS * MLA_V
ROPE_THETA = 10000.0
Q_BLOCK = 128

N_IN = 3 * NA_WIDTH + MLA_Q_LORA + MLA_KV_LORA + MLA_ROPE + 2 * D_MODEL

D_FF = 2816
N_EXPERTS = 8
TOP_K = 2
D_FF_EXPERT = 3584
N_DENSE = (DEPTH + 1) // 2
N_MOE = DEPTH // 2

DEEPNORM_ALPHA = (2 * DEPTH) ** 0.25
DEEPNORM_BETA = (8 * DEPTH) ** -0.25
LN_EPS = 1e-5
RMS_EPS = 1e-6
MASK_VALUE = -1e30

kernel_name = 'hybrid_natten_mla_moe_deepnorm_encoder'


def _layer_norm(x, g, b):
    xf = x.astype(jnp.float32)
    mu = jnp.mean(xf, axis=-1, keepdims=True)
    var = jnp.mean(jnp.square(xf - mu), axis=-1, keepdims=True)
    y = (xf - mu) * lax.rsqrt(var + LN_EPS)
    return (y * g.astype(jnp.float32) + b.astype(jnp.float32)).astype(x.dtype)


def _rms_norm(x, g):
    xf = x.astype(jnp.float32)
    y = xf * lax.rsqrt(jnp.mean(jnp.square(xf), axis=-1, keepdims=True) + RMS_EPS)
    return (y * g.astype(jnp.float32)).astype(x.dtype)


def _rope(x, cos, sin):
    half = x.shape[-1] // 2
    x1, x2 = x[..., :half], x[..., half:]
    return jnp.concatenate([x1 * cos - x2 * sin, x2 * cos + x1 * sin], axis=-1)


def _swiglu(x, w1, w3, w2):
    return (jax.nn.silu(x @ w1) * (x @ w3)) @ w2


def _na_column_structure():
    band_start = np.clip(np.arange(NA_NCB) * NA_COLS - NA_COLS // 2, 0, GRID_W - NA_BAND)
    band_idx = band_start[:, None] + np.arange(NA_BAND)[None, :]
    q_col = np.arange(GRID_W).reshape(NA_NCB, NA_COLS)
    win_start = np.clip(q_col - NA_COLS // 2, 0, GRID_W - NA_COLS)
    k_col = band_idx[:, None, :]
    valid = (k_col >= win_start[..., None]) & (k_col < win_start[..., None] + NA_COLS)
    dc_idx = np.clip(k_col - q_col[..., None] + NA_COLS - 1, 0, 2 * NA_COLS - 2)
    return band_idx.astype(np.int32), valid, dc_idx.astype(np.int32)


def neighborhood_attention(q, k, v, rpb):
    b, s, h, dh = q.shape
    rows = s // GRID_W
    kr = min(NA_ROWS, rows)
    band_idx, valid, dc_idx = _na_column_structure()
    valid = jnp.asarray(valid)[:, :, None, :]
    col_bias = rpb[:, :, dc_idx]
    kg = k.reshape(b, rows, GRID_W, h, dh)
    vg = v.reshape(b, rows, GRID_W, h, dh)
    qg = q.reshape(b, rows, NA_NCB, NA_COLS, h, dh).transpose(1, 0, 2, 3, 4, 5)
    scale = dh ** -0.5

    def row_block(args):
        r, q_row = args
        rs = jnp.clip(r - kr // 2, 0, rows - kr)
        k_band = jnp.take(lax.dynamic_slice_in_dim(kg, rs, kr, axis=1), band_idx, axis=2)
        v_band = jnp.take(lax.dynamic_slice_in_dim(vg, rs, kr, axis=1), band_idx, axis=2)
        dr_idx = rs + jnp.arange(kr, dtype=jnp.int32) - r + (NA_ROWS - 1)
        bias = jnp.take(col_bias, dr_idx, axis=1).transpose(0, 2, 3, 1, 4)
        sc = jnp.einsum('bnqhd,bknjhd->bhnqkj', q_row, k_band).astype(jnp.float32) * scale
        sc = jnp.where(valid, sc + bias.astype(jnp.float32), MASK_VALUE)
        pr = jax.nn.softmax(sc.reshape(sc.shape[:4] + (kr * NA_BAND,)), axis=-1).reshape(sc.shape)
        o = jnp.einsum('bhnqkj,bknjhd->bnqhd', pr.astype(v.dtype), v_band)
        return o.reshape(b, GRID_W, h * dh)

    out = lax.map(row_block, (jnp.arange(rows, dtype=jnp.int32), qg))
    return out.transpose(1, 0, 2, 3).reshape(b, s, h * dh)


def latent_attention(q, k, v):
    b, s, h, dq = q.shape
    nb = s // Q_BLOCK
    scale = dq ** -0.5
    qb = q.reshape(b, nb, Q_BLOCK, h, dq).transpose(1, 0, 2, 3, 4)

    def block(qi):
        sc = jnp.einsum('bqhd,bkhd->bhqk', qi, k).astype(jnp.float32) * scale
        pr = jax.nn.softmax(sc, axis=-1)
        return jnp.einsum('bhqk,bkhd->bqhd', pr.astype(v.dtype), v)

    o = lax.map(block, qb)
    return o.transpose(1, 0, 2, 3, 4).reshape(b, s, h * v.shape[-1])


def moe_swiglu(x, w_router, b_router, w1, w3, w2):
    logits = (x @ w_router).astype(jnp.float32) + b_router.astype(jnp.float32)
    top_vals, top_idx = lax.top_k(logits, TOP_K)
    top_w = jax.nn.softmax(top_vals, axis=-1)
    gates = jnp.sum(jax.nn.one_hot(top_idx, N_EXPERTS, dtype=jnp.float32) * top_w[..., None], axis=-2)
    out = jnp.zeros_like(x)
    for e in range(N_EXPERTS):
        out = out + gates[..., e:e + 1].astype(x.dtype) * _swiglu(x, w1[e], w3[e], w2[e])
    return out


def setup_inputs(seed: int = 0) -> dict:
    key = jax.random.key(seed)
    ks = jax.random.split(key, 26)
    f32 = jnp.float32

    def nrm(k, shape, fan_in, scale=1.0):
        return jax.random.normal(k, shape, f32) * (scale * fan_in ** -0.5)

    def gain(k, shape):
        return 1.0 + 0.05 * jax.random.normal(k, shape, f32)

    def bias(k, shape, s=0.02):
        return s * jax.random.normal(k, shape, f32)

    beta = DEEPNORM_BETA
    return {
        'x': jax.random.normal(ks[0], (BATCH, SEQ, D_MODEL), f32),
        'p': jax.random.normal(ks[1], (DEPTH, BATCH, SEQ, PLE_DIM), f32),
        'w_in': nrm(ks[2], (DEPTH, D_MODEL, N_IN), D_MODEL),
        'b_gate': bias(ks[3], (DEPTH, 2 * D_MODEL)),
        'q_norm_g': gain(ks[4], (DEPTH, MLA_Q_LORA)),
        'w_uq': nrm(ks[5], (DEPTH, MLA_Q_LORA, MLA_HEADS * (MLA_NOPE + MLA_ROPE)), MLA_Q_LORA),
        'kv_norm_g': gain(ks[6], (DEPTH, MLA_KV_LORA)),
        'w_ukv': nrm(ks[7], (DEPTH, MLA_KV_LORA, MLA_HEADS * (MLA_NOPE + MLA_V)), MLA_KV_LORA),
        'na_rpb': bias(ks[8], (DEPTH, NA_HEADS, 2 * NA_ROWS - 1, 2 * NA_COLS - 1), 0.1),
        'w_na_o': nrm(ks[9], (DEPTH, NA_WIDTH, D_MODEL), NA_WIDTH, beta),
        'w_mla_o': nrm(ks[10], (DEPTH, MLA_WIDTH, D_MODEL), MLA_WIDTH, beta),
        'w_out': nrm(ks[11], (DEPTH, D_MODEL, D_MODEL), D_MODEL, beta),
        'ln1_g': gain(ks[12], (DEPTH, D_MODEL)),
        'ln1_b': bias(ks[13], (DEPTH, D_MODEL)),
        'ffn_w1': nrm(ks[14], (N_DENSE, D_MODEL, D_FF), D_MODEL),
        'ffn_w3': nrm(ks[15], (N_DENSE, D_MODEL, D_FF), D_MODEL),
        'ffn_w2': nrm(ks[16], (N_DENSE, D_FF, D_MODEL), D_FF, beta),
        'moe_w_router': nrm(ks[17], (N_MOE, D_MODEL, N_EXPERTS), D_MODEL),
        'moe_b_router': bias(ks[18], (N_MOE, N_EXPERTS), 0.01),
        'moe_w1': nrm(ks[19], (N_MOE, N_EXPERTS, D_MODEL, D_FF_EXPERT), D_MODEL),
        'moe_w3': nrm(ks[20], (N_MOE, N_EXPERTS, D_MODEL, D_FF_EXPERT), D_MODEL),
        'moe_w2': nrm(ks[21], (N_MOE, N_EXPERTS, D_FF_EXPERT, D_MODEL), D_FF_EXPERT, beta),
        'w_ple_gate': nrm(ks[22], (DEPTH, D_MODEL, D_MODEL), D_MODEL),
        'w_ple': nrm(ks[23], (DEPTH, PLE_DIM, D_MODEL), PLE_DIM, beta),
        'ln2_g': gain(ks[24], (DEPTH, D_MODEL)),
        'ln2_b': bias(ks[25], (DEPTH, D_MODEL)),
    }


def reference(x, p, w_in, b_gate, q_norm_g, w_uq, kv_norm_g, w_ukv, na_rpb, w_na_o, w_mla_o,
              w_out, ln1_g, ln1_b, ffn_w1, ffn_w3, ffn_w2, moe_w_router, moe_b_router,
              moe_w1, moe_w3, moe_w2, w_ple_gate, w_ple, ln2_g, ln2_b):
    b, s, _ = x.shape
    pos = jnp.arange(s, dtype=jnp.float32)
    inv_freq = ROPE_THETA ** (-jnp.arange(0, MLA_ROPE // 2, dtype=jnp.float32) * (2.0 / MLA_ROPE))
    ang = pos[:, None] * inv_freq[None, :]
    cos = jnp.cos(ang)[:, None, :].astype(x.dtype)
    sin = jnp.sin(ang)[:, None, :].astype(x.dtype)
    splits = list(np.cumsum([3 * NA_WIDTH, MLA_Q_LORA, MLA_KV_LORA, MLA_ROPE, D_MODEL]))

    for i in range(DEPTH):
        proj = x @ w_in[i]
        na_qkv, q_lat, kv_lat, k_rope, g_na, g_mla = jnp.split(proj, [int(o) for o in splits], axis=-1)

        q_na, k_na, v_na = jnp.split(na_qkv.reshape(b, s, 3, NA_HEADS, NA_HEAD_DIM), 3, axis=2)
        o_na = neighborhood_attention(q_na[:, :, 0], k_na[:, :, 0], v_na[:, :, 0], na_rpb[i])

        q_mla = (_rms_norm(q_lat, q_norm_g[i]) @ w_uq[i]).reshape(b, s, MLA_HEADS, MLA_NOPE + MLA_ROPE)
        q_mla = jnp.concatenate([q_mla[..., :MLA_NOPE], _rope(q_mla[..., MLA_NOPE:], cos, sin)], axis=-1)
        kv = (_rms_norm(kv_lat, kv_norm_g[i]) @ w_ukv[i]).reshape(b, s, MLA_HEADS, MLA_NOPE + MLA_V)
        k_r = jnp.broadcast_to(_rope(k_rope[:, :, None, :], cos, sin), (b, s, MLA_HEADS, MLA_ROPE))
        k_mla = jnp.concatenate([kv[..., :MLA_NOPE], k_r], axis=-1)
        o_mla = latent_attention(q_mla, k_mla, kv[..., MLA_NOPE:])

        merged = (jax.nn.sigmoid(g_na + b_gate[i, :D_MODEL]) * (o_na @ w_na_o[i])
                  + jax.nn.sigmoid(g_mla + b_gate[i, D_MODEL:]) * (o_mla @ w_mla_o[i]))
        x = _layer_norm(DEEPNORM_ALPHA * x + merged @ w_out[i], ln1_g[i], ln1_b[i])

        if i % 2 == 0:
            j = i // 2
            f = _swiglu(x, ffn_w1[j], ffn_w3[j], ffn_w2[j])
        else:
            j = i // 2
            f = moe_swiglu(x, moe_w_router[j], moe_b_router[j], moe_w1[j], moe_w3[j], moe_w2[j])

        ple = jax.nn.sigmoid(x @ w_ple_gate[i]) * (p[i] @ w_ple[i])
        x = _layer_norm(DEEPNORM_ALPHA * x + f + ple, ln2_g[i], ln2_b[i])
    return x
```

```python
import math
from contextlib import ExitStack

import numpy as np
import concourse.bass as bass
import concourse.mybir as mybir
from concourse.bass_utils import run_bass_kernel_spmd

F32 = mybir.dt.float32
BF16 = mybir.dt.bfloat16
AF = mybir.ActivationFunctionType
ALU = mybir.AluOpType
AX = mybir.AxisListType

D = 1024
S = 8192
DEPTH = 2
NCORES = 8
PLE = 256
GRID_W = 64
NA_H = 8
NA_D = 64
MLA_H = 8
NOPE = 64
ROPE = 32
MLA_V = 64
QL = 768
KVL = 256
N_IN = 4640
D_FF = 2816
NE = 8
D_FFE = 3584
ALPHA = (2 * DEPTH) ** 0.25
LN_EPS = 1e-5
RMS_EPS = 1e-6
NA_SCALE = NA_D ** -0.5
MLA_SCALE = (NOPE + ROPE) ** -0.5
NEG = -1e30

C_NAQ, C_NAK, C_NAV = 0, 512, 1024
C_QL = 1536
C_KVL = 2304
C_KR = 2560
C_GNA = 2592
C_GMLA = 3616

U_F = 29
U_S = 22

SAME_ENG_WAIT = True


class Eng:
    def __init__(self, fw, name, h):
        self.fw, self.name, self.h = fw, name, h
        self.sem = fw.new_sem("e_" + name)
        self.count = 0
        self.waited = {}
        self.pending = []

    def wait(self, ev):
        if ev is None:
            return
        sem, val = ev
        if sem is self.sem and (self.name == "pe" or not SAME_ENG_WAIT):
            return
        key = id(sem)
        if self.waited.get(key, 0) >= val:
            return
        self.h.wait_ge(sem, val)
        self.waited[key] = val


class DSem:
    def __init__(self, fw):
        self.sem = fw.new_sem("d%d" % fw.nsem)
        self.count = 0
        fw.all_dsems.append(self)


class Buf:
    def __init__(self, name):
        self.name = name
        self.w = None
        self.r = {}
        self.dsem = None


class PendingEv:
    def __init__(self, eng):
        self.ev = None
        self.eng = eng


class FW:
    def __init__(self, nc, es):
        self.nc, self.es = nc, es
        self.nsem = 0
        self.uid = 0
        self.scope_es = None
        self.scope_stack = []
        self.pe = Eng(self, "pe", nc.tensor)
        self.act = Eng(self, "act", nc.scalar)
        self.dve = Eng(self, "dve", nc.vector)
        self.pool = Eng(self, "pool", nc.gpsimd)
        self.sp = Eng(self, "sp", nc.sync)
        self.all_dsems = []
        self.dsem_free = []
        self.scope_dsems = [[]]

    def new_sem(self, name):
        self.nsem += 1
        return self.es.enter_context(self.nc.semaphore(name))

    @staticmethod
    def _res(eng, ev):
        if isinstance(ev, PendingEv):
            if ev.ev is None:
                assert ev.eng is eng, "dependency on a non-incremented instruction"
                return None
            return ev.ev
        return ev

    def _deps(self, eng, reads, writes):
        for b in reads:
            eng.wait(self._res(eng, b.w))
        for b in writes:
            eng.wait(self._res(eng, b.w))
            for ev in b.r.values():
                eng.wait(self._res(eng, ev))

    def barrier(self):
        engs = [self.pe, self.act, self.dve, self.pool, self.sp]
        evs = [(e.sem, e.count) for e in engs if e.count > 0]
        evs += [(s.sem, s.count) for s in self.all_dsems if s.count > 0]
        for e in engs:
            for ev in evs:
                if ev[0] is e.sem:
                    continue
                e.wait(ev)

    def op(self, eng, fn, reads=(), writes=(), inc=True):
        self._deps(eng, reads, writes)
        ins = fn()
        if inc:
            ins.then_inc(eng.sem, 1)
            eng.count += 1
            ev = (eng.sem, eng.count)
            for pe_ in eng.pending:
                pe_.ev = ev
            eng.pending = []
        else:
            ev = PendingEv(eng)
            eng.pending.append(ev)
        for b in reads:
            b.r[eng.name] = ev
        for b in writes:
            b.w = ev
            b.r = {}
        return ins

    def dma(self, q, out, in_, reads=(), writes=(), **kw):
        owner = (list(writes) + list(reads))[0]
        if owner.dsem is None:
            owner.dsem = self.dsem_free.pop() if self.dsem_free else DSem(self)
            self.scope_dsems[-1].append(owner)
        st = owner.dsem
        self._deps(q, reads, writes)
        ins = q.h.dma_start(out=out, in_=in_, **kw)
        ins.then_inc(st.sem, 16)
        st.count += 16
        ev = (st.sem, st.count)
        for b in reads:
            b.r["dma_" + str(id(st))] = ev
        for b in writes:
            b.w = ev
            b.r = {}
        return ev

    def sb(self, name, shape, dtype):
        self.uid += 1
        return self.scope_es.enter_context(self.nc.sbuf_tensor(f"{name}_{self.uid}", list(shape), dtype))

    def ps(self, name, shape, dtype=F32):
        self.uid += 1
        return self.scope_es.enter_context(self.nc.psum_tensor(f"{name}_{self.uid}", list(shape), dtype))

    def open_scope(self):
        self.scope_stack.append(self.scope_es)
        self.scope_es = ExitStack()
        self.scope_dsems.append([])
        return self.scope_es

    def close_scope(self):
        self.barrier()
        self.scope_es.close()
        self.scope_es = self.scope_stack.pop()
        for b in self.scope_dsems.pop():
            self.dsem_free.append(b.dsem)
            b.dsem = None


class Ring:
    def __init__(self, fw, name, n, shape, dtype, psum=False):
        self.slots = []
        for i in range(n):
            t = fw.ps(f"{name}{i}", shape, dtype) if psum else fw.sb(f"{name}{i}", shape, dtype)
            self.slots.append((t, Buf(f"{name}{i}")))
        self.i = 0

    def next(self):
        s = self.slots[self.i % len(self.slots)]
        self.i += 1
        return s


class Prog:
    def __init__(self, cfg):
        self.cfg = cfg
        self.nc = nc = bass.Bass("TRN2", target_bir_lowering=False)
        self.es = ExitStack()
        self.fw = FW(nc, self.es)
        self.dram = {}

    def din(self, name, shape, dtype=F32):
        t = self.nc.dram_tensor(name, list(shape), dtype, kind="ExternalInput").ap()
        self.dram[name] = t
        return t

    def dout(self, name, shape, dtype=F32):
        t = self.nc.dram_tensor(name, list(shape), dtype, kind="ExternalOutput").ap()
        self.dram[name] = t
        return t

    def dscr(self, name, shape, dtype):
        kind = "ExternalOutput" if name in self.cfg.get("debug_out", ()) else "Internal"
        if name in self.cfg.get("ext_in", ()):
            kind = "ExternalInput"
        t = self.nc.dram_tensor(name, list(shape), dtype, kind=kind).ap()
        self.dram[name] = t
        return t


def mm(fw, out, lhsT, rhs, start, stop, reads, writes, inc=None):
    nc = fw.nc
    if inc is None:
        inc = stop
    return fw.op(fw.pe, lambda: nc.tensor.matmul(out, lhsT, rhs, start=start, stop=stop), reads, writes, inc=inc)


def load_consts(P):
    fw, nc, d = P.fw, P.nc, P.dram
    es = P.es
    c = {}
    c["ident_f"] = es.enter_context(nc.sbuf_tensor("ident_f", [128, 128], F32))
    c["ident_b"] = es.enter_context(nc.sbuf_tensor("ident_b", [128, 128], BF16))
    c["ones_b"] = es.enter_context(nc.sbuf_tensor("ones_b", [128, 128], BF16))
    c["ones_f"] = es.enter_context(nc.sbuf_tensor("ones_f", [128, 128], F32))
    c["B_ident_f"] = Buf("ident_f"); c["B_ident_b"] = Buf("ident_b")
    c["B_ones_b"] = Buf("ones_b"); c["B_ones_f"] = Buf("ones_f")
    fw.dma(fw.sp, c["ident_f"][:], d["c_ident"][:, :], writes=[c["B_ident_f"]])
    fw.op(fw.dve, lambda: nc.vector.tensor_copy(c["ident_b"][:], c["ident_f"][:]), [c["B_ident_f"]], [c["B_ident_b"]])
    fw.op(fw.dve, lambda: nc.vector.memset(c["ones_b"][:], 1.0), [], [c["B_ones_b"]])
    fw.op(fw.dve, lambda: nc.vector.memset(c["ones_f"][:], 1.0), [], [c["B_ones_f"]])
    P.c = c


def phase_A(P, L, x_src):
    fw, nc, d, c = P.fw, P.nc, P.dram, P.c
    TT = 512
    NT = P.cfg.get("ntiles_A", S // TT)
    fw.open_scope()
    sb, ps = fw.sb, fw.ps

    win = sb("win", [128, 8, N_IN], BF16); B_win = Buf("win")
    wuq = sb("wuq", [128, 6, 8, 96], BF16); B_wuq = Buf("wuq")
    wuqr = sb("wuqr", [128, 6, 8, 96], BF16); B_wuqr = Buf("wuqr")
    wkk = sb("wkk", [128, 2, 8, 64], BF16); B_wkk = Buf("wkk")
    wkv = sb("wkv", [128, 2, 512], BF16); B_wkv = Buf("wkv")
    wkr = sb("wkr", [128, 8, 96], BF16); B_wkr = Buf("wkr")
    wkrr = sb("wkrr", [128, 8, 96], BF16); B_wkrr = Buf("wkrr")
    gq = sb("gq", [128, 6], F32); B_gq = Buf("gq")
    gkv = sb("gkv", [128, 2], F32); B_gkv = Buf("gkv")
    bg = sb("bg", [128, 16], F32); B_bg = Buf("bg")
    fw.open_scope()
    wst = Ring(fw, "wst", 3, [128, 1160], F32)

    fw.dma(fw.sp, gq[:], d["q_norm_g"][L].rearrange("(k p) -> p k", p=128), writes=[B_gq], allow_slow_non_contiguous=True)
    fw.dma(fw.sp, gkv[:], d["kv_norm_g"][L].rearrange("(k p) -> p k", p=128), writes=[B_gkv], allow_slow_non_contiguous=True)
    fw.dma(fw.sp, bg[:], d["b_gate"][L].rearrange("(k p) -> p k", p=128), writes=[B_bg], allow_slow_non_contiguous=True)

    first = True
    for k in range(8):
        for qd in range(4):
            t, B = wst.next()
            fw.dma(fw.sp, t[:, :], d["w_in"][L, k * 128:(k + 1) * 128, qd * 1160:(qd + 1) * 1160], writes=[B])
            eng = fw.dve if (k * 4 + qd) % 2 == 0 else fw.pool
            fw.op(eng, lambda t=t, eng=eng: eng.h.tensor_copy(win[:, k, qd * 1160:(qd + 1) * 1160], t[:, :]), [B], [B_win])
    fw.op(fw.dve, lambda: nc.vector.memset(wkr[:], 0.0), [], [B_wkr])
    fw.op(fw.dve, lambda: nc.vector.memset(wkrr[:], 0.0), [], [B_wkrr])
    fw.op(fw.dve, lambda: nc.vector.tensor_copy(wkr[:, :, 64:96], win[:, :, C_KR:C_KR + 32]), [B_win], [B_wkr])
    fw.op(fw.dve, lambda: nc.vector.tensor_scalar(out=wkrr[:, :, 64:80], in0=win[:, :, C_KR + 16:C_KR + 32], scalar1=-1.0, scalar2=None, op0=ALU.mult), [B_win], [B_wkrr])
    fw.op(fw.dve, lambda: nc.vector.tensor_copy(wkrr[:, :, 80:96], win[:, :, C_KR:C_KR + 16]), [B_win], [B_wkrr])
    for k in range(6):
        t, B = wst.next()
        fw.dma(fw.sp, t[:, 0:768], d["w_uq"][L, k * 128:(k + 1) * 128, :], writes=[B])
        fw.op(fw.dve, lambda t=t, k=k: nc.vector.tensor_scalar(out=wuq[:, k, :, :], in0=t[:, 0:768].rearrange("p (h e) -> p h e", h=8), scalar1=gq[:, k:k + 1], scalar2=None, op0=ALU.mult), [B, B_gq], [B_wuq])
    fw.op(fw.pool, lambda: nc.gpsimd.tensor_copy(wuqr[:, :, :, 0:64], wuq[:, :, :, 0:64]), [B_wuq], [B_wuqr])
    fw.op(fw.dve, lambda: nc.vector.tensor_scalar(out=wuqr[:, :, :, 64:80], in0=wuq[:, :, :, 80:96], scalar1=-1.0, scalar2=None, op0=ALU.mult), [B_wuq], [B_wuqr])
    fw.op(fw.pool, lambda: nc.gpsimd.tensor_copy(wuqr[:, :, :, 80:96], wuq[:, :, :, 64:80]), [B_wuq], [B_wuqr])
    for k in range(2):
        t, B = wst.next()
        fw.dma(fw.sp, t[:, 0:1024], d["w_ukv"][L, k * 128:(k + 1) * 128, :], writes=[B])
        tv = t[:, 0:1024].rearrange("p (h e) -> p h e", h=8)
        fw.op(fw.dve, lambda tv=tv, k=k: nc.vector.tensor_scalar(out=wkk[:, k, :, :], in0=tv[:, :, 0:64], scalar1=gkv[:, k:k + 1], scalar2=None, op0=ALU.mult), [B, B_gkv], [B_wkk])
        fw.op(fw.dve, lambda tv=tv, k=k: nc.vector.tensor_scalar(out=wkv[:, k, :].rearrange("p (h e) -> p h e", h=8), in0=tv[:, :, 64:128], scalar1=gkv[:, k:k + 1], scalar2=None, op0=ALU.mult), [B, B_gkv], [B_wkv])

    fw.close_scope()
    xs_ring = Ring(fw, "xs", 2, [128, D], F32)
    xb_ring = Ring(fw, "xb", 2, [128, D], BF16)
    xT_ring = Ring(fw, "xT", 2, [128, 8, TT], BF16)
    out_ring = Ring(fw, "og", 12, [128, TT], BF16)
    cs_ring = Ring(fw, "cs", 2, [96, 2, TT], F32)
    csr_ring = Ring(fw, "csr", 1, [96, 2, TT], F32)
    lat_ring = Ring(fw, "lat", 2, [128, 8, TT], BF16)
    sq_ring = Ring(fw, "sq", 3, [128, TT], BF16)
    rq_ring = Ring(fw, "rq", 1, [128, TT], F32)
    rk_ring = Ring(fw, "rk", 1, [128, TT], F32)
    t1_ring = Ring(fw, "t1", 2, [96, TT], F32)
    t2_ring = Ring(fw, "t2", 2, [96, TT], F32)
    eps_q = sb("epsq", [128, 1], F32); B_eps = Buf("eps")
    eps_k = sb("epsk", [128, 1], F32)
    fw.op(fw.dve, lambda: nc.vector.memset(eps_q[:], RMS_EPS), [], [B_eps])
    fw.op(fw.dve, lambda: nc.vector.memset(eps_k[:], RMS_EPS), [], [B_eps])
    pmm = Ring(fw, "pmm", 4, [128, TT], F32, psum=True)
    ptr = Ring(fw, "ptr", 2, [128, 4, 128], BF16, psum=True)
    pss = Ring(fw, "pss", 2, [128, TT], F32, psum=True)

    evac_i = [0]

    def store(src_tile, Bsrc, dst, npart=128):
        fw.dma(fw.sp, dst, src_tile, reads=[Bsrc])

    def evac_copy(eng, out_ap, in_ap, reads, writes):
        if eng is fw.act:
            fw.op(eng, lambda: nc.scalar.copy(out_ap, in_ap), reads, writes)
        else:
            fw.op(eng, lambda: eng.h.tensor_copy(out_ap, in_ap), reads, writes)

    def prep(t):
        tok0 = t * TT
        xT, B_xT = xT_ring.next()
        for s4 in range(4):
            xs, B_xs = xs_ring.next()
            fw.dma(fw.sp, xs[:, :], x_src[tok0 + s4 * 128: tok0 + (s4 + 1) * 128, :], writes=[B_xs])
            xb, B_xb = xb_ring.next()
            fw.op(fw.dve, lambda xb=xb, xs=xs: nc.vector.tensor_copy(xb[:, :], xs[:, :]), [B_xs], [B_xb])
            for half in range(2):
                pt, B_pt = ptr.next()
                for j in range(4):
                    fc = half * 4 + j
                    fw.op(fw.pe, lambda pt=pt, j=j, fc=fc, xb=xb: nc.tensor.transpose(pt[:, j, :], xb[:, fc * 128:(fc + 1) * 128], c["ident_b"][:]),
                          [B_xb, c["B_ident_b"]], [B_pt], inc=(j == 3))
                fw.op(fw.dve, lambda pt=pt, half=half, s4=s4, xT=xT: nc.vector.tensor_copy(xT[:, half * 4:(half + 1) * 4, s4 * 128:(s4 + 1) * 128], pt[:, :, :]),
                      [B_pt], [B_xT])
        cs, B_cs = cs_ring.next()
        fw.dma(fw.sp, cs[64:96, 0, :], d["c_cos"][:, tok0:tok0 + TT], writes=[B_cs])
        fw.dma(fw.sp, cs[64:96, 1, :], d["c_sin"][:, tok0:tok0 + TT], writes=[B_cs])
        return xT, B_xT, cs, B_cs

    nxt_prep = prep(0) if NT > 0 else None
    for t in range(NT):
        tok0 = t * TT
        xT, B_xT, cs, B_cs = nxt_prep
        nxt_prep = None

        def proj_fm(c0, mw, wt=None, Bw=None, kk=8, rhs_fn=None, rhsB=None):
            pt, B_pt = pmm.next()
            for k in range(kk):
                lhsT = win[:, k, c0:c0 + mw] if wt is None else wt(k)
                rhs = xT[:, k, :] if rhs_fn is None else rhs_fn(k)
                mm(fw, pt[0:mw, :], lhsT, rhs, k == 0, k == kk - 1,
                   [B_win if Bw is None else Bw, B_xT if rhsB is None else rhsB], [B_pt])
            return pt, B_pt

        for which, c0, dst in (("q", C_NAQ, d[f"qT_na{L}"]), ("k", C_NAK, d[f"kT_na{L}"])):
            for m in range(4):
                pt, B_pt = proj_fm(c0 + m * 128, 128)
                og, B_og = out_ring.next()
                evac_copy(fw.act, og[:, :], pt[:, :], [B_pt], [B_og])
                store(og[:, :], B_og, dst[m * 128:(m + 1) * 128, tok0:tok0 + TT])
        for s4 in range(4):
            pt, B_pt = pmm.next()
            for k in range(8):
                mm(fw, pt[:, :], xT[:, k, s4 * 128:(s4 + 1) * 128], win[:, k, C_NAV:C_NAV + 512], k == 0, k == 7, [B_win, B_xT], [B_pt])
            og, B_og = out_ring.next()
            evac_copy(fw.dve, og[:, :], pt[:, :], [B_pt], [B_og])
            store(og[:, :], B_og, d[f"v_na{L}"][tok0 + s4 * 128: tok0 + (s4 + 1) * 128, :])
        for m in range(16):
            pt, B_pt = proj_fm(C_GNA + m * 128, 128)
            og, B_og = out_ring.next()
            fw.op(fw.act, lambda og=og, pt=pt, m=m: nc.scalar.activation(og[:, :], pt[:, :], AF.Sigmoid, bias=bg[:, m:m + 1], scale=1.0), [B_pt, B_bg], [B_og])
            store(og[:, :], B_og, d[f"sgT{L}"][m * 128:(m + 1) * 128, tok0:tok0 + TT])
        if t + 1 < NT:
            nxt_prep = prep(t + 1)
        lat, B_lat = lat_ring.next()
        rstd = {}
        for name, c0, nch, off, dim, rring in (("q", C_QL, 6, 0, QL, rq_ring), ("k", C_KVL, 2, 6, KVL, rk_ring)):
            pssq, B_pssq = pss.next()
            for m in range(nch):
                pt, B_pt = proj_fm(c0 + m * 128, 128)
                if "nodve" not in P.cfg.get("A_var", ""):
                    evac_copy(fw.dve, lat[:, off + m, :], pt[:, :], [B_pt], [B_lat])
                sq, B_sq = sq_ring.next()
                if "noact" in P.cfg.get("A_var", ""):
                    pass
                elif "nosquare" in P.cfg.get("A_var", ""):
                    fw.op(fw.act, lambda sq=sq, pt=pt: nc.scalar.activation(sq[:, :], pt[:, :], AF.Identity), [B_pt], [B_sq])
                else:
                    fw.op(fw.act, lambda sq=sq, m=m: nc.scalar.activation(sq[:, :], lat[:, off + m, :], AF.Square), [B_lat], [B_sq])
                if "noones" not in P.cfg.get("A_var", ""):
                    mm(fw, pssq[:, :], c["ones_b"][:, :], sq[:, :], m == 0, m == nch - 1, [c["B_ones_b"], B_sq], [B_pssq])
            r, B_r = rring.next()
            rstd[name] = (r, B_r)
            if "noones" in P.cfg.get("A_var", ""):
                continue
            var = P.cfg.get("A_var", "")
            if "nosqrt" in var:
                fw.op(fw.act, lambda r=r, pssq=pssq, dim=dim: nc.scalar.activation(r[:, :], pssq[:, :], AF.Identity, bias=eps_q[:, 0:1], scale=1.0 / dim), [B_pssq, B_eps], [B_r])
            else:
                fw.op(fw.act, lambda r=r, pssq=pssq, dim=dim: nc.scalar.activation(r[:, :], pssq[:, :], AF.Sqrt, bias=eps_q[:, 0:1], scale=1.0 / dim), [B_pssq, B_eps], [B_r])
            if "norecip" not in var:
                fw.op(fw.dve, lambda r=r: nc.vector.reciprocal(r[:, :], r[:, :]), [B_r], [B_r])
            rstd[name] = (r, B_r)
        rq, B_rq = rstd["q"]
        rk, B_rk = rstd["k"]
        csr, B_csr = csr_ring.next()
        for j in range(2):
            fw.op(fw.pool, lambda j=j, csr=csr, cs=cs, rq=rq: nc.gpsimd.tensor_tensor(out=csr[64:96, j, :], in0=cs[64:96, j, :], in1=rq[64:96, :], op=ALU.mult), [B_cs, B_rq], [B_csr])
        for h in range(8):
            pa, B_pa = proj_fm(0, 96, wt=lambda k, h=h: wuq[:, k, h, :], Bw=B_wuq, kk=6, rhs_fn=lambda k: lat[:, k, :], rhsB=B_lat)
            pb, B_pb = proj_fm(0, 96, wt=lambda k, h=h: wuqr[:, k, h, :], Bw=B_wuqr, kk=6, rhs_fn=lambda k: lat[:, k, :], rhsB=B_lat)
            og, B_og = out_ring.next()
            fw.op(fw.dve, lambda og=og, pa=pa: nc.vector.tensor_tensor(out=og[0:64, :], in0=pa[0:64, :], in1=rq[0:64, :], op=ALU.mult), [B_pa, B_rq], [B_og])
            t1, B_t1 = t1_ring.next()
            t2, B_t2 = t2_ring.next()
            fw.op(fw.dve, lambda t1=t1, pa=pa: nc.vector.tensor_tensor(out=t1[64:96, :], in0=pa[64:96, :], in1=csr[64:96, 0, :], op=ALU.mult), [B_pa, B_csr], [B_t1])
            fw.op(fw.dve, lambda t2=t2, pb=pb: nc.vector.tensor_tensor(out=t2[64:96, :], in0=pb[64:96, :], in1=csr[64:96, 1, :], op=ALU.mult), [B_pb, B_csr], [B_t2])
            fw.op(fw.pool, lambda og=og, t1=t1, t2=t2: nc.gpsimd.tensor_tensor(out=og[64:96, :], in0=t1[64:96, :], in1=t2[64:96, :], op=ALU.add), [B_t1, B_t2], [B_og])
            store(og[0:96, :], B_og, d[f"qT_mla{L}"][h, :, tok0:tok0 + TT])
        pa, B_pa = proj_fm(0, 96, wt=lambda k: wkr[:, k, :], Bw=B_wkr)
        pb, B_pb = proj_fm(0, 96, wt=lambda k: wkrr[:, k, :], Bw=B_wkrr)
        og, B_og = out_ring.next()
        t1, B_t1 = t1_ring.next()
        t2, B_t2 = t2_ring.next()
        fw.op(fw.dve, lambda: nc.vector.tensor_tensor(out=t1[64:96, :], in0=pa[64:96, :], in1=cs[64:96, 0, :], op=ALU.mult), [B_pa, B_cs], [B_t1])
        fw.op(fw.dve, lambda: nc.vector.tensor_tensor(out=t2[64:96, :], in0=pb[64:96, :], in1=cs[64:96, 1, :], op=ALU.mult), [B_pb, B_cs], [B_t2])
        fw.op(fw.pool, lambda: nc.gpsimd.tensor_tensor(out=og[64:96, :], in0=t1[64:96, :], in1=t2[64:96, :], op=ALU.add), [B_t1, B_t2], [B_og])
        store(og[64:96, :], B_og, d[f"krT{L}"][:, tok0:tok0 + TT])
        for h in range(8):
            pt, B_pt = proj_fm(0, 64, wt=lambda k, h=h: wkk[:, k, h, :], Bw=B_wkk, kk=2, rhs_fn=lambda k: lat[:, 6 + k, :], rhsB=B_lat)
            og, B_og = out_ring.next()
            fw.op(fw.dve, lambda og=og, pt=pt: nc.vector.tensor_tensor(out=og[0:64, :], in0=pt[0:64, :], in1=rk[0:64, :], op=ALU.mult), [B_pt, B_rk], [B_og])
            store(og[0:64, :], B_og, d[f"kT_mla{L}"][h, :, tok0:tok0 + TT])
        kvn = []
        for k in range(2):
            sqt, B_sqt = sq_ring.next()
            fw.op(fw.dve, lambda sqt=sqt, k=k: nc.vector.tensor_tensor(out=sqt[:, :], in0=lat[:, 6 + k, :], in1=rk[:, :], op=ALU.mult), [B_lat, B_rk], [B_sqt])
            kvn.append((sqt, B_sqt))
        for s4 in range(4):
            pt, B_pt = pmm.next()
            for k in range(2):
                mm(fw, pt[:, :], kvn[k][0][:, s4 * 128:(s4 + 1) * 128], wkv[:, k, :], k == 0, k == 1, [B_wkv, kvn[k][1]], [B_pt])
            og, B_og = out_ring.next()
            evac_copy(fw.act, og[:, :], pt[:, :], [B_pt], [B_og])
            store(og[:, :], B_og, d[f"v_mla{L}"][tok0 + s4 * 128: tok0 + (s4 + 1) * 128, :])
    fw.close_scope()


def host_consts():
    pos = np.arange(S, dtype=np.float32)
    inv_freq = (10000.0 ** (-np.arange(0, ROPE // 2, dtype=np.float32) * np.float32(2.0 / ROPE))).astype(np.float32)
    ang = (pos[None, :] * inv_freq[:, None]).astype(np.float32)
    cos = np.cos(ang).astype(np.float32)
    sin = np.sin(ang).astype(np.float32)
    dr_idx, dc_idx, validf, valids = na_host_index()
    return {
        "c_ident": np.eye(128, dtype=np.float32),
        "c_cos": np.ascontiguousarray(np.concatenate([cos, cos], 0)),
        "c_sin": np.ascontiguousarray(np.concatenate([sin, sin], 0)),
        "c_maskf": np.ascontiguousarray(np.where(validf, 0.0, NEG / NA_SCALE).astype(np.float32).reshape(128, U_F * 64)),
        "c_masks": np.ascontiguousarray(np.where(valids, 0.0, NEG / NA_SCALE).astype(np.float32).reshape(128, U_S * 64)),
    }


def host_na_table(na_rpb):
    dr_idx, dc_idx, validf, valids = na_host_index()
    g = na_rpb[:, :, dr_idx, dc_idx]
    g = np.where(validf[None, None], g, np.float32(0.0)).astype(np.float32)
    return np.ascontiguousarray(g.reshape(DEPTH, NA_H, 128, U_F * 64))


W_SHAPES = {
    "w_in": [DEPTH, D, N_IN], "b_gate": [DEPTH, 2 * D], "q_norm_g": [DEPTH, QL], "w_uq": [DEPTH, QL, 768],
    "kv_norm_g": [DEPTH, KVL], "w_ukv": [DEPTH, KVL, 1024], "w_na_o": [DEPTH, 512, D], "w_mla_o": [DEPTH, 512, D],
    "w_out": [DEPTH, D, D], "ln1_g": [DEPTH, D], "ln1_b": [DEPTH, D],
    "ffn_w1": [1, D, D_FF], "ffn_w3": [1, D, D_FF], "ffn_w2": [1, D_FF, D],
    "moe_w_router": [1, D, NE], "moe_b_router": [1, NE],
    "moe_w1": [1, NE, D, D_FFE], "moe_w3": [1, NE, D, D_FFE], "moe_w2": [1, NE, D_FFE, D],
    "w_ple_gate": [DEPTH, D, D], "w_ple": [DEPTH, PLE, D], "ln2_g": [DEPTH, D], "ln2_b": [DEPTH, D],
}
C_SHAPES = {"c_ident": [128, 128], "c_cos": [32, S], "c_sin": [32, S], "c_maskf": [128, U_F * 64], "c_masks": [128, U_S * 64],
            "c_natab": [DEPTH, NA_H, 128, U_F * 64]}


def declare_io(P):
    P.din("x", [S, D])
    P.din("p", [DEPTH, S, PLE])
    for k, shp in W_SHAPES.items():
        P.din(k, shp)
    for k, shp in C_SHAPES.items():
        P.din(k, shp)
    for L in range(DEPTH):
        P.dscr(f"qT_na{L}", [512, S], BF16)
        P.dscr(f"kT_na{L}", [512, S], BF16)
        P.dscr(f"v_na{L}", [S, 512], BF16)
        P.dscr(f"sgT{L}", [2048, S], BF16)
        P.dscr(f"qT_mla{L}", [8, 96, S], BF16)
        P.dscr(f"krT{L}", [32, S], BF16)
        P.dscr(f"kT_mla{L}", [8, 64, S], BF16)
        P.dscr(f"v_mla{L}", [S, 512], BF16)
        P.dscr(f"o_naT{L}", [512, S], BF16)
        P.dscr(f"o_mlaT{L}", [512, S], BF16)


def na_host_index():
    p = np.arange(128)
    krp, kc = p // 64, p % 64
    u = np.arange(U_F)
    qc = np.arange(64)
    dr = (krp[:, None, None] + 14 - u[None, :, None]) + 0 * qc[None, None, :]
    ws = np.clip(qc - 8, 0, 48)
    colvalid = (kc[:, None, None] >= ws[None, None, :]) & (kc[:, None, None] < ws[None, None, :] + 16)
    colvalid = colvalid & (dr == dr)
    dc = kc[:, None, None] - qc[None, None, :] + 15 + 0 * u[None, :, None]
    validf = (np.abs(dr) <= 7) & colvalid
    dr_idx = np.clip(dr + 7, 0, 14)
    dc_idx = np.clip(dc, 0, 30)
    valids = validf[:, 4:4 + U_S, :] & (dr[:, 4:4 + U_S, :] >= -4) & (dr[:, 4:4 + U_S, :] <= 3)
    return dr_idx, dc_idx, validf, valids


def phase_B(P, L, side_w=None):
    fw, nc, d, c = P.fw, P.nc, P.dram, P.c
    fw.open_scope()
    sb, ps = fw.sb, fw.ps
    NQB = 512

    pS = Ring(fw, "pS", 3, [128, 2, NQB], F32, psum=True)
    pacc = Ring(fw, "pacc", 2, [128, NQB], F32, psum=True)
    Pr = Ring(fw, "Pr", 4, [128, 2, NQB], BF16)
    pending_norm = [None]
    accs = Ring(fw, "accs", 2, [128, NQB], F32)
    rden = Ring(fw, "rden", 2, [128, NQB], F32)
    ost = Ring(fw, "ost", 3, [64, NQB], BF16)
    sqr = Ring(fw, "sqr", 2, [128, NQB], BF16)
    nmx = Ring(fw, "nmx", 2, [128, 34], F32)

    def norm_bound(QT, B_Q, KT, B_K, pb, Kd, scale):
        nm, B_nm = nmx.next()
        for which, (T, B_T) in enumerate(((QT, B_Q), (KT, B_K))):
            for blk in range(S // NQB):
                sq, B_sq = sqr.next()
                fw.op(fw.pool, lambda sq=sq, T=T, blk=blk: nc.gpsimd.tensor_tensor(out=sq[pb:pb + Kd, :], in0=T[pb:pb + Kd, blk * NQB:(blk + 1) * NQB], in1=T[pb:pb + Kd, blk * NQB:(blk + 1) * NQB], op=ALU.mult), [B_T], [B_sq])
                pn, B_pn = pS.next()
                mm(fw, pn[:, 0, :], c["ones_b"][pb:pb + Kd, :], sq[pb:pb + Kd, :], True, True, [c["B_ones_b"], B_sq], [B_pn])
                col = which * 16 + blk
                fw.op(fw.dve, lambda pn=pn, nm=nm, col=col: nc.vector.tensor_reduce(out=nm[:, col:col + 1], in_=pn[:, 0, :], axis=AX.X, op=ALU.max), [B_pn], [B_nm])
        fw.op(fw.dve, lambda nm=nm: nc.vector.tensor_reduce(out=nm[:, 32:33], in_=nm[:, 0:16], axis=AX.X, op=ALU.max), [B_nm], [B_nm])
        fw.op(fw.dve, lambda nm=nm: nc.vector.tensor_reduce(out=nm[:, 33:34], in_=nm[:, 16:32], axis=AX.X, op=ALU.max), [B_nm], [B_nm])
        fw.op(fw.dve, lambda nm=nm: nc.vector.tensor_tensor(out=nm[:, 32:33], in0=nm[:, 32:33], in1=nm[:, 33:34], op=ALU.mult), [B_nm], [B_nm])
        fw.op(fw.act, lambda nm=nm: nc.scalar.activation(nm[:, 33:34], nm[:, 32:33], AF.Sqrt, scale=scale * scale), [B_nm], [B_nm])
        fw.op(fw.dve, lambda nm=nm: nc.vector.tensor_scalar(out=nm[:, 33:34], in0=nm[:, 33:34], scalar1=-1.0, scalar2=None, op0=ALU.mult), [B_nm], [B_nm])
        return nm[:, 33:34], B_nm

    def attn_group(QT, B_Q, pb, Kd, q0, nq, items, scale, negc, B_negc, dst):
        pairs = [items[i:i + 2] for i in range(0, len(items), 2)]
        acc, B_acc = pacc.next()
        sbufs = {}

        def emit_S(pi):
            St, B_S = pS.next()
            for j, (kT, B_k, v, B_v, tab, B_tab) in enumerate(pairs[pi]):
                mm(fw, St[:, j, 0:nq], kT, QT[pb:pb + Kd, q0:q0 + nq], True, tab is None, [B_k, B_Q], [B_S])
                if tab is not None:
                    mm(fw, St[:, j, 0:nq], c["ident_b"][:, :], tab, False, True, [c["B_ident_b"], B_tab], [B_S])
            sbufs[pi] = (St, B_S)

        def emit_PV(pi):
            St, B_S = sbufs.pop(pi)
            n = len(pairs[pi])
            Pt, B_P = Pr.next()
            fw.op(fw.act, lambda: nc.scalar.activation(Pt[:, 0:n, 0:nq], St[:, 0:n, 0:nq], AF.Exp, bias=negc, scale=scale), [B_S, B_negc], [B_P])
            for j, (kT, B_k, v, B_v, tab, B_tab) in enumerate(pairs[pi]):
                first = (pi == 0 and j == 0)
                last = (pi == len(pairs) - 1 and j == n - 1)
                mm(fw, acc[0:65, 0:nq], v, Pt[:, j, 0:nq], first, last, [B_v, B_P], [B_acc])

        LA = 3
        for pi in range(min(LA, len(pairs))):
            emit_S(pi)
        for pi in range(len(pairs)):
            emit_PV(pi)
            if pi + LA < len(pairs):
                emit_S(pi + LA)

        def norm():
            a_s, B_as = accs.next()
            fw.op(fw.dve, lambda: nc.vector.tensor_copy(a_s[0:65, 0:nq], acc[0:65, 0:nq]), [B_acc], [B_as])
            rd, B_rd = rden.next()
            fw.op(fw.dve, lambda: nc.vector.reciprocal(rd[64:65, 0:nq], a_s[64:65, 0:nq]), [B_as], [B_rd])
            bc, B_bc = pS.next()
            mm(fw, bc[0:64, 0, 0:nq], c["ones_f"][64:65, 0:64], rd[64:65, 0:nq], True, True, [c["B_ones_f"], B_rd], [B_bc])
            o, B_o = ost.next()
            fw.op(fw.dve, lambda: nc.vector.tensor_tensor(out=o[0:64, 0:nq], in0=a_s[0:64, 0:nq], in1=bc[0:64, 0, 0:nq], op=ALU.mult), [B_as, B_bc], [B_o])
            fw.dma(fw.sp, dst, o[0:64, 0:nq], reads=[B_o])

        if pending_norm[0] is not None:
            pending_norm[0]()
        pending_norm[0] = norm

    def flush_norm():
        if pending_norm[0] is not None:
            pending_norm[0]()
            pending_norm[0] = None

    if P.cfg.get("do_na", True):
        fw.open_scope()
        mkf = sb("mkf", [128, U_F * 64], F32); B_mkf = Buf("mkf")
        mks = sb("mks", [128, U_S * 64], F32); B_mks = Buf("mks")
        fw.dma(fw.sp, mkf[:, :], d["c_maskf"][:, :], writes=[B_mkf])
        fw.dma(fw.sp, mks[:, :], d["c_masks"][:, :], writes=[B_mks])
        gst = Ring(fw, "gst", 2, [128, U_F * 64], F32)
        ttf = Ring(fw, "ttf", 2, [128, U_F * 64], BF16)
        tts = Ring(fw, "tts", 2, [128, U_S * 64], BF16)
        QTr = Ring(fw, "naQ", 2, [128, S], BF16)
        KTr = Ring(fw, "naK", 2, [128, S], BF16)
        Vr = Ring(fw, "naV", 2, [128, 64, 2, 65], BF16)
        heads = P.cfg.get("na_heads", list(range(NA_H)))
        pair_bufs = {}

        def na_load_pair(hp):
            if hp in pair_bufs:
                return
            QT, B_Q = QTr.next(); KT, B_K = KTr.next(); V, B_V = Vr.next()
            fw.dma(fw.sp, QT[:, :], d[f"qT_na{L}"][hp * 128:(hp + 1) * 128, :], writes=[B_Q])
            fw.dma(fw.sp, KT[:, :], d[f"kT_na{L}"][hp * 128:(hp + 1) * 128, :], writes=[B_K])
            fw.op(fw.pool, lambda V=V: nc.gpsimd.memset(V[:, :, :, :], 1.0), [], [B_V])
            for g in range(8):
                for hh in range(2):
                    fw.dma(fw.sp, V[:, g * 8:(g + 1) * 8, hh, 0:64],
                           d[f"v_na{L}"][g * 1024:(g + 1) * 1024, (hp * 2 + hh) * 64:(hp * 2 + hh + 1) * 64].rearrange("(kt p) e -> p kt e", p=128),
                           writes=[B_V])
            pair_bufs.clear() if len(pair_bufs) >= 2 else None
            pair_bufs[hp] = (QT, B_Q, KT, B_K, V, B_V)

        def na_tables(h):
            g_, B_g = gst.next()
            fw.dma(fw.sp, g_[:, :], d["c_natab"][L, h], writes=[B_g])
            tf, B_tf = ttf.next(); ts_, B_ts = tts.next()
            fw.op(fw.dve, lambda: nc.vector.scalar_tensor_tensor(out=tf[:, :], in0=g_[:, :], scalar=1.0 / NA_SCALE, in1=mkf[:, :], op0=ALU.mult, op1=ALU.add), [B_g, B_mkf], [B_tf])
            fw.op(fw.dve, lambda: nc.vector.scalar_tensor_tensor(out=ts_[:, :], in0=g_[:, 4 * 64:(4 + U_S) * 64], scalar=1.0 / NA_SCALE, in1=mks[:, :], op0=ALU.mult, op1=ALU.add), [B_g, B_mks], [B_ts])
            return tf, B_tf, ts_, B_ts

        def na_bound(h):
            QT, B_Q, KT, B_K, V, B_V = pair_bufs[h // 2]
            return norm_bound(QT, B_Q, KT, B_K, (h % 2) * 64, 64, NA_SCALE)

        prepped = {}
        na_load_pair(heads[0] // 2)
        prepped[heads[0]] = (na_tables(heads[0]), na_bound(heads[0]))
        for hi, h in enumerate(heads):
            hp, pb = h // 2, (h % 2) * 64
            hn = heads[hi + 1] if hi + 1 < len(heads) else None
            QT, B_Q, KT, B_K, V, B_V = pair_bufs[hp]
            (tf, B_tf, ts_, B_ts), (negc, B_negc) = prepped.pop(h)
            if hn is not None and (h % 2 == 0 or hn // 2 == hp):
                if hn // 2 != hp and len(pair_bufs) < 2:
                    na_load_pair(hn // 2)
            dst_all = d[f"o_naT{L}"][h * 64:(h + 1) * 64, :]

            def item(kt, tab):
                return (KT[pb:pb + 64, kt * 128:(kt + 1) * 128], B_K, V[:, kt, h % 2, :], B_V, tab, None)

            groups = []
            for r in range(4):
                its = [item(i, tf[:, (14 - 2 * i + r) * 64:(14 - 2 * i + r) * 64 + 64]) for i in range(4)]
                groups.append((r * 64, 64, its, B_tf))
            its = [item(i - 2, ts_[:, (2 * (7 - i) + 4) * 64:(2 * (7 - i) + 4) * 64 + 256]) for i in range(2, 8)]
            groups.append((256, 256, its, B_ts))
            for j in range(1, 15):
                its = [item(4 * j - 2 + i, ts_[:, 2 * (7 - i) * 64:2 * (7 - i) * 64 + 512]) for i in range(8)]
                groups.append((512 * j, 512, its, B_ts))
            its = [item(58 + i, ts_[:, 2 * (7 - i) * 64:2 * (7 - i) * 64 + 320]) for i in range(6)]
            groups.append((7680, 320, its, B_ts))
            for r in range(125, 128):
                its = [item(60 + i, tf[:, (14 - 2 * i + r - 120) * 64:(14 - 2 * i + r - 120) * 64 + 64]) for i in range(4)]
                groups.append((r * 64, 64, its, B_tf))
            gsel = P.cfg.get("na_groups", None)
            sel = [g for gi, g in enumerate(groups) if gsel is None or gi in gsel]
            for gi, (q0, nq, its, B_tab) in enumerate(sel):
                its = [(a_, b_, v, bv, tab, B_tab) for (a_, b_, v, bv, tab, _) in its]
                attn_group(QT, B_Q, pb, 64, q0, nq, its, NA_SCALE, negc, B_negc, dst_all[:, q0:q0 + nq])
                if hn is not None and gi == (len(sel) * 2) // 3:
                    if hn // 2 not in pair_bufs:
                        if len(pair_bufs) >= 2:
                            pair_bufs.pop(min(pair_bufs))
                        na_load_pair(hn // 2)
                    prepped[hn] = (na_tables(hn), na_bound(hn))
            if hn is not None and hn not in prepped:
                if hn // 2 not in pair_bufs:
                    if len(pair_bufs) >= 2:
                        pair_bufs.pop(min(pair_bufs))
                    na_load_pair(hn // 2)
                prepped[hn] = (na_tables(hn), na_bound(hn))
        flush_norm()
        fw.close_scope()

    if P.cfg.get("do_mla", True):
        fw.open_scope()
        QTr = Ring(fw, "mQ", 2, [96, S], BF16)
        KTr = Ring(fw, "mK", 2, [96, S], BF16)
        Vr = Ring(fw, "mV", 2, [128, 64, 65], BF16)
        heads = P.cfg.get("mla_heads", list(range(MLA_H)))
        qblocks = P.cfg.get("mla_qblocks", list(range(S // NQB)))
        wgen, w_per_group = None, 0
        if side_w is not None:
            wc = WConv(P, [fw.pool])
            wc.alloc(nbig=1)
            wgen = wc.units(side_w)
            w_per_group = 7

        def step_w(n):
            nonlocal wgen
            for _ in range(n):
                if wgen is None:
                    return
                try:
                    next(wgen)
                except StopIteration:
                    wgen = None

        def mla_load(h):
            QT, B_Q = QTr.next(); KT, B_K = KTr.next(); V, B_V = Vr.next()
            fw.dma(fw.sp, QT[:, :], d[f"qT_mla{L}"][h], writes=[B_Q])
            fw.dma(fw.sp, KT[0:64, :], d[f"kT_mla{L}"][h], writes=[B_K])
            fw.dma(fw.sp, KT[64:96, :], d[f"krT{L}"][:, :], writes=[B_K])
            fw.op(fw.pool, lambda V=V: nc.gpsimd.memset(V[:, :, :], 1.0), [], [B_V])
            for g in range(8):
                fw.dma(fw.sp, V[:, g * 8:(g + 1) * 8, 0:64],
                       d[f"v_mla{L}"][g * 1024:(g + 1) * 1024, h * 64:(h + 1) * 64].rearrange("(kt p) e -> p kt e", p=128),
                       writes=[B_V])
            return QT, B_Q, KT, B_K, V, B_V

        cur = mla_load(heads[0])
        cur_neg = norm_bound(cur[0], cur[1], cur[2], cur[3], 0, 96, MLA_SCALE)
        for hi, h in enumerate(heads):
            QT, B_Q, KT, B_K, V, B_V = cur
            negc, B_negc = cur_neg
            nxt = mla_load(heads[hi + 1]) if hi + 1 < len(heads) else None
            nxt_neg = None
            for qi, qb in enumerate(qblocks):
                its = [(KT[0:96, kt * 128:(kt + 1) * 128], B_K, V[:, kt, :], B_V, None, None) for kt in range(64)]
                attn_group(QT, B_Q, 0, 96, qb * NQB, NQB, its, MLA_SCALE, negc, B_negc, d[f"o_mlaT{L}"][h * 64:(h + 1) * 64, qb * NQB:(qb + 1) * NQB])
                step_w(w_per_group)
                if nxt is not None and qi == max(0, len(qblocks) - 4):
                    nxt_neg = norm_bound(nxt[0], nxt[1], nxt[2], nxt[3], 0, 96, MLA_SCALE)
            if nxt is not None and nxt_neg is None:
                nxt_neg = norm_bound(nxt[0], nxt[1], nxt[2], nxt[3], 0, 96, MLA_SCALE)
            cur, cur_neg = nxt, nxt_neg
        flush_norm()
        step_w(1 << 30)
        fw.close_scope()
    fw.close_scope()


def ffn_dims(L):
    if L % 2 == 0:
        return 1, D_FF, "ffn_w1", "ffn_w3", "ffn_w2"
    return NE, D_FFE, "moe_w1", "moe_w3", "moe_w2"


class WConv:
    def __init__(self, P, engs):
        self.P, self.engs, self.cnt = P, engs, 0
        self.pending_store = None

    def alloc(self, nbig=1):
        fw = self.P.fw
        self.stg = Ring(fw, "wstg", 3, [128, 2048], F32)
        self.cvt = Ring(fw, "wcvt", 3, [128, 2048], BF16)
        self.HC = 7
        self.big = Ring(fw, "w13b", nbig, [128, self.HC, 2, 8, 128], BF16)
        self.st2 = Ring(fw, "wst2", 2, [128, self.HC * 128], F32)

    def convert(self, out_ap, in_ap, reads, writes):
        fw, nc = self.P.fw, self.P.nc
        e = self.engs[self.cnt % len(self.engs)]
        self.cnt += 1
        if e is fw.act:
            fw.op(e, lambda: nc.scalar.copy(out_ap, in_ap), reads, writes)
        else:
            fw.op(e, lambda: e.h.tensor_copy(out_ap, in_ap), reads, writes)

    def flush_store(self):
        if self.pending_store is not None:
            dst, src, B_c = self.pending_store
            self.P.fw.dma(self.P.fw.sp, dst, src, reads=[B_c])
            self.pending_store = None

    def conv_rows(self, src, dst, ncols):
        fw = self.P.fw
        for c0 in range(0, ncols, 2048):
            w = min(2048, ncols - c0)
            s_, B_s = self.stg.next()
            fw.dma(fw.sp, s_[:, 0:w], src[:, c0:c0 + w], writes=[B_s])
            c_, B_c = self.cvt.next()
            self.convert(c_[:, 0:w], s_[:, 0:w], [B_s], [B_c])
            self.flush_store()
            self.pending_store = (dst[:, c0:c0 + w], c_[:, 0:w], B_c)

    def units(self, layers):
        P, fw, d = self.P, self.P.fw, self.P.dram
        HC = self.HC
        for L in layers:
            for name, K_ in (("w_na_o", 512), ("w_mla_o", 512), ("w_out", D), ("w_ple_gate", D), ("w_ple", PLE)):
                for k in range(K_ // 128):
                    self.conv_rows(d[name][L, k * 128:(k + 1) * 128, :], d[f"{name}_s{L}"][:, k, :], D)
                    yield
            ne, dff, n1, n3, n2 = ffn_dims(L)
            j = L // 2
            nch = dff // 128
            w1 = d[n1][j] if ne > 1 else d[n1]
            w3 = d[n3][j] if ne > 1 else d[n3]
            w2 = d[n2][j] if ne > 1 else d[n2]
            for e in range(ne):
                for c_ in range(nch):
                    self.conv_rows(w2[e, c_ * 128:(c_ + 1) * 128, :], d[f"w2s{L}"][e, c_], D)
                    yield
            self.flush_store()
            for e in range(ne):
                for c0 in range(0, nch, HC):
                    gn = min(HC, nch - c0)
                    bt, B_b = self.big.next()
                    for i, wsrc in enumerate((w1, w3)):
                        for k in range(8):
                            s_, B_s = self.st2.next()
                            fw.dma(fw.sp, s_[:, 0:gn * 128], wsrc[e, k * 128:(k + 1) * 128, c0 * 128:(c0 + gn) * 128], writes=[B_s])
                            self.convert(bt[:, 0:gn, i, k, :], s_[:, 0:gn * 128].rearrange("p (c j) -> p c j", j=128), [B_s], [B_b])
                            yield
                    for cc in range(gn):
                        fw.dma(fw.sp, d[f"w13s{L}"][e, c0 + cc], bt[:, cc, :, :, :].rearrange("p i k j -> p (i k j)"), reads=[B_b])
                    yield


def phase_W(P, layers):
    fw = P.fw
    fw.open_scope()
    wc = WConv(P, [fw.dve, fw.pool, fw.act])
    wc.alloc(nbig=2)
    for _ in wc.units(layers):
        pass
    fw.close_scope()


def declare_scratch_C(P):
    for L in range(DEPTH):
        P.dscr(f"w_na_o_s{L}", [128, 4, D], BF16)
        P.dscr(f"w_mla_o_s{L}", [128, 4, D], BF16)
        P.dscr(f"w_out_s{L}", [128, 8, D], BF16)
        P.dscr(f"w_ple_gate_s{L}", [128, 8, D], BF16)
        P.dscr(f"w_ple_s{L}", [128, 2, D], BF16)
        ne, dff, _, _, _ = ffn_dims(L)
        P.dscr(f"w13s{L}", [ne, dff // 128, 128, 2 * 8 * 128], BF16)
        P.dscr(f"w2s{L}", [ne, dff // 128, 128, D], BF16)
    P.dscr("x_mid", [S, D], F32)


def phase_C(P, L, x_src, x_dst):
    fw, nc, d, c = P.fw, P.nc, P.dram, P.c
    moe = (L % 2 == 1)
    ne, dff, _, _, _ = ffn_dims(L)
    nch = dff // 128
    TT = 1024
    NS = TT // 128
    NT = P.cfg.get("ntiles_C", S // TT)
    G = 4
    fw.open_scope()
    sb = fw.sb

    lnp = sb("lnp", [128, 4, D], F32); B_lnp = Buf("lnp")
    for i, nm in enumerate(("ln1_g", "ln1_b", "ln2_g", "ln2_b")):
        fw.dma(fw.sp, lnp[:, i, :], d[nm][L:L + 1, :].broadcast_to([128, D]), writes=[B_lnp])
    cst = sb("cst", [128, 4], F32); B_cst = Buf("cst")
    fw.op(fw.dve, lambda: nc.vector.memset(cst[:, 0:1], LN_EPS), [], [B_cst])
    fw.op(fw.dve, lambda: nc.vector.memset(cst[:, 1:2], -0.5), [], [B_cst])
    if moe:
        wr = sb("wr", [128, 8, NE], F32); B_wr = Buf("wr")
        fw.dma(fw.sp, wr[:, :, :], d["moe_w_router"][L // 2].rearrange("(k p) e -> p k e", p=128), writes=[B_wr])
        br = sb("br", [128, NE], F32); B_br = Buf("br")
        fw.dma(fw.sp, br[:, :], d["moe_b_router"][L // 2:L // 2 + 1, :].broadcast_to([128, NE]), writes=[B_br])
    gates = sb("gates", [128, NS, NE], F32); B_gates = Buf("gates")

    xacc = sb("xacc", [128, NS, D], F32)
    B_xa = [Buf(f"xacc{s}") for s in range(NS)]
    x1T = sb("x1T", [128, 8, TT], BF16); B_x1T = Buf("x1T")
    oT = Ring(fw, "oT", 2, [128, 4, 512], BF16)
    sgr = Ring(fw, "sgr", 4, [128, 512], BF16)
    mrg = Ring(fw, "mrg", 1, [128, 8, 512], BF16)
    tmp = Ring(fw, "tmpf", 3, [128, 512], F32)
    wring = Ring(fw, "wring", 4, [128, 4, D], BF16)
    x1Tf = Ring(fw, "x1Tf", 2, [128, 8, 128], F32)
    pfr = Ring(fw, "pfr", 2, [128, PLE], F32)
    pT = sb("pT", [128, 2, TT], BF16); B_pT = Buf("pT")
    w13r = Ring(fw, "w13r", 3, [128, 2, 8, 128], BF16)
    w2r = Ring(fw, "w2r", 2, [128, G, D], BF16)
    hTr = Ring(fw, "hTr", 2, [128, G, TT], BF16)
    silr = Ring(fw, "silr", 2, [128, 512], F32)
    stat = Ring(fw, "stat", 2, [128, 16], F32)
    rlg = Ring(fw, "rlg", 2, [128, 32], F32)
    pmm = Ring(fw, "pmmC", 6, [128, 512], F32, psum=True)
    ptr = Ring(fw, "ptrC", 2, [128, 4, 128], F32, psum=True)

    def load_w(name, k0, nk):
        wt, B_w = wring.next()
        fw.dma(fw.sp, wt[:, 0:nk, :], d[f"{name}_s{L}"][:, k0:k0 + nk, :], writes=[B_w])
        return wt, B_w

    def layer_norm(s, gi):
        st, B_st = stat.next()
        xs = xacc[:, s, :]
        for hh in range(2):
            fw.op(fw.dve, lambda hh=hh: nc.vector.bn_stats(st[:, hh * 6:(hh + 1) * 6], xacc[:, s, hh * 512:(hh + 1) * 512]), [B_xa[s]], [B_st])
        fw.op(fw.dve, lambda: nc.vector.bn_aggr(st[:, 12:14], st[:, 0:12]), [B_st], [B_st])
        fw.op(fw.pool, lambda: nc.gpsimd.tensor_scalar(out=st[:, 14:15], in0=st[:, 13:14], scalar1=cst[:, 0:1], scalar2=None, op0=ALU.add), [B_st, B_cst], [B_st])
        fw.op(fw.pool, lambda: nc.gpsimd.tensor_tensor(out=st[:, 14:15], in0=st[:, 14:15], in1=cst[:, 1:2], op=ALU.pow), [B_st, B_cst], [B_st])
        fw.op(fw.dve, lambda: nc.vector.scalar_tensor_tensor(out=xs, in0=xs, scalar=st[:, 12:13], in1=lnp[:, gi, :], op0=ALU.subtract, op1=ALU.mult), [B_st, B_lnp], [B_xa[s]])
        fw.op(fw.dve, lambda: nc.vector.scalar_tensor_tensor(out=xs, in0=xs, scalar=st[:, 14:15], in1=lnp[:, gi + 1, :], op0=ALU.mult, op1=ALU.add), [B_st, B_lnp], [B_xa[s]])

    wbr = {}
    pre = {}
    wo_pre = {}
    preloaded = False

    def c1a(t, th):
        if t not in wbr:
            wbr.clear()
            wbr[t] = (load_w("w_na_o", 0, 4), load_w("w_mla_o", 0, 4))
        (wna, B_wna), (wml, B_wml) = wbr[t]
        t0 = t * TT + th * 512
        ona, B_ona = oT.next()
        oml, B_oml = oT.next()
        fw.dma(fw.sp, ona[:, :, :], d[f"o_naT{L}"][:, t0:t0 + 512].rearrange("(k p) t -> p k t", p=128), writes=[B_ona])
        fw.dma(fw.sp, oml[:, :, :], d[f"o_mlaT{L}"][:, t0:t0 + 512].rearrange("(k p) t -> p k t", p=128), writes=[B_oml])
        mg, B_mg = mrg.next()
        for fc in range(8):
            sgn, B_sgn = sgr.next()
            sgm, B_sgm = sgr.next()
            fw.dma(fw.sp, sgn[:, :], d[f"sgT{L}"][fc * 128:(fc + 1) * 128, t0:t0 + 512], writes=[B_sgn])
            fw.dma(fw.sp, sgm[:, :], d[f"sgT{L}"][D + fc * 128:D + (fc + 1) * 128, t0:t0 + 512], writes=[B_sgm])
            pn, B_pn = pmm.next()
            for k in range(4):
                mm(fw, pn[:, :], wna[:, k, fc * 128:(fc + 1) * 128], ona[:, k, :], k == 0, k == 3, [B_wna, B_ona], [B_pn])
            pm, B_pm = pmm.next()
            for k in range(4):
                mm(fw, pm[:, :], wml[:, k, fc * 128:(fc + 1) * 128], oml[:, k, :], k == 0, k == 3, [B_wml, B_oml], [B_pm])
            t1, B_t1 = tmp.next()
            t2, B_t2 = tmp.next()
            fw.op(fw.dve, lambda: nc.vector.tensor_tensor(out=t1[:, :], in0=pn[:, :], in1=sgn[:, :], op=ALU.mult), [B_pn, B_sgn], [B_t1])
            fw.op(fw.dve, lambda: nc.vector.tensor_tensor(out=t2[:, :], in0=pm[:, :], in1=sgm[:, :], op=ALU.mult), [B_pm, B_sgm], [B_t2])
            fw.op(fw.pool, lambda: nc.gpsimd.tensor_tensor(out=mg[:, fc, :], in0=t1[:, :], in1=t2[:, :], op=ALU.add), [B_t1, B_t2], [B_mg])
        return mg, B_mg

    for t in range(NT):
        tok0 = t * TT
        if not preloaded:
            for s in range(NS):
                fw.dma(fw.sp, xacc[:, s, :], x_src[tok0 + s * 128: tok0 + (s + 1) * 128, :], writes=[B_xa[s]])
        preloaded = False
        for th in range(TT // 512):
            if (t, th) in pre:
                mg, B_mg = pre.pop((t, th))
            else:
                mg, B_mg = c1a(t, th)
            if th == 0:
                wo = wo_pre.pop(t) if t in wo_pre else [load_w("w_out", 0, 4), load_w("w_out", 4, 4)]
            for s4 in range(4):
                s = th * 4 + s4
                for half in range(2):
                    pp, B_pp = pmm.next()
                    for k in range(8):
                        wt, B_w = wo[k // 4]
                        mm(fw, pp[:, :], mg[:, k, s4 * 128:(s4 + 1) * 128], wt[:, k % 4, half * 512:(half + 1) * 512], k == 0, k == 7, [B_mg, B_w], [B_pp])
                    xh = xacc[:, s, half * 512:(half + 1) * 512]
                    fw.op(fw.dve, lambda: nc.vector.scalar_tensor_tensor(out=xh, in0=xh, scalar=ALPHA, in1=pp[:, :], op0=ALU.mult, op1=ALU.add), [B_pp], [B_xa[s]])
                layer_norm(s, 0)
        for s in range(NS):
            xf, B_xf = x1Tf.next()
            for hf in range(2):
                pt_, B_pt = ptr.next()
                for j in range(4):
                    fc = hf * 4 + j
                    fw.op(fw.pe, lambda fc=fc, j=j: nc.tensor.transpose(pt_[:, j, :], xacc[:, s, fc * 128:(fc + 1) * 128], c["ident_f"][:]),
                          [B_xa[s], c["B_ident_f"]], [B_pt], inc=(j == 3))
                fw.op(fw.dve, lambda: nc.vector.tensor_copy(x1T[:, hf * 4:(hf + 1) * 4, s * 128:(s + 1) * 128], pt_[:, :, :]), [B_pt], [B_x1T])
                if moe:
                    fw.op(fw.dve, lambda: nc.vector.tensor_copy(xf[:, hf * 4:(hf + 1) * 4, :], pt_[:, :, :]), [B_pt], [B_xf])
            if moe:
                pr_, B_pr = pmm.next()
                for k in range(8):
                    mm(fw, pr_[:, 0:NE], xf[:, k, :], wr[:, k, :], k == 0, k == 7, [B_xf, B_wr], [B_pr])
                lg, B_lg = rlg.next()
                fw.op(fw.dve, lambda: nc.vector.tensor_tensor(out=lg[:, 0:8], in0=pr_[:, 0:NE], in1=br[:, :], op=ALU.add), [B_pr, B_br], [B_lg])
                fw.op(fw.dve, lambda: nc.vector.max(out=lg[:, 8:16], in_=lg[:, 0:8]), [B_lg], [B_lg])
                fw.op(fw.dve, lambda: nc.vector.tensor_tensor(out=lg[:, 16:17], in0=lg[:, 8:9], in1=lg[:, 9:10], op=ALU.add), [B_lg], [B_lg])
                fw.op(fw.dve, lambda: nc.vector.tensor_scalar(out=lg[:, 24:32], in0=lg[:, 0:8], scalar1=2.0, scalar2=lg[:, 16:17], op0=ALU.mult, op1=ALU.subtract), [B_lg], [B_lg])
                fw.op(fw.act, lambda: nc.scalar.activation(lg[:, 24:32], lg[:, 24:32], AF.Sigmoid), [B_lg], [B_lg])
                fw.op(fw.dve, lambda: nc.vector.tensor_scalar(out=lg[:, 16:24], in0=lg[:, 0:8], scalar1=lg[:, 9:10], scalar2=None, op0=ALU.is_ge), [B_lg], [B_lg])
                fw.op(fw.dve, lambda: nc.vector.tensor_tensor(out=gates[:, s, :], in0=lg[:, 16:24], in1=lg[:, 24:32], op=ALU.mult), [B_lg], [B_gates])
            fw.op(fw.act, lambda: nc.scalar.mul(xacc[:, s, :], xacc[:, s, :], ALPHA), [], [B_xa[s]])
        for s in range(NS):
            pf, B_pf = pfr.next()
            fw.dma(fw.sp, pf[:, :], d["p"][L, tok0 + s * 128: tok0 + (s + 1) * 128, :], writes=[B_pf])
            pt_, B_pt = ptr.next()
            for k in range(2):
                fw.op(fw.pe, lambda k=k: nc.tensor.transpose(pt_[:, k, :], pf[:, k * 128:(k + 1) * 128], c["ident_f"][:]),
                      [B_pf, c["B_ident_f"]], [B_pt], inc=(k == 1))
            fw.op(fw.dve, lambda: nc.vector.tensor_copy(pT[:, :, s * 128:(s + 1) * 128], pt_[:, 0:2, :]), [B_pt], [B_pT])
        wpg = [load_w("w_ple_gate", 0, 4), load_w("w_ple_gate", 4, 4)]
        wp, B_wp = load_w("w_ple", 0, 2)
        for s in range(NS):
            for half in range(2):
                pg, B_pg = pmm.next()
                for k in range(8):
                    wt, B_w = wpg[k // 4]
                    mm(fw, pg[:, :], x1T[:, k, s * 128:(s + 1) * 128], wt[:, k % 4, half * 512:(half + 1) * 512], k == 0, k == 7, [B_x1T, B_w], [B_pg])
                pq, B_pq = pmm.next()
                for k in range(2):
                    mm(fw, pq[:, :], pT[:, k, s * 128:(s + 1) * 128], wp[:, k, half * 512:(half + 1) * 512], k == 0, k == 1, [B_pT, B_wp], [B_pq])
                sg_, B_sg = silr.next()
                fw.op(fw.act, lambda: nc.scalar.activation(sg_[:, :], pg[:, :], AF.Sigmoid), [B_pg], [B_sg])
                t1, B_t1 = tmp.next()
                fw.op(fw.dve, lambda: nc.vector.tensor_tensor(out=t1[:, :], in0=sg_[:, :], in1=pq[:, :], op=ALU.mult), [B_sg, B_pq], [B_t1])
                xh = xacc[:, s, half * 512:(half + 1) * 512]
                fw.op(fw.pool, lambda: nc.gpsimd.tensor_tensor(out=xh, in0=xh, in1=t1[:, :], op=ALU.add), [B_t1], [B_xa[s]])
        exps = P.cfg.get("experts", list(range(ne)))
        for e in exps:
            for c0 in range(0, nch, G):
                gn = min(G, nch - c0)
                w2t, B_w2 = w2r.next()
                fw.dma(fw.sp, w2t[:, 0:gn, :], d[f"w2s{L}"][e, c0:c0 + gn].rearrange("c p n -> p c n"), writes=[B_w2])
                hT, B_hT = hTr.next()
                for ci in range(gn):
                    w13, B_w13 = w13r.next()
                    fw.dma(fw.sp, w13[:, :, :, :], d[f"w13s{L}"][e, c0 + ci].rearrange("p (i k j) -> p i k j", i=2, k=8), writes=[B_w13])
                    for th in range(TT // 512):
                        pa, B_pa = pmm.next()
                        for k in range(8):
                            mm(fw, pa[:, :], w13[:, 0, k, :], x1T[:, k, th * 512:(th + 1) * 512], k == 0, k == 7, [B_w13, B_x1T], [B_pa])
                        pb_, B_pb_ = pmm.next()
                        for k in range(8):
                            mm(fw, pb_[:, :], w13[:, 1, k, :], x1T[:, k, th * 512:(th + 1) * 512], k == 0, k == 7, [B_w13, B_x1T], [B_pb_])
                        sl, B_sl = silr.next()
                        fw.op(fw.act, lambda: nc.scalar.activation(sl[:, :], pa[:, :], AF.Silu), [B_pa], [B_sl])
                        fw.op(fw.dve, lambda: nc.vector.tensor_tensor(out=hT[:, ci, th * 512:(th + 1) * 512], in0=sl[:, :], in1=pb_[:, :], op=ALU.mult), [B_sl, B_pb_], [B_hT])
                is_last = (e == exps[-1] and c0 + G >= nch)
                if is_last and t + 1 < NT:
                    pre[(t + 1, 0)] = c1a(t + 1, 0)
                    wo_pre[t + 1] = [load_w("w_out", 0, 4), load_w("w_out", 4, 4)]
                for s in range(NS):
                    for half in range(2):
                        pd, B_pd = pmm.next()
                        for ci in range(gn):
                            mm(fw, pd[:, :], hT[:, ci, s * 128:(s + 1) * 128], w2t[:, ci, half * 512:(half + 1) * 512], ci == 0, ci == gn - 1, [B_hT, B_w2], [B_pd])
                        xh = xacc[:, s, half * 512:(half + 1) * 512]
                        if moe:
                            fw.op(fw.dve, lambda: nc.vector.scalar_tensor_tensor(out=xh, in0=pd[:, :], scalar=gates[:, s, e:e + 1], in1=xh, op0=ALU.mult, op1=ALU.add), [B_pd, B_gates], [B_xa[s]])
                        else:
                            fw.op(fw.dve, lambda: nc.vector.tensor_tensor(out=xh, in0=xh, in1=pd[:, :], op=ALU.add), [B_pd], [B_xa[s]])
                    if is_last:
                        layer_norm(s, 2)
                        fw.dma(fw.sp, x_dst[tok0 + s * 128: tok0 + (s + 1) * 128, :], xacc[:, s, :], reads=[B_xa[s]])
                        if t + 1 < NT:
                            fw.dma(fw.sp, xacc[:, s, :], x_src[tok0 + TT + s * 128: tok0 + TT + (s + 1) * 128, :], writes=[B_xa[s]])
                            preloaded = True
    fw.close_scope()


def build_program(cfg=None):
    cfg = dict(cfg or {})
    P = Prog(cfg)
    declare_io(P)
    declare_scratch_C(P)
    out = P.dout("out", [S, D])
    load_consts(P)
    layers = cfg.get("layers", list(range(DEPTH)))
    overlap = cfg.get("overlap_w", True) and len(layers) > 1 and cfg.get("do_B", True) and cfg.get("do_mla", True)
    phase_W(P, layers[:1] if overlap else layers)
    for L in layers:
        x_src = P.dram["x"] if L == layers[0] else P.dram["x_mid"]
        x_dst = out if L == layers[-1] else P.dram["x_mid"]
        if cfg.get("do_A", True):
            phase_A(P, L, x_src)
        if cfg.get("do_B", True):
            phase_B(P, L, side_w=(layers[1:] if (overlap and L == layers[0]) else None))
        if cfg.get("do_C", True):
            phase_C(P, L, x_src, x_dst)
    P.fw.barrier()
    P.es.close()
    return P


def make_in_maps(inputs, cores):
    consts = host_consts()
    consts["c_natab"] = host_na_table(np.asarray(inputs["na_rpb"], dtype=np.float32))
    shared = {k: np.ascontiguousarray(np.asarray(inputs[k], dtype=np.float32)) for k in W_SHAPES}
    shared.update(consts)
    maps = []
    for b in cores:
        m = dict(shared)
        m["x"] = np.ascontiguousarray(np.asarray(inputs["x"][b], dtype=np.float32))
        m["p"] = np.ascontiguousarray(np.asarray(inputs["p"][:, b], dtype=np.float32))
        maps.append(m)
    return maps


def kernel(**inputs):
    P = build_program()
    maps = make_in_maps(inputs, list(range(NCORES)))
    res = run_bass_kernel_spmd(P.nc, maps, core_ids=list(range(NCORES)))
    return np.stack([np.asarray(r["out"], dtype=np.float32) for r in res.results], axis=0)
```

```python
import math
from contextlib import ExitStack

import numpy as np
import concourse.bass as bass
import concourse.mybir as mybir
from concourse.bass_utils import run_bass_kernel_spmd

F32 = mybir.dt.float32
BF16 = mybir.dt.bfloat16
AF = mybir.ActivationFunctionType
ALU = mybir.AluOpType
AX = mybir.AxisListType

D = 1024
S = 8192
DEPTH = 2
NCORES = 8
PLE = 256
GRID_W = 64
NA_H = 8
NA_D = 64
MLA_H = 8
NOPE = 64
ROPE = 32
MLA_V = 64
QL = 768
KVL = 256
N_IN = 4640
D_FF = 2816
NE = 8
D_FFE = 3584
ALPHA = (2 * DEPTH) ** 0.25
LN_EPS = 1e-5
RMS_EPS = 1e-6
NA_SCALE = NA_D ** -0.5
MLA_SCALE = (NOPE + ROPE) ** -0.5
NEG = -1e30

C_NAQ, C_NAK, C_NAV = 0, 512, 1024
C_QL = 1536
C_KVL = 2304
C_KR = 2560
C_GNA = 2592
C_GMLA = 3616

U_F = 29
U_S = 22

SAME_ENG_WAIT = True


class Eng:
    def __init__(self, fw, name, h):
        self.fw, self.name, self.h = fw, name, h
        self.sem = fw.new_sem("e_" + name)
        self.count = 0
        self.waited = {}
        self.pending = []

    def wait(self, ev):
        if ev is None:
            return
        sem, val = ev
        if sem is self.sem and (self.name == "pe" or not SAME_ENG_WAIT):
            return
        key = id(sem)
        if self.waited.get(key, 0) >= val:
            return
        self.h.wait_ge(sem, val)
        self.waited[key] = val


class DSem:
    def __init__(self, fw):
        self.sem = fw.new_sem("d%d" % fw.nsem)
        self.count = 0
        fw.all_dsems.append(self)


class Buf:
    def __init__(self, name):
        self.name = name
        self.w = None
        self.r = {}
        self.dsem = None


class PendingEv:
    def __init__(self, eng):
        self.ev = None
        self.eng = eng


class FW:
    def __init__(self, nc, es):
        self.nc, self.es = nc, es
        self.nsem = 0
        self.uid = 0
        self.scope_es = None
        self.scope_stack = []
        self.pe = Eng(self, "pe", nc.tensor)
        self.act = Eng(self, "act", nc.scalar)
        self.dve = Eng(self, "dve", nc.vector)
        self.pool = Eng(self, "pool", nc.gpsimd)
        self.sp = Eng(self, "sp", nc.sync)
        self.all_dsems = []
        self.dsem_free = []
        self.scope_dsems = [[]]

    def new_sem(self, name):
        self.nsem += 1
        return self.es.enter_context(self.nc.semaphore(name))

    @staticmethod
    def _res(eng, ev):
        if isinstance(ev, PendingEv):
            if ev.ev is None:
                assert ev.eng is eng, "dependency on a non-incremented instruction"
                return None
            return ev.ev
        return ev

    def _deps(self, eng, reads, writes):
        for b in reads:
            eng.wait(self._res(eng, b.w))
        for b in writes:
            eng.wait(self._res(eng, b.w))
            for ev in b.r.values():
                eng.wait(self._res(eng, ev))

    def barrier(self):
        engs = [self.pe, self.act, self.dve, self.pool, self.sp]
        evs = [(e.sem, e.count) for e in engs if e.count > 0]
        evs += [(s.sem, s.count) for s in self.all_dsems if s.count > 0]
        for e in engs:
            for ev in evs:
                if ev[0] is e.sem:
                    continue
                e.wait(ev)

    def op(self, eng, fn, reads=(), writes=(), inc=True):
        self._deps(eng, reads, writes)
        ins = fn()
        if inc:
            ins.then_inc(eng.sem, 1)
            eng.count += 1
            ev = (eng.sem, eng.count)
            for pe_ in eng.pending:
                pe_.ev = ev
            eng.pending = []
        else:
            ev = PendingEv(eng)
            eng.pending.append(ev)
        for b in reads:
            b.r[eng.name] = ev
        for b in writes:
            b.w = ev
            b.r = {}
        return ins

    def dma(self, q, out, in_, reads=(), writes=(), **kw):
        owner = (list(writes) + list(reads))[0]
        if owner.dsem is None:
            owner.dsem = self.dsem_free.pop() if self.dsem_free else DSem(self)
            self.scope_dsems[-1].append(owner)
        st = owner.dsem
        self._deps(q, reads, writes)
        ins = q.h.dma_start(out=out, in_=in_, **kw)
        ins.then_inc(st.sem, 16)
        st.count += 16
        ev = (st.sem, st.count)
        for b in reads:
            b.r["dma_" + str(id(st))] = ev
        for b in writes:
            b.w = ev
            b.r = {}
        return ev

    def sb(self, name, shape, dtype):
        self.uid += 1
        return self.scope_es.enter_context(self.nc.sbuf_tensor(f"{name}_{self.uid}", list(shape), dtype))

    def ps(self, name, shape, dtype=F32):
        self.uid += 1
        return self.scope_es.enter_context(self.nc.psum_tensor(f"{name}_{self.uid}", list(shape), dtype))

    def open_scope(self):
        self.scope_stack.append(self.scope_es)
        self.scope_es = ExitStack()
        self.scope_dsems.append([])
        return self.scope_es

    def close_scope(self):
        self.barrier()
        self.scope_es.close()
        self.scope_es = self.scope_stack.pop()
        for b in self.scope_dsems.pop():
            self.dsem_free.append(b.dsem)
            b.dsem = None


class Ring:
    def __init__(self, fw, name, n, shape, dtype, psum=False):
        self.slots = []
        for i in range(n):
            t = fw.ps(f"{name}{i}", shape, dtype) if psum else fw.sb(f"{name}{i}", shape, dtype)
            self.slots.append((t, Buf(f"{name}{i}")))
        self.i = 0

    def next(self):
        s = self.slots[self.i % len(self.slots)]
        self.i += 1
        return s


class Prog:
    def __init__(self, cfg):
        self.cfg = cfg
        self.nc = nc = bass.Bass("TRN2", target_bir_lowering=False)
        self.es = ExitStack()
        self.fw = FW(nc, self.es)
        self.dram = {}

    def din(self, name, shape, dtype=F32):
        t = self.nc.dram_tensor(name, list(shape), dtype, kind="ExternalInput").ap()
        self.dram[name] = t
        return t

    def dout(self, name, shape, dtype=F32):
        t = self.nc.dram_tensor(name, list(shape), dtype, kind="ExternalOutput").ap()
        self.dram[name] = t
        return t

    def dscr(self, name, shape, dtype):
        kind = "ExternalOutput" if name in self.cfg.get("debug_out", ()) else "Internal"
        if name in self.cfg.get("ext_in", ()):
            kind = "ExternalInput"
        t = self.nc.dram_tensor(name, list(shape), dtype, kind=kind).ap()
        self.dram[name] = t
        return t


def mm(fw, out, lhsT, rhs, start, stop, reads, writes, inc=None):
    nc = fw.nc
    if inc is None:
        inc = stop
    return fw.op(fw.pe, lambda: nc.tensor.matmul(out, lhsT, rhs, start=start, stop=stop), reads, writes, inc=inc)


def load_consts(P):
    fw, nc, d = P.fw, P.nc, P.dram
    es = P.es
    c = {}
    c["ident_f"] = es.enter_context(nc.sbuf_tensor("ident_f", [128, 128], F32))
    c["ident_b"] = es.enter_context(nc.sbuf_tensor("ident_b", [128, 128], BF16))
    c["ones_b"] = es.enter_context(nc.sbuf_tensor("ones_b", [128, 128], BF16))
    c["ones_f"] = es.enter_context(nc.sbuf_tensor("ones_f", [128, 128], F32))
    c["B_ident_f"] = Buf("ident_f"); c["B_ident_b"] = Buf("ident_b")
    c["B_ones_b"] = Buf("ones_b"); c["B_ones_f"] = Buf("ones_f")
    fw.dma(fw.sp, c["ident_f"][:], d["c_ident"][:, :], writes=[c["B_ident_f"]])
    fw.op(fw.dve, lambda: nc.vector.tensor_copy(c["ident_b"][:], c["ident_f"][:]), [c["B_ident_f"]], [c["B_ident_b"]])
    fw.op(fw.dve, lambda: nc.vector.memset(c["ones_b"][:], 1.0), [], [c["B_ones_b"]])
    fw.op(fw.dve, lambda: nc.vector.memset(c["ones_f"][:], 1.0), [], [c["B_ones_f"]])
    P.c = c


def phase_A(P, L, x_src):
    fw, nc, d, c = P.fw, P.nc, P.dram, P.c
    TT = 512
    NT = P.cfg.get("ntiles_A", S // TT)
    fw.open_scope()
    sb, ps = fw.sb, fw.ps

    win = sb("win", [128, 8, N_IN], BF16); B_win = Buf("win")
    wuq = sb("wuq", [128, 6, 8, 96], BF16); B_wuq = Buf("wuq")
    wuqr = sb("wuqr", [128, 6, 8, 96], BF16); B_wuqr = Buf("wuqr")
    wkk = sb("wkk", [128, 2, 8, 64], BF16); B_wkk = Buf("wkk")
    wkv = sb("wkv", [128, 2, 512], BF16); B_wkv = Buf("wkv")
    wkr = sb("wkr", [128, 8, 96], BF16); B_wkr = Buf("wkr")
    wkrr = sb("wkrr", [128, 8, 96], BF16); B_wkrr = Buf("wkrr")
    gq = sb("gq", [128, 6], F32); B_gq = Buf("gq")
    gkv = sb("gkv", [128, 2], F32); B_gkv = Buf("gkv")
    bg = sb("bg", [128, 16], F32); B_bg = Buf("bg")
    fw.open_scope()
    wst = Ring(fw, "wst", 3, [128, 1160], F32)

    fw.dma(fw.sp, gq[:], d["q_norm_g"][L].rearrange("(k p) -> p k", p=128), writes=[B_gq], allow_slow_non_contiguous=True)
    fw.dma(fw.sp, gkv[:], d["kv_norm_g"][L].rearrange("(k p) -> p k", p=128), writes=[B_gkv], allow_slow_non_contiguous=True)
    fw.dma(fw.sp, bg[:], d["b_gate"][L].rearrange("(k p) -> p k", p=128), writes=[B_bg], allow_slow_non_contiguous=True)

    first = True
    for k in range(8):
        for qd in range(4):
            t, B = wst.next()
            fw.dma(fw.sp, t[:, :], d["w_in"][L, k * 128:(k + 1) * 128, qd * 1160:(qd + 1) * 1160], writes=[B])
            eng = fw.dve if (k * 4 + qd) % 2 == 0 else fw.pool
            fw.op(eng, lambda t=t, eng=eng: eng.h.tensor_copy(win[:, k, qd * 1160:(qd + 1) * 1160], t[:, :]), [B], [B_win])
    fw.op(fw.dve, lambda: nc.vector.memset(wkr[:], 0.0), [], [B_wkr])
    fw.op(fw.dve, lambda: nc.vector.memset(wkrr[:], 0.0), [], [B_wkrr])
    fw.op(fw.dve, lambda: nc.vector.tensor_copy(wkr[:, :, 64:96], win[:, :, C_KR:C_KR + 32]), [B_win], [B_wkr])
    fw.op(fw.dve, lambda: nc.vector.tensor_scalar(out=wkrr[:, :, 64:80], in0=win[:, :, C_KR + 16:C_KR + 32], scalar1=-1.0, scalar2=None, op0=ALU.mult), [B_win], [B_wkrr])
    fw.op(fw.dve, lambda: nc.vector.tensor_copy(wkrr[:, :, 80:96], win[:, :, C_KR:C_KR + 16]), [B_win], [B_wkrr])
    for k in range(6):
        t, B = wst.next()
        fw.dma(fw.sp, t[:, 0:768], d["w_uq"][L, k * 128:(k + 1) * 128, :], writes=[B])
        fw.op(fw.dve, lambda t=t, k=k: nc.vector.tensor_scalar(out=wuq[:, k, :, :], in0=t[:, 0:768].rearrange("p (h e) -> p h e", h=8), scalar1=gq[:, k:k + 1], scalar2=None, op0=ALU.mult), [B, B_gq], [B_wuq])
    fw.op(fw.pool, lambda: nc.gpsimd.tensor_copy(wuqr[:, :, :, 0:64], wuq[:, :, :, 0:64]), [B_wuq], [B_wuqr])
    fw.op(fw.dve, lambda: nc.vector.tensor_scalar(out=wuqr[:, :, :, 64:80], in0=wuq[:, :, :, 80:96], scalar1=-1.0, scalar2=None, op0=ALU.mult), [B_wuq], [B_wuqr])
    fw.op(fw.pool, lambda: nc.gpsimd.tensor_copy(wuqr[:, :, :, 80:96], wuq[:, :, :, 64:80]), [B_wuq], [B_wuqr])
    for k in range(2):
        t, B = wst.next()
        fw.dma(fw.sp, t[:, 0:1024], d["w_ukv"][L, k * 128:(k + 1) * 128, :], writes=[B])
        tv = t[:, 0:1024].rearrange("p (h e) -> p h e", h=8)
        fw.op(fw.dve, lambda tv=tv, k=k: nc.vector.tensor_scalar(out=wkk[:, k, :, :], in0=tv[:, :, 0:64], scalar1=gkv[:, k:k + 1], scalar2=None, op0=ALU.mult), [B, B_gkv], [B_wkk])
        fw.op(fw.dve, lambda tv=tv, k=k: nc.vector.tensor_scalar(out=wkv[:, k, :].rearrange("p (h e) -> p h e", h=8), in0=tv[:, :, 64:128], scalar1=gkv[:, k:k + 1], scalar2=None, op0=ALU.mult), [B, B_gkv], [B_wkv])

    fw.close_scope()
    xs_ring = Ring(fw, "xs", 2, [128, D], F32)
    xb_ring = Ring(fw, "xb", 2, [128, D], BF16)
    xT_ring = Ring(fw, "xT", 2, [128, 8, TT], BF16)
    out_ring = Ring(fw, "og", 12, [128, TT], BF16)
    cs_ring = Ring(fw, "cs", 2, [96, 2, TT], F32)
    csr_ring = Ring(fw, "csr", 1, [96, 2, TT], F32)
    lat_ring = Ring(fw, "lat", 2, [128, 8, TT], BF16)
    sq_ring = Ring(fw, "sq", 3, [128, TT], BF16)
    rq_ring = Ring(fw, "rq", 1, [128, TT], F32)
    rk_ring = Ring(fw, "rk", 1, [128, TT], F32)
    t1_ring = Ring(fw, "t1", 2, [96, TT], F32)
    t2_ring = Ring(fw, "t2", 2, [96, TT], F32)
    eps_q = sb("epsq", [128, 1], F32); B_eps = Buf("eps")
    eps_k = sb("epsk", [128, 1], F32)
    fw.op(fw.dve, lambda: nc.vector.memset(eps_q[:], RMS_EPS), [], [B_eps])
    fw.op(fw.dve, lambda: nc.vector.memset(eps_k[:], RMS_EPS), [], [B_eps])
    pmm = Ring(fw, "pmm", 4, [128, TT], F32, psum=True)
    ptr = Ring(fw, "ptr", 2, [128, 4, 128], BF16, psum=True)
    pss = Ring(fw, "pss", 2, [128, TT], F32, psum=True)

    evac_i = [0]

    def store(src_tile, Bsrc, dst, npart=128):
        fw.dma(fw.sp, dst, src_tile, reads=[Bsrc])

    def evac_copy(eng, out_ap, in_ap, reads, writes):
        if eng is fw.act:
            fw.op(eng, lambda: nc.scalar.copy(out_ap, in_ap), reads, writes)
        else:
            fw.op(eng, lambda: eng.h.tensor_copy(out_ap, in_ap), reads, writes)

    def prep(t):
        tok0 = t * TT
        xT, B_xT = xT_ring.next()
        for s4 in range(4):
            xs, B_xs = xs_ring.next()
            fw.dma(fw.sp, xs[:, :], x_src[tok0 + s4 * 128: tok0 + (s4 + 1) * 128, :], writes=[B_xs])
            xb, B_xb = xb_ring.next()
            fw.op(fw.dve, lambda xb=xb, xs=xs: nc.vector.tensor_copy(xb[:, :], xs[:, :]), [B_xs], [B_xb])
            for half in range(2):
                pt, B_pt = ptr.next()
                for j in range(4):
                    fc = half * 4 + j
                    fw.op(fw.pe, lambda pt=pt, j=j, fc=fc, xb=xb: nc.tensor.transpose(pt[:, j, :], xb[:, fc * 128:(fc + 1) * 128], c["ident_b"][:]),
                          [B_xb, c["B_ident_b"]], [B_pt], inc=(j == 3))
                fw.op(fw.dve, lambda pt=pt, half=half, s4=s4, xT=xT: nc.vector.tensor_copy(xT[:, half * 4:(half + 1) * 4, s4 * 128:(s4 + 1) * 128], pt[:, :, :]),
                      [B_pt], [B_xT])
        cs, B_cs = cs_ring.next()
        fw.dma(fw.sp, cs[64:96, 0, :], d["c_cos"][:, tok0:tok0 + TT], writes=[B_cs])
        fw.dma(fw.sp, cs[64:96, 1, :], d["c_sin"][:, tok0:tok0 + TT], writes=[B_cs])
        return xT, B_xT, cs, B_cs

    nxt_prep = prep(0) if NT > 0 else None
    for t in range(NT):
        tok0 = t * TT
        xT, B_xT, cs, B_cs = nxt_prep
        nxt_prep = None

        def proj_fm(c0, mw, wt=None, Bw=None, kk=8, rhs_fn=None, rhsB=None):
            pt, B_pt = pmm.next()
            for k in range(kk):
                lhsT = win[:, k, c0:c0 + mw] if wt is None else wt(k)
                rhs = xT[:, k, :] if rhs_fn is None else rhs_fn(k)
                mm(fw, pt[0:mw, :], lhsT, rhs, k == 0, k == kk - 1,
                   [B_win if Bw is None else Bw, B_xT if rhsB is None else rhsB], [B_pt])
            return pt, B_pt

        for which, c0, dst in (("q", C_NAQ, d[f"qT_na{L}"]), ("k", C_NAK, d[f"kT_na{L}"])):
            for m in range(4):
                pt, B_pt = proj_fm(c0 + m * 128, 128)
                og, B_og = out_ring.next()
                evac_copy(fw.act, og[:, :], pt[:, :], [B_pt], [B_og])
                store(og[:, :], B_og, dst[m * 128:(m + 1) * 128, tok0:tok0 + TT])
        for s4 in range(4):
            pt, B_pt = pmm.next()
            for k in range(8):
                mm(fw, pt[:, :], xT[:, k, s4 * 128:(s4 + 1) * 128], win[:, k, C_NAV:C_NAV + 512], k == 0, k == 7, [B_win, B_xT], [B_pt])
            og, B_og = out_ring.next()
            evac_copy(fw.dve, og[:, :], pt[:, :], [B_pt], [B_og])
            store(og[:, :], B_og, d[f"v_na{L}"][tok0 + s4 * 128: tok0 + (s4 + 1) * 128, :])
        for m in range(16):
            pt, B_pt = proj_fm(C_GNA + m * 128, 128)
            og, B_og = out_ring.next()
            fw.op(fw.act, lambda og=og, pt=pt, m=m: nc.scalar.activation(og[:, :], pt[:, :], AF.Sigmoid, bias=bg[:, m:m + 1], scale=1.0), [B_pt, B_bg], [B_og])
            store(og[:, :], B_og, d[f"sgT{L}"][m * 128:(m + 1) * 128, tok0:tok0 + TT])
        if t + 1 < NT:
            nxt_prep = prep(t + 1)
        lat, B_lat = lat_ring.next()
        rstd = {}
        for name, c0, nch, off, dim, rring in (("q", C_QL, 6, 0, QL, rq_ring), ("k", C_KVL, 2, 6, KVL, rk_ring)):
            pssq, B_pssq = pss.next()
            for m in range(nch):
                pt, B_pt = proj_fm(c0 + m * 128, 128)
                if "nodve" not in P.cfg.get("A_var", ""):
                    evac_copy(fw.dve, lat[:, off + m, :], pt[:, :], [B_pt], [B_lat])
                sq, B_sq = sq_ring.next()
                if "noact" in P.cfg.get("A_var", ""):
                    pass
                elif "nosquare" in P.cfg.get("A_var", ""):
                    fw.op(fw.act, lambda sq=sq, pt=pt: nc.scalar.activation(sq[:, :], pt[:, :], AF.Identity), [B_pt], [B_sq])
                else:
                    fw.op(fw.act, lambda sq=sq, m=m: nc.scalar.activation(sq[:, :], lat[:, off + m, :], AF.Square), [B_lat], [B_sq])
                if "noones" not in P.cfg.get("A_var", ""):
                    mm(fw, pssq[:, :], c["ones_b"][:, :], sq[:, :], m == 0, m == nch - 1, [c["B_ones_b"], B_sq], [B_pssq])
            r, B_r = rring.next()
            rstd[name] = (r, B_r)
            if "noones" in P.cfg.get("A_var", ""):
                continue
            var = P.cfg.get("A_var", "")
            if "nosqrt" in var:
                fw.op(fw.act, lambda r=r, pssq=pssq, dim=dim: nc.scalar.activation(r[:, :], pssq[:, :], AF.Identity, bias=eps_q[:, 0:1], scale=1.0 / dim), [B_pssq, B_eps], [B_r])
            else:
                fw.op(fw.act, lambda r=r, pssq=pssq, dim=dim: nc.scalar.activation(r[:, :], pssq[:, :], AF.Sqrt, bias=eps_q[:, 0:1], scale=1.0 / dim), [B_pssq, B_eps], [B_r])
            if "norecip" not in var:
                fw.op(fw.dve, lambda r=r: nc.vector.reciprocal(r[:, :], r[:, :]), [B_r], [B_r])
            rstd[name] = (r, B_r)
        rq, B_rq = rstd["q"]
        rk, B_rk = rstd["k"]
        csr, B_csr = csr_ring.next()
        for j in range(2):
            fw.op(fw.pool, lambda j=j, csr=csr, cs=cs, rq=rq: nc.gpsimd.tensor_tensor(out=csr[64:96, j, :], in0=cs[64:96, j, :], in1=rq[64:96, :], op=ALU.mult), [B_cs, B_rq], [B_csr])
        for h in range(8):
            pa, B_pa = proj_fm(0, 96, wt=lambda k, h=h: wuq[:, k, h, :], Bw=B_wuq, kk=6, rhs_fn=lambda k: lat[:, k, :], rhsB=B_lat)
            pb, B_pb = proj_fm(0, 96, wt=lambda k, h=h: wuqr[:, k, h, :], Bw=B_wuqr, kk=6, rhs_fn=lambda k: lat[:, k, :], rhsB=B_lat)
            og, B_og = out_ring.next()
            fw.op(fw.dve, lambda og=og, pa=pa: nc.vector.tensor_tensor(out=og[0:64, :], in0=pa[0:64, :], in1=rq[0:64, :], op=ALU.mult), [B_pa, B_rq], [B_og])
            t1, B_t1 = t1_ring.next()
            t2, B_t2 = t2_ring.next()
            fw.op(fw.dve, lambda t1=t1, pa=pa: nc.vector.tensor_tensor(out=t1[64:96, :], in0=pa[64:96, :], in1=csr[64:96, 0, :], op=ALU.mult), [B_pa, B_csr], [B_t1])
            fw.op(fw.dve, lambda t2=t2, pb=pb: nc.vector.tensor_tensor(out=t2[64:96, :], in0=pb[64:96, :], in1=csr[64:96, 1, :], op=ALU.mult), [B_pb, B_csr], [B_t2])
            fw.op(fw.pool, lambda og=og, t1=t1, t2=t2: nc.gpsimd.tensor_tensor(out=og[64:96, :], in0=t1[64:96, :], in1=t2[64:96, :], op=ALU.add), [B_t1, B_t2], [B_og])
            store(og[0:96, :], B_og, d[f"qT_mla{L}"][h, :, tok0:tok0 + TT])
        pa, B_pa = proj_fm(0, 96, wt=lambda k: wkr[:, k, :], Bw=B_wkr)
        pb, B_pb = proj_fm(0, 96, wt=lambda k: wkrr[:, k, :], Bw=B_wkrr)
        og, B_og = out_ring.next()
        t1, B_t1 = t1_ring.next()
        t2, B_t2 = t2_ring.next()
        fw.op(fw.dve, lambda: nc.vector.tensor_tensor(out=t1[64:96, :], in0=pa[64:96, :], in1=cs[64:96, 0, :], op=ALU.mult), [B_pa, B_cs], [B_t1])
        fw.op(fw.dve, lambda: nc.vector.tensor_tensor(out=t2[64:96, :], in0=pb[64:96, :], in1=cs[64:96, 1, :], op=ALU.mult), [B_pb, B_cs], [B_t2])
        fw.op(fw.pool, lambda: nc.gpsimd.tensor_tensor(out=og[64:96, :], in0=t1[64:96, :], in1=t2[64:96, :], op=ALU.add), [B_t1, B_t2], [B_og])
        store(og[64:96, :], B_og, d[f"krT{L}"][:, tok0:tok0 + TT])
        for h in range(8):
            pt, B_pt = proj_fm(0, 64, wt=lambda k, h=h: wkk[:, k, h, :], Bw=B_wkk, kk=2, rhs_fn=lambda k: lat[:, 6 + k, :], rhsB=B_lat)
            og, B_og = out_ring.next()
            fw.op(fw.dve, lambda og=og, pt=pt: nc.vector.tensor_tensor(out=og[0:64, :], in0=pt[0:64, :], in1=rk[0:64, :], op=ALU.mult), [B_pt, B_rk], [B_og])
            store(og[0:64, :], B_og, d[f"kT_mla{L}"][h, :, tok0:tok0 + TT])
        kvn = []
        for k in range(2):
            sqt, B_sqt = sq_ring.next()
            fw.op(fw.dve, lambda sqt=sqt, k=k: nc.vector.tensor_tensor(out=sqt[:, :], in0=lat[:, 6 + k, :], in1=rk[:, :], op=ALU.mult), [B_lat, B_rk], [B_sqt])
            kvn.append((sqt, B_sqt))
        for s4 in range(4):
            pt, B_pt = pmm.next()
            for k in range(2):
                mm(fw, pt[:, :], kvn[k][0][:, s4 * 128:(s4 + 1) * 128], wkv[:, k, :], k == 0, k == 1, [B_wkv, kvn[k][1]], [B_pt])
            og, B_og = out_ring.next()
            evac_copy(fw.act, og[:, :], pt[:, :], [B_pt], [B_og])
            store(og[:, :], B_og, d[f"v_mla{L}"][tok0 + s4 * 128: tok0 + (s4 + 1) * 128, :])
    fw.close_scope()


def host_consts():
    pos = np.arange(S, dtype=np.float32)
    inv_freq = (10000.0 ** (-np.arange(0, ROPE // 2, dtype=np.float32) * np.float32(2.0 / ROPE))).astype(np.float32)
    ang = (pos[None, :] * inv_freq[:, None]).astype(np.float32)
    cos = np.cos(ang).astype(np.float32)
    sin = np.sin(ang).astype(np.float32)
    dr_idx, dc_idx, validf, valids = na_host_index()
    return {
        "c_ident": np.eye(128, dtype=np.float32),
        "c_cos": np.ascontiguousarray(np.concatenate([cos, cos], 0)),
        "c_sin": np.ascontiguousarray(np.concatenate([sin, sin], 0)),
        "c_maskf": np.ascontiguousarray(np.where(validf, 0.0, NEG / NA_SCALE).astype(np.float32).reshape(128, U_F * 64)),
        "c_masks": np.ascontiguousarray(np.where(valids, 0.0, NEG / NA_SCALE).astype(np.float32).reshape(128, U_S * 64)),
    }


def host_na_table(na_rpb):
    dr_idx, dc_idx, validf, valids = na_host_index()
    g = na_rpb[:, :, dr_idx, dc_idx]
    g = np.where(validf[None, None], g, np.float32(0.0)).astype(np.float32)
    return np.ascontiguousarray(g.reshape(DEPTH, NA_H, 128, U_F * 64))


W_SHAPES = {
    "w_in": [DEPTH, D, N_IN], "b_gate": [DEPTH, 2 * D], "q_norm_g": [DEPTH, QL], "w_uq": [DEPTH, QL, 768],
    "kv_norm_g": [DEPTH, KVL], "w_ukv": [DEPTH, KVL, 1024], "w_na_o": [DEPTH, 512, D], "w_mla_o": [DEPTH, 512, D],
    "w_out": [DEPTH, D, D], "ln1_g": [DEPTH, D], "ln1_b": [DEPTH, D],
    "ffn_w1": [1, D, D_FF], "ffn_w3": [1, D, D_FF], "ffn_w2": [1, D_FF, D],
    "moe_w_router": [1, D, NE], "moe_b_router": [1, NE],
    "moe_w1": [1, NE, D, D_FFE], "moe_w3": [1, NE, D, D_FFE], "moe_w2": [1, NE, D_FFE, D],
    "w_ple_gate": [DEPTH, D, D], "w_ple": [DEPTH, PLE, D], "ln2_g": [DEPTH, D], "ln2_b": [DEPTH, D],
}
C_SHAPES = {"c_ident": [128, 128], "c_cos": [32, S], "c_sin": [32, S], "c_maskf": [128, U_F * 64], "c_masks": [128, U_S * 64],
            "c_natab": [DEPTH, NA_H, 128, U_F * 64]}


def declare_io(P):
    P.din("x", [S, D])
    P.din("p", [DEPTH, S, PLE])
    for k, shp in W_SHAPES.items():
        P.din(k, shp)
    for k, shp in C_SHAPES.items():
        P.din(k, shp)
    for L in range(DEPTH):
        P.dscr(f"qT_na{L}", [512, S], BF16)
        P.dscr(f"kT_na{L}", [512, S], BF16)
        P.dscr(f"v_na{L}", [S, 512], BF16)
        P.dscr(f"sgT{L}", [2048, S], BF16)
        P.dscr(f"qT_mla{L}", [8, 96, S], BF16)
        P.dscr(f"krT{L}", [32, S], BF16)
        P.dscr(f"kT_mla{L}", [8, 64, S], BF16)
        P.dscr(f"v_mla{L}", [S, 512], BF16)
        P.dscr(f"o_naT{L}", [512, S], BF16)
        P.dscr(f"o_mlaT{L}", [512, S], BF16)


def na_host_index():
    p = np.arange(128)
    krp, kc = p // 64, p % 64
    u = np.arange(U_F)
    qc = np.arange(64)
    dr = (krp[:, None, None] + 14 - u[None, :, None]) + 0 * qc[None, None, :]
    ws = np.clip(qc - 8, 0, 48)
    colvalid = (kc[:, None, None] >= ws[None, None, :]) & (kc[:, None, None] < ws[None, None, :] + 16)
    colvalid = colvalid & (dr == dr)
    dc = kc[:, None, None] - qc[None, None, :] + 15 + 0 * u[None, :, None]
    validf = (np.abs(dr) <= 7) & colvalid
    dr_idx = np.clip(dr + 7, 0, 14)
    dc_idx = np.clip(dc, 0, 30)
    valids = validf[:, 4:4 + U_S, :] & (dr[:, 4:4 + U_S, :] >= -4) & (dr[:, 4:4 + U_S, :] <= 3)
    return dr_idx, dc_idx, validf, valids


def phase_B(P, L, side_w=None):
    fw, nc, d, c = P.fw, P.nc, P.dram, P.c
    fw.open_scope()
    sb, ps = fw.sb, fw.ps
    NQB = 512

    pS = Ring(fw, "pS", 3, [128, 2, NQB], F32, psum=True)
    pacc = Ring(fw, "pacc", 2, [128, NQB], F32, psum=True)
    Pr = Ring(fw, "Pr", 4, [128, 2, NQB], BF16)
    pending_norm = [None]
    accs = Ring(fw, "accs", 2, [128, NQB], F32)
    rden = Ring(fw, "rden", 2, [128, NQB], F32)
    ost = Ring(fw, "ost", 3, [64, NQB], BF16)
    sqr = Ring(fw, "sqr", 2, [128, NQB], BF16)
    nmx = Ring(fw, "nmx", 2, [128, 34], F32)

    def norm_bound(QT, B_Q, KT, B_K, pb, Kd, scale):
        nm, B_nm = nmx.next()
        for which, (T, B_T) in enumerate(((QT, B_Q), (KT, B_K))):
            for blk in range(S // NQB):
                sq, B_sq = sqr.next()
                fw.op(fw.pool, lambda sq=sq, T=T, blk=blk: nc.gpsimd.tensor_tensor(out=sq[pb:pb + Kd, :], in0=T[pb:pb + Kd, blk * NQB:(blk + 1) * NQB], in1=T[pb:pb + Kd, blk * NQB:(blk + 1) * NQB], op=ALU.mult), [B_T], [B_sq])
                pn, B_pn = pS.next()
                mm(fw, pn[:, 0, :], c["ones_b"][pb:pb + Kd, :], sq[pb:pb + Kd, :], True, True, [c["B_ones_b"], B_sq], [B_pn])
                col = which * 16 + blk
                fw.op(fw.dve, lambda pn=pn, nm=nm, col=col: nc.vector.tensor_reduce(out=nm[:, col:col + 1], in_=pn[:, 0, :], axis=AX.X, op=ALU.max), [B_pn], [B_nm])
        fw.op(fw.dve, lambda nm=nm: nc.vector.tensor_reduce(out=nm[:, 32:33], in_=nm[:, 0:16], axis=AX.X, op=ALU.max), [B_nm], [B_nm])
        fw.op(fw.dve, lambda nm=nm: nc.vector.tensor_reduce(out=nm[:, 33:34], in_=nm[:, 16:32], axis=AX.X, op=ALU.max), [B_nm], [B_nm])
        fw.op(fw.dve, lambda nm=nm: nc.vector.tensor_tensor(out=nm[:, 32:33], in0=nm[:, 32:33], in1=nm[:, 33:34], op=ALU.mult), [B_nm], [B_nm])
        fw.op(fw.act, lambda nm=nm: nc.scalar.activation(nm[:, 33:34], nm[:, 32:33], AF.Sqrt, scale=scale * scale), [B_nm], [B_nm])
        fw.op(fw.dve, lambda nm=nm: nc.vector.tensor_scalar(out=nm[:, 33:34], in0=nm[:, 33:34], scalar1=-1.0, scalar2=None, op0=ALU.mult), [B_nm], [B_nm])
        return nm[:, 33:34], B_nm

    def attn_group(QT, B_Q, pb, Kd, q0, nq, items, scale, negc, B_negc, dst):
        pairs = [items[i:i + 2] for i in range(0, len(items), 2)]
        acc, B_acc = pacc.next()
        sbufs = {}

        def emit_S(pi):
            St, B_S = pS.next()
            for j, (kT, B_k, v, B_v, tab, B_tab) in enumerate(pairs[pi]):
                mm(fw, St[:, j, 0:nq], kT, QT[pb:pb + Kd, q0:q0 + nq], True, tab is None, [B_k, B_Q], [B_S])
                if tab is not None:
                    mm(fw, St[:, j, 0:nq], c["ident_b"][:, :], tab, False, True, [c["B_ident_b"], B_tab], [B_S])
            sbufs[pi] = (St, B_S)

        def emit_PV(pi):
            St, B_S = sbufs.pop(pi)
            n = len(pairs[pi])
            Pt, B_P = Pr.next()
            fw.op(fw.act, lambda: nc.scalar.activation(Pt[:, 0:n, 0:nq], St[:, 0:n, 0:nq], AF.Exp, bias=negc, scale=scale), [B_S, B_negc], [B_P])
            for j, (kT, B_k, v, B_v, tab, B_tab) in enumerate(pairs[pi]):
                first = (pi == 0 and j == 0)
                last = (pi == len(pairs) - 1 and j == n - 1)
                mm(fw, acc[0:65, 0:nq], v, Pt[:, j, 0:nq], first, last, [B_v, B_P], [B_acc])

        LA = 3
        for pi in range(min(LA, len(pairs))):
            emit_S(pi)
        for pi in range(len(pairs)):
            emit_PV(pi)
            if pi + LA < len(pairs):
                emit_S(pi + LA)

        def norm():
            a_s, B_as = accs.next()
            fw.op(fw.dve, lambda: nc.vector.tensor_copy(a_s[0:65, 0:nq], acc[0:65, 0:nq]), [B_acc], [B_as])
            rd, B_rd = rden.next()
            fw.op(fw.dve, lambda: nc.vector.reciprocal(rd[64:65, 0:nq], a_s[64:65, 0:nq]), [B_as], [B_rd])
            bc, B_bc = pS.next()
            mm(fw, bc[0:64, 0, 0:nq], c["ones_f"][64:65, 0:64], rd[64:65, 0:nq], True, True, [c["B_ones_f"], B_rd], [B_bc])
            o, B_o = ost.next()
            fw.op(fw.dve, lambda: nc.vector.tensor_tensor(out=o[0:64, 0:nq], in0=a_s[0:64, 0:nq], in1=bc[0:64, 0, 0:nq], op=ALU.mult), [B_as, B_bc], [B_o])
            fw.dma(fw.sp, dst, o[0:64, 0:nq], reads=[B_o])

        if pending_norm[0] is not None:
            pending_norm[0]()
        pending_norm[0] = norm

    def flush_norm():
        if pending_norm[0] is not None:
            pending_norm[0]()
            pending_norm[0] = None

    if P.cfg.get("do_na", True):
        fw.open_scope()
        mkf = sb("mkf", [128, U_F * 64], F32); B_mkf = Buf("mkf")
        mks = sb("mks", [128, U_S * 64], F32); B_mks = Buf("mks")
        fw.dma(fw.sp, mkf[:, :], d["c_maskf"][:, :], writes=[B_mkf])
        fw.dma(fw.sp, mks[:, :], d["c_masks"][:, :], writes=[B_mks])
        gst = Ring(fw, "gst", 2, [128, U_F * 64], F32)
        ttf = Ring(fw, "ttf", 2, [128, U_F * 64], BF16)
        tts = Ring(fw, "tts", 2, [128, U_S * 64], BF16)
        QTr = Ring(fw, "naQ", 2, [128, S], BF16)
        KTr = Ring(fw, "naK", 2, [128, S], BF16)
        Vr = Ring(fw, "naV", 2, [128, 64, 2, 65], BF16)
        heads = P.cfg.get("na_heads", list(range(NA_H)))
        pair_bufs = {}

        def na_load_pair(hp):
            if hp in pair_bufs:
                return
            QT, B_Q = QTr.next(); KT, B_K = KTr.next(); V, B_V = Vr.next()
            fw.dma(fw.sp, QT[:, :], d[f"qT_na{L}"][hp * 128:(hp + 1) * 128, :], writes=[B_Q])
            fw.dma(fw.sp, KT[:, :], d[f"kT_na{L}"][hp * 128:(hp + 1) * 128, :], writes=[B_K])
            fw.op(fw.pool, lambda V=V: nc.gpsimd.memset(V[:, :, :, :], 1.0), [], [B_V])
            for g in range(8):
                for hh in range(2):
                    fw.dma(fw.sp, V[:, g * 8:(g + 1) * 8, hh, 0:64],
                           d[f"v_na{L}"][g * 1024:(g + 1) * 1024, (hp * 2 + hh) * 64:(hp * 2 + hh + 1) * 64].rearrange("(kt p) e -> p kt e", p=128),
                           writes=[B_V])
            pair_bufs.clear() if len(pair_bufs) >= 2 else None
            pair_bufs[hp] = (QT, B_Q, KT, B_K, V, B_V)

        def na_tables(h):
            g_, B_g = gst.next()
            fw.dma(fw.sp, g_[:, :], d["c_natab"][L, h], writes=[B_g])
            tf, B_tf = ttf.next(); ts_, B_ts = tts.next()
            fw.op(fw.dve, lambda: nc.vector.scalar_tensor_tensor(out=tf[:, :], in0=g_[:, :], scalar=1.0 / NA_SCALE, in1=mkf[:, :], op0=ALU.mult, op1=ALU.add), [B_g, B_mkf], [B_tf])
            fw.op(fw.dve, lambda: nc.vector.scalar_tensor_tensor(out=ts_[:, :], in0=g_[:, 4 * 64:(4 + U_S) * 64], scalar=1.0 / NA_SCALE, in1=mks[:, :], op0=ALU.mult, op1=ALU.add), [B_g, B_mks], [B_ts])
            return tf, B_tf, ts_, B_ts

        def na_bound(h):
            QT, B_Q, KT, B_K, V, B_V = pair_bufs[h // 2]
            return norm_bound(QT, B_Q, KT, B_K, (h % 2) * 64, 64, NA_SCALE)

        prepped = {}
        na_load_pair(heads[0] // 2)
        prepped[heads[0]] = (na_tables(heads[0]), na_bound(heads[0]))
        for hi, h in enumerate(heads):
            hp, pb = h // 2, (h % 2) * 64
            hn = heads[hi + 1] if hi + 1 < len(heads) else None
            QT, B_Q, KT, B_K, V, B_V = pair_bufs[hp]
            (tf, B_tf, ts_, B_ts), (negc, B_negc) = prepped.pop(h)
            if hn is not None and (h % 2 == 0 or hn // 2 == hp):
                if hn // 2 != hp and len(pair_bufs) < 2:
                    na_load_pair(hn // 2)
            dst_all = d[f"o_naT{L}"][h * 64:(h + 1) * 64, :]

            def item(kt, tab):
                return (KT[pb:pb + 64, kt * 128:(kt + 1) * 128], B_K, V[:, kt, h % 2, :], B_V, tab, None)

            groups = []
            for r in range(4):
                its = [item(i, tf[:, (14 - 2 * i + r) * 64:(14 - 2 * i + r) * 64 + 64]) for i in range(4)]
                groups.append((r * 64, 64, its, B_tf))
            its = [item(i - 2, ts_[:, (2 * (7 - i) + 4) * 64:(2 * (7 - i) + 4) * 64 + 256]) for i in range(2, 8)]
            groups.append((256, 256, its, B_ts))
            for j in range(1, 15):
                its = [item(4 * j - 2 + i, ts_[:, 2 * (7 - i) * 64:2 * (7 - i) * 64 + 512]) for i in range(8)]
                groups.append((512 * j, 512, its, B_ts))
            its = [item(58 + i, ts_[:, 2 * (7 - i) * 64:2 * (7 - i) * 64 + 320]) for i in range(6)]
            groups.append((7680, 320, its, B_ts))
            for r in range(125, 128):
                its = [item(60 + i, tf[:, (14 - 2 * i + r - 120) * 64:(14 - 2 * i + r - 120) * 64 + 64]) for i in range(4)]
                groups.append((r * 64, 64, its, B_tf))
            gsel = P.cfg.get("na_groups", None)
            sel = [g for gi, g in enumerate(groups) if gsel is None or gi in gsel]
            for gi, (q0, nq, its, B_tab) in enumerate(sel):
                its = [(a_, b_, v, bv, tab, B_tab) for (a_, b_, v, bv, tab, _) in its]
                attn_group(QT, B_Q, pb, 64, q0, nq, its, NA_SCALE, negc, B_negc, dst_all[:, q0:q0 + nq])
                if hn is not None and gi == (len(sel) * 2) // 3:
                    if hn // 2 not in pair_bufs:
                        if len(pair_bufs) >= 2:
                            pair_bufs.pop(min(pair_bufs))
                        na_load_pair(hn // 2)
                    prepped[hn] = (na_tables(hn), na_bound(hn))
            if hn is not None and hn not in prepped:
                if hn // 2 not in pair_bufs:
                    if len(pair_bufs) >= 2:
                        pair_bufs.pop(min(pair_bufs))
                    na_load_pair(hn // 2)
                prepped[hn] = (na_tables(hn), na_bound(hn))
        flush_norm()
        fw.close_scope()

    if P.cfg.get("do_mla", True):
        fw.open_scope()
        QTr = Ring(fw, "mQ", 2, [96, S], BF16)
        KTr = Ring(fw, "mK", 2, [96, S], BF16)
        Vr = Ring(fw, "mV", 2, [128, 64, 65], BF16)
        heads = P.cfg.get("mla_heads", list(range(MLA_H)))
        qblocks = P.cfg.get("mla_qblocks", list(range(S // NQB)))
        wgen, w_per_group = None, 0
        if side_w is not None:
            wc = WConv(P, [fw.pool])
            wc.alloc(nbig=1)
            wgen = wc.units(side_w)
            w_per_group = 7

        def step_w(n):
            nonlocal wgen
            for _ in range(n):
                if wgen is None:
                    return
                try:
                    next(wgen)
                except StopIteration:
                    wgen = None

        def mla_load(h):
            QT, B_Q = QTr.next(); KT, B_K = KTr.next(); V, B_V = Vr.next()
            fw.dma(fw.sp, QT[:, :], d[f"qT_mla{L}"][h], writes=[B_Q])
            fw.dma(fw.sp, KT[0:64, :], d[f"kT_mla{L}"][h], writes=[B_K])
            fw.dma(fw.sp, KT[64:96, :], d[f"krT{L}"][:, :], writes=[B_K])
            fw.op(fw.pool, lambda V=V: nc.gpsimd.memset(V[:, :, :], 1.0), [], [B_V])
            for g in range(8):
                fw.dma(fw.sp, V[:, g * 8:(g + 1) * 8, 0:64],
                       d[f"v_mla{L}"][g * 1024:(g + 1) * 1024, h * 64:(h + 1) * 64].rearrange("(kt p) e -> p kt e", p=128),
                       writes=[B_V])
            return QT, B_Q, KT, B_K, V, B_V

        cur = mla_load(heads[0])
        cur_neg = norm_bound(cur[0], cur[1], cur[2], cur[3], 0, 96, MLA_SCALE)
        for hi, h in enumerate(heads):
            QT, B_Q, KT, B_K, V, B_V = cur
            negc, B_negc = cur_neg
            nxt = mla_load(heads[hi + 1]) if hi + 1 < len(heads) else None
            nxt_neg = None
            for qi, qb in enumerate(qblocks):
                its = [(KT[0:96, kt * 128:(kt + 1) * 128], B_K, V[:, kt, :], B_V, None, None) for kt in range(64)]
                attn_group(QT, B_Q, 0, 96, qb * NQB, NQB, its, MLA_SCALE, negc, B_negc, d[f"o_mlaT{L}"][h * 64:(h + 1) * 64, qb * NQB:(qb + 1) * NQB])
                step_w(w_per_group)
                if nxt is not None and qi == max(0, len(qblocks) - 4):
                    nxt_neg = norm_bound(nxt[0], nxt[1], nxt[2], nxt[3], 0, 96, MLA_SCALE)
            if nxt is not None and nxt_neg is None:
                nxt_neg = norm_bound(nxt[0], nxt[1], nxt[2], nxt[3], 0, 96, MLA_SCALE)
            cur, cur_neg = nxt, nxt_neg
        flush_norm()
        step_w(1 << 30)
        fw.close_scope()
    fw.close_scope()


def ffn_dims(L):
    if L % 2 == 0:
        return 1, D_FF, "ffn_w1", "ffn_w3", "ffn_w2"
    return NE, D_FFE, "moe_w1", "moe_w3", "moe_w2"


class WConv:
    def __init__(self, P, engs):
        self.P, self.engs, self.cnt = P, engs, 0
        self.pending_store = None

    def alloc(self, nbig=1):
        fw = self.P.fw
        self.stg = Ring(fw, "wstg", 3, [128, 2048], F32)
        self.cvt = Ring(fw, "wcvt", 3, [128, 2048], BF16)
        self.HC = 7
        self.big = Ring(fw, "w13b", nbig, [128, self.HC, 2, 8, 128], BF16)
        self.st2 = Ring(fw, "wst2", 2, [128, self.HC * 128], F32)

    def convert(self, out_ap, in_ap, reads, writes):
        fw, nc = self.P.fw, self.P.nc
        e = self.engs[self.cnt % len(self.engs)]
        self.cnt += 1
        if e is fw.act:
            fw.op(e, lambda: nc.scalar.copy(out_ap, in_ap), reads, writes)
        else:
            fw.op(e, lambda: e.h.tensor_copy(out_ap, in_ap), reads, writes)

    def flush_store(self):
        if self.pending_store is not None:
            dst, src, B_c = self.pending_store
            self.P.fw.dma(self.P.fw.sp, dst, src, reads=[B_c])
            self.pending_store = None

    def conv_rows(self, src, dst, ncols):
        fw = self.P.fw
        for c0 in range(0, ncols, 2048):
            w = min(2048, ncols - c0)
            s_, B_s = self.stg.next()
            fw.dma(fw.sp, s_[:, 0:w], src[:, c0:c0 + w], writes=[B_s])
            c_, B_c = self.cvt.next()
            self.convert(c_[:, 0:w], s_[:, 0:w], [B_s], [B_c])
            self.flush_store()
            self.pending_store = (dst[:, c0:c0 + w], c_[:, 0:w], B_c)

    def units(self, layers):
        P, fw, d = self.P, self.P.fw, self.P.dram
        HC = self.HC
        for L in layers:
            for name, K_ in (("w_na_o", 512), ("w_mla_o", 512), ("w_out", D), ("w_ple_gate", D), ("w_ple", PLE)):
                for k in range(K_ // 128):
                    self.conv_rows(d[name][L, k * 128:(k + 1) * 128, :], d[f"{name}_s{L}"][:, k, :], D)
                    yield
            ne, dff, n1, n3, n2 = ffn_dims(L)
            j = L // 2
            nch = dff // 128
            w1 = d[n1][j] if ne > 1 else d[n1]
            w3 = d[n3][j] if ne > 1 else d[n3]
            w2 = d[n2][j] if ne > 1 else d[n2]
            for e in range(ne):
                for c_ in range(nch):
                    self.conv_rows(w2[e, c_ * 128:(c_ + 1) * 128, :], d[f"w2s{L}"][e, c_], D)
                    yield
            self.flush_store()
            for e in range(ne):
                for c0 in range(0, nch, HC):
                    gn = min(HC, nch - c0)
                    bt, B_b = self.big.next()
                    for i, wsrc in enumerate((w1, w3)):
                        for k in range(8):
                            s_, B_s = self.st2.next()
                            fw.dma(fw.sp, s_[:, 0:gn * 128], wsrc[e, k * 128:(k + 1) * 128, c0 * 128:(c0 + gn) * 128], writes=[B_s])
                            self.convert(bt[:, 0:gn, i, k, :], s_[:, 0:gn * 128].rearrange("p (c j) -> p c j", j=128), [B_s], [B_b])
                            yield
                    for cc in range(gn):
                        fw.dma(fw.sp, d[f"w13s{L}"][e, c0 + cc], bt[:, cc, :, :, :].rearrange("p i k j -> p (i k j)"), reads=[B_b])
                    yield


def phase_W(P, layers):
    fw = P.fw
    fw.open_scope()
    wc = WConv(P, [fw.dve, fw.pool, fw.act])
    wc.alloc(nbig=2)
    for _ in wc.units(layers):
        pass
    fw.close_scope()


def declare_scratch_C(P):
    for L in range(DEPTH):
        P.dscr(f"w_na_o_s{L}", [128, 4, D], BF16)
        P.dscr(f"w_mla_o_s{L}", [128, 4, D], BF16)
        P.dscr(f"w_out_s{L}", [128, 8, D], BF16)
        P.dscr(f"w_ple_gate_s{L}", [128, 8, D], BF16)
        P.dscr(f"w_ple_s{L}", [128, 2, D], BF16)
        ne, dff, _, _, _ = ffn_dims(L)
        P.dscr(f"w13s{L}", [ne, dff // 128, 128, 2 * 8 * 128], BF16)
        P.dscr(f"w2s{L}", [ne, dff // 128, 128, D], BF16)
    P.dscr("x_mid", [S, D], F32)


def phase_C(P, L, x_src, x_dst):
    fw, nc, d, c = P.fw, P.nc, P.dram, P.c
    moe = (L % 2 == 1)
    ne, dff, _, _, _ = ffn_dims(L)
    nch = dff // 128
    TT = 1024
    NS = TT // 128
    NT = P.cfg.get("ntiles_C", S // TT)
    G = 4
    fw.open_scope()
    sb = fw.sb

    lnp = sb("lnp", [128, 4, D], F32); B_lnp = Buf("lnp")
    for i, nm in enumerate(("ln1_g", "ln1_b", "ln2_g", "ln2_b")):
        fw.dma(fw.sp, lnp[:, i, :], d[nm][L:L + 1, :].broadcast_to([128, D]), writes=[B_lnp])
    cst = sb("cst", [128, 4], F32); B_cst = Buf("cst")
    fw.op(fw.dve, lambda: nc.vector.memset(cst[:, 0:1], LN_EPS), [], [B_cst])
    fw.op(fw.dve, lambda: nc.vector.memset(cst[:, 1:2], -0.5), [], [B_cst])
    if moe:
        wr = sb("wr", [128, 8, NE], F32); B_wr = Buf("wr")
        fw.dma(fw.sp, wr[:, :, :], d["moe_w_router"][L // 2].rearrange("(k p) e -> p k e", p=128), writes=[B_wr])
        br = sb("br", [128, NE], F32); B_br = Buf("br")
        fw.dma(fw.sp, br[:, :], d["moe_b_router"][L // 2:L // 2 + 1, :].broadcast_to([128, NE]), writes=[B_br])
    gates = sb("gates", [128, NS, NE], F32); B_gates = Buf("gates")

    xacc = sb("xacc", [128, NS, D], F32)
    B_xa = [Buf(f"xacc{s}") for s in range(NS)]
    x1T = sb("x1T", [128, 8, TT], BF16); B_x1T = Buf("x1T")
    oT = Ring(fw, "oT", 2, [128, 4, 512], BF16)
    sgr = Ring(fw, "sgr", 4, [128, 512], BF16)
    mrg = Ring(fw, "mrg", 1, [128, 8, 512], BF16)
    tmp = Ring(fw, "tmpf", 3, [128, 512], F32)
    wring = Ring(fw, "wring", 4, [128, 4, D], BF16)
    x1Tf = Ring(fw, "x1Tf", 2, [128, 8, 128], F32)
    pfr = Ring(fw, "pfr", 2, [128, PLE], F32)
    pT = sb("pT", [128, 2, TT], BF16); B_pT = Buf("pT")
    w13r = Ring(fw, "w13r", 3, [128, 2, 8, 128], BF16)
    w2r = Ring(fw, "w2r", 2, [128, G, D], BF16)
    hTr = Ring(fw, "hTr", 2, [128, G, TT], BF16)
    silr = Ring(fw, "silr", 2, [128, 512], F32)
    stat = Ring(fw, "stat", 2, [128, 16], F32)
    rlg = Ring(fw, "rlg", 2, [128, 32], F32)
    pmm = Ring(fw, "pmmC", 6, [128, 512], F32, psum=True)
    ptr = Ring(fw, "ptrC", 2, [128, 4, 128], F32, psum=True)

    def load_w(name, k0, nk):
        wt, B_w = wring.next()
        fw.dma(fw.sp, wt[:, 0:nk, :], d[f"{name}_s{L}"][:, k0:k0 + nk, :], writes=[B_w])
        return wt, B_w

    def layer_norm(s, gi):
        st, B_st = stat.next()
        xs = xacc[:, s, :]
        for hh in range(2):
            fw.op(fw.dve, lambda hh=hh: nc.vector.bn_stats(st[:, hh * 6:(hh + 1) * 6], xacc[:, s, hh * 512:(hh + 1) * 512]), [B_xa[s]], [B_st])
        fw.op(fw.dve, lambda: nc.vector.bn_aggr(st[:, 12:14], st[:, 0:12]), [B_st], [B_st])
        fw.op(fw.pool, lambda: nc.gpsimd.tensor_scalar(out=st[:, 14:15], in0=st[:, 13:14], scalar1=cst[:, 0:1], scalar2=None, op0=ALU.add), [B_st, B_cst], [B_st])
        fw.op(fw.pool, lambda: nc.gpsimd.tensor_tensor(out=st[:, 14:15], in0=st[:, 14:15], in1=cst[:, 1:2], op=ALU.pow), [B_st, B_cst], [B_st])
        fw.op(fw.pool, lambda: nc.gpsimd.tensor_tensor(out=st[:, 15:16], in0=st[:, 12:13], in1=st[:, 14:15], op=ALU.mult), [B_st], [B_st])
        fw.op(fw.pool, lambda: nc.gpsimd.tensor_scalar(out=st[:, 15:16], in0=st[:, 15:16], scalar1=-1.0, scalar2=None, op0=ALU.mult), [B_st], [B_st])
        fw.op(fw.act, lambda: nc.scalar.activation(xs, xs, AF.Identity, bias=st[:, 15:16], scale=st[:, 14:15]), [B_st], [B_xa[s]])
        fw.op(fw.dve, lambda: nc.vector.tensor_tensor(out=xs, in0=xs, in1=lnp[:, gi, :], op=ALU.mult), [B_lnp], [B_xa[s]])
        fw.op(fw.pool, lambda: nc.gpsimd.tensor_tensor(out=xs, in0=xs, in1=lnp[:, gi + 1, :], op=ALU.add), [B_lnp], [B_xa[s]])

    wbr = {}
    pre = {}
    preloaded = False

    def c1a(t, th):
        if t not in wbr:
            wbr.clear()
            wbr[t] = (load_w("w_na_o", 0, 4), load_w("w_mla_o", 0, 4))
        (wna, B_wna), (wml, B_wml) = wbr[t]
        t0 = t * TT + th * 512
        ona, B_ona = oT.next()
        oml, B_oml = oT.next()
        fw.dma(fw.sp, ona[:, :, :], d[f"o_naT{L}"][:, t0:t0 + 512].rearrange("(k p) t -> p k t", p=128), writes=[B_ona])
        fw.dma(fw.sp, oml[:, :, :], d[f"o_mlaT{L}"][:, t0:t0 + 512].rearrange("(k p) t -> p k t", p=128), writes=[B_oml])
        mg, B_mg = mrg.next()
        for fc in range(8):
            sgn, B_sgn = sgr.next()
            sgm, B_sgm = sgr.next()
            fw.dma(fw.sp, sgn[:, :], d[f"sgT{L}"][fc * 128:(fc + 1) * 128, t0:t0 + 512], writes=[B_sgn])
            fw.dma(fw.sp, sgm[:, :], d[f"sgT{L}"][D + fc * 128:D + (fc + 1) * 128, t0:t0 + 512], writes=[B_sgm])
            pn, B_pn = pmm.next()
            for k in range(4):
                mm(fw, pn[:, :], wna[:, k, fc * 128:(fc + 1) * 128], ona[:, k, :], k == 0, k == 3, [B_wna, B_ona], [B_pn])
            pm, B_pm = pmm.next()
            for k in range(4):
                mm(fw, pm[:, :], wml[:, k, fc * 128:(fc + 1) * 128], oml[:, k, :], k == 0, k == 3, [B_wml, B_oml], [B_pm])
            t1, B_t1 = tmp.next()
            t2, B_t2 = tmp.next()
            fw.op(fw.dve, lambda: nc.vector.tensor_tensor(out=t1[:, :], in0=pn[:, :], in1=sgn[:, :], op=ALU.mult), [B_pn, B_sgn], [B_t1])
            fw.op(fw.dve, lambda: nc.vector.tensor_tensor(out=t2[:, :], in0=pm[:, :], in1=sgm[:, :], op=ALU.mult), [B_pm, B_sgm], [B_t2])
            fw.op(fw.pool, lambda: nc.gpsimd.tensor_tensor(out=mg[:, fc, :], in0=t1[:, :], in1=t2[:, :], op=ALU.add), [B_t1, B_t2], [B_mg])
        return mg, B_mg

    for t in range(NT):
        tok0 = t * TT
        if not preloaded:
            for s in range(NS):
                fw.dma(fw.sp, xacc[:, s, :], x_src[tok0 + s * 128: tok0 + (s + 1) * 128, :], writes=[B_xa[s]])
        preloaded = False
        for th in range(TT // 512):
            if (t, th) in pre:
                mg, B_mg = pre.pop((t, th))
            else:
                mg, B_mg = c1a(t, th)
            if th == 0:
                wo = [load_w("w_out", 0, 4), load_w("w_out", 4, 4)]
            for s4 in range(4):
                s = th * 4 + s4
                for half in range(2):
                    pp, B_pp = pmm.next()
                    for k in range(8):
                        wt, B_w = wo[k // 4]
                        mm(fw, pp[:, :], mg[:, k, s4 * 128:(s4 + 1) * 128], wt[:, k % 4, half * 512:(half + 1) * 512], k == 0, k == 7, [B_mg, B_w], [B_pp])
                    xh = xacc[:, s, half * 512:(half + 1) * 512]
                    fw.op(fw.dve, lambda: nc.vector.scalar_tensor_tensor(out=xh, in0=xh, scalar=ALPHA, in1=pp[:, :], op0=ALU.mult, op1=ALU.add), [B_pp], [B_xa[s]])
                layer_norm(s, 0)
        for s in range(NS):
            xf, B_xf = x1Tf.next()
            for hf in range(2):
                pt_, B_pt = ptr.next()
                for j in range(4):
                    fc = hf * 4 + j
                    fw.op(fw.pe, lambda fc=fc, j=j: nc.tensor.transpose(pt_[:, j, :], xacc[:, s, fc * 128:(fc + 1) * 128], c["ident_f"][:]),
                          [B_xa[s], c["B_ident_f"]], [B_pt], inc=(j == 3))
                fw.op(fw.dve, lambda: nc.vector.tensor_copy(x1T[:, hf * 4:(hf + 1) * 4, s * 128:(s + 1) * 128], pt_[:, :, :]), [B_pt], [B_x1T])
                if moe:
                    fw.op(fw.dve, lambda: nc.vector.tensor_copy(xf[:, hf * 4:(hf + 1) * 4, :], pt_[:, :, :]), [B_pt], [B_xf])
            if moe:
                pr_, B_pr = pmm.next()
                for k in range(8):
                    mm(fw, pr_[:, 0:NE], xf[:, k, :], wr[:, k, :], k == 0, k == 7, [B_xf, B_wr], [B_pr])
                lg, B_lg = rlg.next()
                fw.op(fw.dve, lambda: nc.vector.tensor_tensor(out=lg[:, 0:8], in0=pr_[:, 0:NE], in1=br[:, :], op=ALU.add), [B_pr, B_br], [B_lg])
                fw.op(fw.dve, lambda: nc.vector.max(out=lg[:, 8:16], in_=lg[:, 0:8]), [B_lg], [B_lg])
                fw.op(fw.dve, lambda: nc.vector.tensor_tensor(out=lg[:, 16:17], in0=lg[:, 8:9], in1=lg[:, 9:10], op=ALU.add), [B_lg], [B_lg])
                fw.op(fw.dve, lambda: nc.vector.tensor_scalar(out=lg[:, 24:32], in0=lg[:, 0:8], scalar1=2.0, scalar2=lg[:, 16:17], op0=ALU.mult, op1=ALU.subtract), [B_lg], [B_lg])
                fw.op(fw.act, lambda: nc.scalar.activation(lg[:, 24:32], lg[:, 24:32], AF.Sigmoid), [B_lg], [B_lg])
                fw.op(fw.dve, lambda: nc.vector.tensor_scalar(out=lg[:, 16:24], in0=lg[:, 0:8], scalar1=lg[:, 9:10], scalar2=None, op0=ALU.is_ge), [B_lg], [B_lg])
                fw.op(fw.dve, lambda: nc.vector.tensor_tensor(out=gates[:, s, :], in0=lg[:, 16:24], in1=lg[:, 24:32], op=ALU.mult), [B_lg], [B_gates])
            fw.op(fw.act, lambda: nc.scalar.mul(xacc[:, s, :], xacc[:, s, :], ALPHA), [], [B_xa[s]])
        for s in range(NS):
            pf, B_pf = pfr.next()
            fw.dma(fw.sp, pf[:, :], d["p"][L, tok0 + s * 128: tok0 + (s + 1) * 128, :], writes=[B_pf])
            pt_, B_pt = ptr.next()
            for k in range(2):
                fw.op(fw.pe, lambda k=k: nc.tensor.transpose(pt_[:, k, :], pf[:, k * 128:(k + 1) * 128], c["ident_f"][:]),
                      [B_pf, c["B_ident_f"]], [B_pt], inc=(k == 1))
            fw.op(fw.dve, lambda: nc.vector.tensor_copy(pT[:, :, s * 128:(s + 1) * 128], pt_[:, 0:2, :]), [B_pt], [B_pT])
        wpg = [load_w("w_ple_gate", 0, 4), load_w("w_ple_gate", 4, 4)]
        wp, B_wp = load_w("w_ple", 0, 2)
        for s in range(NS):
            for half in range(2):
                pg, B_pg = pmm.next()
                for k in range(8):
                    wt, B_w = wpg[k // 4]
                    mm(fw, pg[:, :], x1T[:, k, s * 128:(s + 1) * 128], wt[:, k % 4, half * 512:(half + 1) * 512], k == 0, k == 7, [B_x1T, B_w], [B_pg])
                pq, B_pq = pmm.next()
                for k in range(2):
                    mm(fw, pq[:, :], pT[:, k, s * 128:(s + 1) * 128], wp[:, k, half * 512:(half + 1) * 512], k == 0, k == 1, [B_pT, B_wp], [B_pq])
                sg_, B_sg = silr.next()
                fw.op(fw.act, lambda: nc.scalar.activation(sg_[:, :], pg[:, :], AF.Sigmoid), [B_pg], [B_sg])
                t1, B_t1 = tmp.next()
                fw.op(fw.dve, lambda: nc.vector.tensor_tensor(out=t1[:, :], in0=sg_[:, :], in1=pq[:, :], op=ALU.mult), [B_sg, B_pq], [B_t1])
                xh = xacc[:, s, half * 512:(half + 1) * 512]
                fw.op(fw.pool, lambda: nc.gpsimd.tensor_tensor(out=xh, in0=xh, in1=t1[:, :], op=ALU.add), [B_t1], [B_xa[s]])
        exps = P.cfg.get("experts", list(range(ne)))
        for e in exps:
            for c0 in range(0, nch, G):
                gn = min(G, nch - c0)
                w2t, B_w2 = w2r.next()
                fw.dma(fw.sp, w2t[:, 0:gn, :], d[f"w2s{L}"][e, c0:c0 + gn].rearrange("c p n -> p c n"), writes=[B_w2])
                hT, B_hT = hTr.next()
                for ci in range(gn):
                    w13, B_w13 = w13r.next()
                    fw.dma(fw.sp, w13[:, :, :, :], d[f"w13s{L}"][e, c0 + ci].rearrange("p (i k j) -> p i k j", i=2, k=8), writes=[B_w13])
                    for th in range(TT // 512):
                        pa, B_pa = pmm.next()
                        for k in range(8):
                            mm(fw, pa[:, :], w13[:, 0, k, :], x1T[:, k, th * 512:(th + 1) * 512], k == 0, k == 7, [B_w13, B_x1T], [B_pa])
                        pb_, B_pb_ = pmm.next()
                        for k in range(8):
                            mm(fw, pb_[:, :], w13[:, 1, k, :], x1T[:, k, th * 512:(th + 1) * 512], k == 0, k == 7, [B_w13, B_x1T], [B_pb_])
                        sl, B_sl = silr.next()
                        fw.op(fw.act, lambda: nc.scalar.activation(sl[:, :], pa[:, :], AF.Silu), [B_pa], [B_sl])
                        fw.op(fw.dve, lambda: nc.vector.tensor_tensor(out=hT[:, ci, th * 512:(th + 1) * 512], in0=sl[:, :], in1=pb_[:, :], op=ALU.mult), [B_sl, B_pb_], [B_hT])
                is_last = (e == exps[-1] and c0 + G >= nch)
                if is_last and t + 1 < NT:
                    pre[(t + 1, 0)] = c1a(t + 1, 0)
                for s in range(NS):
                    for half in range(2):
                        pd, B_pd = pmm.next()
                        for ci in range(gn):
                            mm(fw, pd[:, :], hT[:, ci, s * 128:(s + 1) * 128], w2t[:, ci, half * 512:(half + 1) * 512], ci == 0, ci == gn - 1, [B_hT, B_w2], [B_pd])
                        xh = xacc[:, s, half * 512:(half + 1) * 512]
                        if moe:
                            fw.op(fw.dve, lambda: nc.vector.scalar_tensor_tensor(out=xh, in0=pd[:, :], scalar=gates[:, s, e:e + 1], in1=xh, op0=ALU.mult, op1=ALU.add), [B_pd, B_gates], [B_xa[s]])
                        else:
                            fw.op(fw.dve, lambda: nc.vector.tensor_tensor(out=xh, in0=xh, in1=pd[:, :], op=ALU.add), [B_pd], [B_xa[s]])
                    if is_last:
                        layer_norm(s, 2)
                        fw.dma(fw.sp, x_dst[tok0 + s * 128: tok0 + (s + 1) * 128, :], xacc[:, s, :], reads=[B_xa[s]])
                        if t + 1 < NT:
                            fw.dma(fw.sp, xacc[:, s, :], x_src[tok0 + TT + s * 128: tok0 + TT + (s + 1) * 128, :], writes=[B_xa[s]])
                            preloaded = True
    fw.close_scope()


def build_program(cfg=None):
    cfg = dict(cfg or {})
    P = Prog(cfg)
    declare_io(P)
    declare_scratch_C(P)
    out = P.dout("out", [S, D])
    load_consts(P)
    layers = cfg.get("layers", list(range(DEPTH)))
    overlap = cfg.get("overlap_w", True) and len(layers) > 1 and cfg.get("do_B", True) and cfg.get("do_mla", True)
    phase_W(P, layers[:1] if overlap else layers)
    for L in layers:
        x_src = P.dram["x"] if L == layers[0] else P.dram["x_mid"]
        x_dst = out if L == layers[-1] else P.dram["x_mid"]
        if cfg.get("do_A", True):
            phase_A(P, L, x_src)
        if cfg.get("do_B", True):
            phase_B(P, L, side_w=(layers[1:] if (overlap and L == layers[0]) else None))
        if cfg.get("do_C", True):
            phase_C(P, L, x_src, x_dst)
    P.fw.barrier()
    P.es.close()
    return P


def make_in_maps(inputs, cores):
    consts = host_consts()
    consts["c_natab"] = host_na_table(np.asarray(inputs["na_rpb"], dtype=np.float32))
    shared = {k: np.ascontiguousarray(np.asarray(inputs[k], dtype=np.float32)) for k in W_SHAPES}
    shared.update(consts)
    maps = []
    for b in cores:
        m = dict(shared)
        m["x"] = np.ascontiguousarray(np.asarray(inputs["x"][b], dtype=np.float32))
        m["p"] = np.ascontiguousarray(np.asarray(inputs["p"][:, b], dtype=np.float32))
        maps.append(m)
    return maps


def kernel(**inputs):
    P = build_program()
    maps = make_in_maps(inputs, list(range(NCORES)))
    res = run_bass_kernel_spmd(P.nc, maps, core_ids=list(range(NCORES)))
    return np.stack([np.asarray(r["out"], dtype=np.float32) for r in res.results], axis=0)
```

```python
import math
from contextlib import ExitStack

import numpy as np
import concourse.bass as bass
import concourse.mybir as mybir
from concourse.bass_utils import run_bass_kernel_spmd

F32 = mybir.dt.float32
BF16 = mybir.dt.bfloat16
AF = mybir.ActivationFunctionType
ALU = mybir.AluOpType
AX = mybir.AxisListType

D = 1024
S = 8192
DEPTH = 2
NCORES = 8
PLE = 256
GRID_W = 64
NA_H = 8
NA_D = 64
MLA_H = 8
NOPE = 64
ROPE = 32
MLA_V = 64
QL = 768
KVL = 256
N_IN = 4640
D_FF = 2816
NE = 8
D_FFE = 3584
ALPHA = (2 * DEPTH) ** 0.25
LN_EPS = 1e-5
RMS_EPS = 1e-6
NA_SCALE = NA_D ** -0.5
MLA_SCALE = (NOPE + ROPE) ** -0.5
NEG = -1e30

C_NAQ, C_NAK, C_NAV = 0, 512, 1024
C_QL = 1536
C_KVL = 2304
C_KR = 2560
C_GNA = 2592
C_GMLA = 3616

U_F = 29
U_S = 22

SAME_ENG_WAIT = True


class Eng:
    def __init__(self, fw, name, h):
        self.fw, self.name, self.h = fw, name, h
        self.sem = fw.new_sem("e_" + name)
        self.count = 0
        self.waited = {}
        self.pending = []

    def wait(self, ev):
        if ev is None:
            return
        sem, val = ev
        if sem is self.sem and (self.name == "pe" or not SAME_ENG_WAIT):
            return
        key = id(sem)
        if self.waited.get(key, 0) >= val:
            return
        self.h.wait_ge(sem, val)
        self.waited[key] = val


class DSem:
    def __init__(self, fw):
        self.sem = fw.new_sem("d%d" % fw.nsem)
        self.count = 0
        fw.all_dsems.append(self)


class Buf:
    def __init__(self, name):
        self.name = name
        self.w = None
        self.r = {}
        self.dsem = None


class PendingEv:
    def __init__(self, eng):
        self.ev = None
        self.eng = eng


class FW:
    def __init__(self, nc, es):
        self.nc, self.es = nc, es
        self.nsem = 0
        self.uid = 0
        self.scope_es = None
        self.scope_stack = []
        self.pe = Eng(self, "pe", nc.tensor)
        self.act = Eng(self, "act", nc.scalar)
        self.dve = Eng(self, "dve", nc.vector)
        self.pool = Eng(self, "pool", nc.gpsimd)
        self.sp = Eng(self, "sp", nc.sync)
        self.all_dsems = []
        self.dsem_free = []
        self.scope_dsems = [[]]

    def new_sem(self, name):
        self.nsem += 1
        return self.es.enter_context(self.nc.semaphore(name))

    @staticmethod
    def _res(eng, ev):
        if isinstance(ev, PendingEv):
            if ev.ev is None:
                assert ev.eng is eng, "dependency on a non-incremented instruction"
                return None
            return ev.ev
        return ev

    def _deps(self, eng, reads, writes):
        for b in reads:
            eng.wait(self._res(eng, b.w))
        for b in writes:
            eng.wait(self._res(eng, b.w))
            for ev in b.r.values():
                eng.wait(self._res(eng, ev))

    def barrier(self):
        engs = [self.pe, self.act, self.dve, self.pool, self.sp]
        evs = [(e.sem, e.count) for e in engs if e.count > 0]
        evs += [(s.sem, s.count) for s in self.all_dsems if s.count > 0]
        for e in engs:
            for ev in evs:
                if ev[0] is e.sem:
                    continue
                e.wait(ev)

    def op(self, eng, fn, reads=(), writes=(), inc=True):
        self._deps(eng, reads, writes)
        ins = fn()
        if inc:
            ins.then_inc(eng.sem, 1)
            eng.count += 1
            ev = (eng.sem, eng.count)
            for pe_ in eng.pending:
                pe_.ev = ev
            eng.pending = []
        else:
            ev = PendingEv(eng)
            eng.pending.append(ev)
        for b in reads:
            b.r[eng.name] = ev
        for b in writes:
            b.w = ev
            b.r = {}
        return ins

    def dma(self, q, out, in_, reads=(), writes=(), **kw):
        owner = (list(writes) + list(reads))[0]
        if owner.dsem is None:
            owner.dsem = self.dsem_free.pop() if self.dsem_free else DSem(self)
            self.scope_dsems[-1].append(owner)
        st = owner.dsem
        self._deps(q, reads, writes)
        ins = q.h.dma_start(out=out, in_=in_, **kw)
        ins.then_inc(st.sem, 16)
        st.count += 16
        ev = (st.sem, st.count)
        for b in reads:
            b.r["dma_" + str(id(st))] = ev
        for b in writes:
            b.w = ev
            b.r = {}
        return ev

    def sb(self, name, shape, dtype):
        self.uid += 1
        return self.scope_es.enter_context(self.nc.sbuf_tensor(f"{name}_{self.uid}", list(shape), dtype))

    def ps(self, name, shape, dtype=F32):
        self.uid += 1
        return self.scope_es.enter_context(self.nc.psum_tensor(f"{name}_{self.uid}", list(shape), dtype))

    def open_scope(self):
        self.scope_stack.append(self.scope_es)
        self.scope_es = ExitStack()
        self.scope_dsems.append([])
        return self.scope_es

    def close_scope(self):
        self.barrier()
        self.scope_es.close()
        self.scope_es = self.scope_stack.pop()
        for b in self.scope_dsems.pop():
            self.dsem_free.append(b.dsem)
            b.dsem = None


class Ring:
    def __init__(self, fw, name, n, shape, dtype, psum=False):
        self.slots = []
        for i in range(n):
            t = fw.ps(f"{name}{i}", shape, dtype) if psum else fw.sb(f"{name}{i}", shape, dtype)
            self.slots.append((t, Buf(f"{name}{i}")))
        self.i = 0

    def next(self):
        s = self.slots[self.i % len(self.slots)]
        self.i += 1
        return s


class Prog:
    def __init__(self, cfg):
        self.cfg = cfg
        self.nc = nc = bass.Bass("TRN2", target_bir_lowering=False)
        self.es = ExitStack()
        self.fw = FW(nc, self.es)
        self.dram = {}

    def din(self, name, shape, dtype=F32):
        t = self.nc.dram_tensor(name, list(shape), dtype, kind="ExternalInput").ap()
        self.dram[name] = t
        return t

    def dout(self, name, shape, dtype=F32):
        t = self.nc.dram_tensor(name, list(shape), dtype, kind="ExternalOutput").ap()
        self.dram[name] = t
        return t

    def dscr(self, name, shape, dtype):
        kind = "ExternalOutput" if name in self.cfg.get("debug_out", ()) else "Internal"
        if name in self.cfg.get("ext_in", ()):
            kind = "ExternalInput"
        t = self.nc.dram_tensor(name, list(shape), dtype, kind=kind).ap()
        self.dram[name] = t
        return t


def mm(fw, out, lhsT, rhs, start, stop, reads, writes, inc=None):
    nc = fw.nc
    if inc is None:
        inc = stop
    return fw.op(fw.pe, lambda: nc.tensor.matmul(out, lhsT, rhs, start=start, stop=stop), reads, writes, inc=inc)


def load_consts(P):
    fw, nc, d = P.fw, P.nc, P.dram
    es = P.es
    c = {}
    c["ident_f"] = es.enter_context(nc.sbuf_tensor("ident_f", [128, 128], F32))
    c["ident_b"] = es.enter_context(nc.sbuf_tensor("ident_b", [128, 128], BF16))
    c["ones_b"] = es.enter_context(nc.sbuf_tensor("ones_b", [128, 128], BF16))
    c["ones_f"] = es.enter_context(nc.sbuf_tensor("ones_f", [128, 128], F32))
    c["B_ident_f"] = Buf("ident_f"); c["B_ident_b"] = Buf("ident_b")
    c["B_ones_b"] = Buf("ones_b"); c["B_ones_f"] = Buf("ones_f")
    fw.dma(fw.sp, c["ident_f"][:], d["c_ident"][:, :], writes=[c["B_ident_f"]])
    fw.op(fw.dve, lambda: nc.vector.tensor_copy(c["ident_b"][:], c["ident_f"][:]), [c["B_ident_f"]], [c["B_ident_b"]])
    fw.op(fw.dve, lambda: nc.vector.memset(c["ones_b"][:], 1.0), [], [c["B_ones_b"]])
    fw.op(fw.dve, lambda: nc.vector.memset(c["ones_f"][:], 1.0), [], [c["B_ones_f"]])
    P.c = c


def phase_A(P, L, x_src):
    fw, nc, d, c = P.fw, P.nc, P.dram, P.c
    TT = 512
    NT = P.cfg.get("ntiles_A", S // TT)
    fw.open_scope()
    sb, ps = fw.sb, fw.ps

    win = sb("win", [128, 8, N_IN], BF16); B_win = Buf("win")
    wuq = sb("wuq", [128, 6, 8, 96], BF16); B_wuq = Buf("wuq")
    wuqr = sb("wuqr", [128, 6, 8, 96], BF16); B_wuqr = Buf("wuqr")
    wkk = sb("wkk", [128, 2, 8, 64], BF16); B_wkk = Buf("wkk")
    wkv = sb("wkv", [128, 2, 512], BF16); B_wkv = Buf("wkv")
    wkr = sb("wkr", [128, 8, 96], BF16); B_wkr = Buf("wkr")
    wkrr = sb("wkrr", [128, 8, 96], BF16); B_wkrr = Buf("wkrr")
    gq = sb("gq", [128, 6], F32); B_gq = Buf("gq")
    gkv = sb("gkv", [128, 2], F32); B_gkv = Buf("gkv")
    bg = sb("bg", [128, 16], F32); B_bg = Buf("bg")
    fw.open_scope()
    wst = Ring(fw, "wst", 3, [128, 1160], F32)

    fw.dma(fw.sp, gq[:], d["q_norm_g"][L].rearrange("(k p) -> p k", p=128), writes=[B_gq], allow_slow_non_contiguous=True)
    fw.dma(fw.sp, gkv[:], d["kv_norm_g"][L].rearrange("(k p) -> p k", p=128), writes=[B_gkv], allow_slow_non_contiguous=True)
    fw.dma(fw.sp, bg[:], d["b_gate"][L].rearrange("(k p) -> p k", p=128), writes=[B_bg], allow_slow_non_contiguous=True)

    first = True
    for k in range(8):
        for qd in range(4):
            t, B = wst.next()
            fw.dma(fw.sp, t[:, :], d["w_in"][L, k * 128:(k + 1) * 128, qd * 1160:(qd + 1) * 1160], writes=[B])
            eng = fw.dve if (k * 4 + qd) % 2 == 0 else fw.pool
            fw.op(eng, lambda t=t, eng=eng: eng.h.tensor_copy(win[:, k, qd * 1160:(qd + 1) * 1160], t[:, :]), [B], [B_win])
    fw.op(fw.dve, lambda: nc.vector.memset(wkr[:], 0.0), [], [B_wkr])
    fw.op(fw.dve, lambda: nc.vector.memset(wkrr[:], 0.0), [], [B_wkrr])
    fw.op(fw.dve, lambda: nc.vector.tensor_copy(wkr[:, :, 64:96], win[:, :, C_KR:C_KR + 32]), [B_win], [B_wkr])
    fw.op(fw.dve, lambda: nc.vector.tensor_scalar(out=wkrr[:, :, 64:80], in0=win[:, :, C_KR + 16:C_KR + 32], scalar1=-1.0, scalar2=None, op0=ALU.mult), [B_win], [B_wkrr])
    fw.op(fw.dve, lambda: nc.vector.tensor_copy(wkrr[:, :, 80:96], win[:, :, C_KR:C_KR + 16]), [B_win], [B_wkrr])
    for k in range(6):
        t, B = wst.next()
        fw.dma(fw.sp, t[:, 0:768], d["w_uq"][L, k * 128:(k + 1) * 128, :], writes=[B])
        fw.op(fw.dve, lambda t=t, k=k: nc.vector.tensor_scalar(out=wuq[:, k, :, :], in0=t[:, 0:768].rearrange("p (h e) -> p h e", h=8), scalar1=gq[:, k:k + 1], scalar2=None, op0=ALU.mult), [B, B_gq], [B_wuq])
    fw.op(fw.pool, lambda: nc.gpsimd.tensor_copy(wuqr[:, :, :, 0:64], wuq[:, :, :, 0:64]), [B_wuq], [B_wuqr])
    fw.op(fw.dve, lambda: nc.vector.tensor_scalar(out=wuqr[:, :, :, 64:80], in0=wuq[:, :, :, 80:96], scalar1=-1.0, scalar2=None, op0=ALU.mult), [B_wuq], [B_wuqr])
    fw.op(fw.pool, lambda: nc.gpsimd.tensor_copy(wuqr[:, :, :, 80:96], wuq[:, :, :, 64:80]), [B_wuq], [B_wuqr])
    for k in range(2):
        t, B = wst.next()
        fw.dma(fw.sp, t[:, 0:1024], d["w_ukv"][L, k * 128:(k + 1) * 128, :], writes=[B])
        tv = t[:, 0:1024].rearrange("p (h e) -> p h e", h=8)
        fw.op(fw.dve, lambda tv=tv, k=k: nc.vector.tensor_scalar(out=wkk[:, k, :, :], in0=tv[:, :, 0:64], scalar1=gkv[:, k:k + 1], scalar2=None, op0=ALU.mult), [B, B_gkv], [B_wkk])
        fw.op(fw.dve, lambda tv=tv, k=k: nc.vector.tensor_scalar(out=wkv[:, k, :].rearrange("p (h e) -> p h e", h=8), in0=tv[:, :, 64:128], scalar1=gkv[:, k:k + 1], scalar2=None, op0=ALU.mult), [B, B_gkv], [B_wkv])

    fw.close_scope()
    xs_ring = Ring(fw, "xs", 2, [128, D], F32)
    xb_ring = Ring(fw, "xb", 2, [128, D], BF16)
    xT_ring = Ring(fw, "xT", 2, [128, 8, TT], BF16)
    out_ring = Ring(fw, "og", 12, [128, TT], BF16)
    cs_ring = Ring(fw, "cs", 2, [96, 2, TT], F32)
    csr_ring = Ring(fw, "csr", 1, [96, 2, TT], F32)
    lat_ring = Ring(fw, "lat", 2, [128, 8, TT], BF16)
    sq_ring = Ring(fw, "sq", 3, [128, TT], BF16)
    rq_ring = Ring(fw, "rq", 1, [128, TT], F32)
    rk_ring = Ring(fw, "rk", 1, [128, TT], F32)
    t1_ring = Ring(fw, "t1", 2, [96, TT], F32)
    t2_ring = Ring(fw, "t2", 2, [96, TT], F32)
    eps_q = sb("epsq", [128, 1], F32); B_eps = Buf("eps")
    eps_k = sb("epsk", [128, 1], F32)
    fw.op(fw.dve, lambda: nc.vector.memset(eps_q[:], RMS_EPS), [], [B_eps])
    fw.op(fw.dve, lambda: nc.vector.memset(eps_k[:], RMS_EPS), [], [B_eps])
    pmm = Ring(fw, "pmm", 5, [128, TT], F32, psum=True)
    ptr = Ring(fw, "ptr", 2, [128, 4, 128], BF16, psum=True)
    pss = Ring(fw, "pss", 1, [128, TT], F32, psum=True)

    evac_i = [0]

    def store(src_tile, Bsrc, dst, npart=128):
        fw.dma(fw.sp, dst, src_tile, reads=[Bsrc])

    def evac_copy(eng, out_ap, in_ap, reads, writes):
        if eng is fw.act:
            fw.op(eng, lambda: nc.scalar.copy(out_ap, in_ap), reads, writes)
        else:
            fw.op(eng, lambda: eng.h.tensor_copy(out_ap, in_ap), reads, writes)

    def prep(t):
        tok0 = t * TT
        xT, B_xT = xT_ring.next()
        for s4 in range(4):
            xs, B_xs = xs_ring.next()
            fw.dma(fw.sp, xs[:, :], x_src[tok0 + s4 * 128: tok0 + (s4 + 1) * 128, :], writes=[B_xs])
            xb, B_xb = xb_ring.next()
            fw.op(fw.dve, lambda xb=xb, xs=xs: nc.vector.tensor_copy(xb[:, :], xs[:, :]), [B_xs], [B_xb])
            for half in range(2):
                pt, B_pt = ptr.next()
                for j in range(4):
                    fc = half * 4 + j
                    fw.op(fw.pe, lambda pt=pt, j=j, fc=fc, xb=xb: nc.tensor.transpose(pt[:, j, :], xb[:, fc * 128:(fc + 1) * 128], c["ident_b"][:]),
                          [B_xb, c["B_ident_b"]], [B_pt], inc=(j == 3))
                fw.op(fw.dve, lambda pt=pt, half=half, s4=s4, xT=xT: nc.vector.tensor_copy(xT[:, half * 4:(half + 1) * 4, s4 * 128:(s4 + 1) * 128], pt[:, :, :]),
                      [B_pt], [B_xT])
        cs, B_cs = cs_ring.next()
        fw.dma(fw.sp, cs[64:96, 0, :], d["c_cos"][:, tok0:tok0 + TT], writes=[B_cs])
        fw.dma(fw.sp, cs[64:96, 1, :], d["c_sin"][:, tok0:tok0 + TT], writes=[B_cs])
        return xT, B_xT, cs, B_cs

    nxt_prep = prep(0) if NT > 0 else None
    for t in range(NT):
        tok0 = t * TT
        xT, B_xT, cs, B_cs = nxt_prep
        nxt_prep = None

        def proj_fm(c0, mw, wt=None, Bw=None, kk=8, rhs_fn=None, rhsB=None):
            pt, B_pt = pmm.next()
            for k in range(kk):
                lhsT = win[:, k, c0:c0 + mw] if wt is None else wt(k)
                rhs = xT[:, k, :] if rhs_fn is None else rhs_fn(k)
                mm(fw, pt[0:mw, :], lhsT, rhs, k == 0, k == kk - 1,
                   [B_win if Bw is None else Bw, B_xT if rhsB is None else rhsB], [B_pt])
            return pt, B_pt

        for which, c0, dst in (("q", C_NAQ, d[f"qT_na{L}"]), ("k", C_NAK, d[f"kT_na{L}"])):
            for m in range(4):
                pt, B_pt = proj_fm(c0 + m * 128, 128)
                og, B_og = out_ring.next()
                evac_copy(fw.act, og[:, :], pt[:, :], [B_pt], [B_og])
                store(og[:, :], B_og, dst[m * 128:(m + 1) * 128, tok0:tok0 + TT])
        for s4 in range(4):
            pt, B_pt = pmm.next()
            for k in range(8):
                mm(fw, pt[:, :], xT[:, k, s4 * 128:(s4 + 1) * 128], win[:, k, C_NAV:C_NAV + 512], k == 0, k == 7, [B_win, B_xT], [B_pt])
            og, B_og = out_ring.next()
            evac_copy(fw.dve, og[:, :], pt[:, :], [B_pt], [B_og])
            store(og[:, :], B_og, d[f"v_na{L}"][tok0 + s4 * 128: tok0 + (s4 + 1) * 128, :])
        for m in range(16):
            pt, B_pt = proj_fm(C_GNA + m * 128, 128)
            og, B_og = out_ring.next()
            fw.op(fw.act, lambda og=og, pt=pt, m=m: nc.scalar.activation(og[:, :], pt[:, :], AF.Sigmoid, bias=bg[:, m:m + 1], scale=1.0), [B_pt, B_bg], [B_og])
            store(og[:, :], B_og, d[f"sgT{L}"][m * 128:(m + 1) * 128, tok0:tok0 + TT])
        if t + 1 < NT:
            nxt_prep = prep(t + 1)
        lat, B_lat = lat_ring.next()
        rstd = {}
        for name, c0, nch, off, dim, rring in (("q", C_QL, 6, 0, QL, rq_ring), ("k", C_KVL, 2, 6, KVL, rk_ring)):
            pssq, B_pssq = pss.next()
            for m in range(nch):
                pt, B_pt = proj_fm(c0 + m * 128, 128)
                if "nodve" not in P.cfg.get("A_var", ""):
                    evac_copy(fw.dve, lat[:, off + m, :], pt[:, :], [B_pt], [B_lat])
                sq, B_sq = sq_ring.next()
                if "noact" in P.cfg.get("A_var", ""):
                    pass
                elif "nosquare" in P.cfg.get("A_var", ""):
                    fw.op(fw.act, lambda sq=sq, pt=pt: nc.scalar.activation(sq[:, :], pt[:, :], AF.Identity), [B_pt], [B_sq])
                else:
                    fw.op(fw.act, lambda sq=sq, m=m: nc.scalar.activation(sq[:, :], lat[:, off + m, :], AF.Square), [B_lat], [B_sq])
                if "noones" not in P.cfg.get("A_var", ""):
                    mm(fw, pssq[:, :], c["ones_b"][:, :], sq[:, :], m == 0, m == nch - 1, [c["B_ones_b"], B_sq], [B_pssq])
            r, B_r = rring.next()
            rstd[name] = (r, B_r)
            if "noones" in P.cfg.get("A_var", ""):
                continue
            var = P.cfg.get("A_var", "")
            if "nosqrt" in var:
                fw.op(fw.act, lambda r=r, pssq=pssq, dim=dim: nc.scalar.activation(r[:, :], pssq[:, :], AF.Identity, bias=eps_q[:, 0:1], scale=1.0 / dim), [B_pssq, B_eps], [B_r])
            else:
                fw.op(fw.act, lambda r=r, pssq=pssq, dim=dim: nc.scalar.activation(r[:, :], pssq[:, :], AF.Sqrt, bias=eps_q[:, 0:1], scale=1.0 / dim), [B_pssq, B_eps], [B_r])
            if "norecip" not in var:
                fw.op(fw.dve, lambda r=r: nc.vector.reciprocal(r[:, :], r[:, :]), [B_r], [B_r])
            rstd[name] = (r, B_r)
        rq, B_rq = rstd["q"]
        rk, B_rk = rstd["k"]
        csr, B_csr = csr_ring.next()
        for j in range(2):
            fw.op(fw.pool, lambda j=j, csr=csr, cs=cs, rq=rq: nc.gpsimd.tensor_tensor(out=csr[64:96, j, :], in0=cs[64:96, j, :], in1=rq[64:96, :], op=ALU.mult), [B_cs, B_rq], [B_csr])
        for h in range(8):
            pa, B_pa = proj_fm(0, 96, wt=lambda k, h=h: wuq[:, k, h, :], Bw=B_wuq, kk=6, rhs_fn=lambda k: lat[:, k, :], rhsB=B_lat)
            pb, B_pb = proj_fm(0, 96, wt=lambda k, h=h: wuqr[:, k, h, :], Bw=B_wuqr, kk=6, rhs_fn=lambda k: lat[:, k, :], rhsB=B_lat)
            og, B_og = out_ring.next()
            fw.op(fw.dve, lambda og=og, pa=pa: nc.vector.tensor_tensor(out=og[0:64, :], in0=pa[0:64, :], in1=rq[0:64, :], op=ALU.mult), [B_pa, B_rq], [B_og])
            t1, B_t1 = t1_ring.next()
            t2, B_t2 = t2_ring.next()
            fw.op(fw.dve, lambda t1=t1, pa=pa: nc.vector.tensor_tensor(out=t1[64:96, :], in0=pa[64:96, :], in1=csr[64:96, 0, :], op=ALU.mult), [B_pa, B_csr], [B_t1])
            fw.op(fw.dve, lambda t2=t2, pb=pb: nc.vector.tensor_tensor(out=t2[64:96, :], in0=pb[64:96, :], in1=csr[64:96, 1, :], op=ALU.mult), [B_pb, B_csr], [B_t2])
            fw.op(fw.pool, lambda og=og, t1=t1, t2=t2: nc.gpsimd.tensor_tensor(out=og[64:96, :], in0=t1[64:96, :], in1=t2[64:96, :], op=ALU.add), [B_t1, B_t2], [B_og])
            store(og[0:96, :], B_og, d[f"qT_mla{L}"][h, :, tok0:tok0 + TT])
        pa, B_pa = proj_fm(0, 96, wt=lambda k: wkr[:, k, :], Bw=B_wkr)
        pb, B_pb = proj_fm(0, 96, wt=lambda k: wkrr[:, k, :], Bw=B_wkrr)
        og, B_og = out_ring.next()
        t1, B_t1 = t1_ring.next()
        t2, B_t2 = t2_ring.next()
        fw.op(fw.dve, lambda: nc.vector.tensor_tensor(out=t1[64:96, :], in0=pa[64:96, :], in1=cs[64:96, 0, :], op=ALU.mult), [B_pa, B_cs], [B_t1])
        fw.op(fw.dve, lambda: nc.vector.tensor_tensor(out=t2[64:96, :], in0=pb[64:96, :], in1=cs[64:96, 1, :], op=ALU.mult), [B_pb, B_cs], [B_t2])
        fw.op(fw.pool, lambda: nc.gpsimd.tensor_tensor(out=og[64:96, :], in0=t1[64:96, :], in1=t2[64:96, :], op=ALU.add), [B_t1, B_t2], [B_og])
        store(og[64:96, :], B_og, d[f"krT{L}"][:, tok0:tok0 + TT])
        for h in range(8):
            pt, B_pt = proj_fm(0, 64, wt=lambda k, h=h: wkk[:, k, h, :], Bw=B_wkk, kk=2, rhs_fn=lambda k: lat[:, 6 + k, :], rhsB=B_lat)
            og, B_og = out_ring.next()
            fw.op(fw.dve, lambda og=og, pt=pt: nc.vector.tensor_tensor(out=og[0:64, :], in0=pt[0:64, :], in1=rk[0:64, :], op=ALU.mult), [B_pt, B_rk], [B_og])
            store(og[0:64, :], B_og, d[f"kT_mla{L}"][h, :, tok0:tok0 + TT])
        kvn = []
        for k in range(2):
            sqt, B_sqt = sq_ring.next()
            fw.op(fw.dve, lambda sqt=sqt, k=k: nc.vector.tensor_tensor(out=sqt[:, :], in0=lat[:, 6 + k, :], in1=rk[:, :], op=ALU.mult), [B_lat, B_rk], [B_sqt])
            kvn.append((sqt, B_sqt))
        for s4 in range(4):
            pt, B_pt = pmm.next()
            for k in range(2):
                mm(fw, pt[:, :], kvn[k][0][:, s4 * 128:(s4 + 1) * 128], wkv[:, k, :], k == 0, k == 1, [B_wkv, kvn[k][1]], [B_pt])
            og, B_og = out_ring.next()
            evac_copy(fw.act, og[:, :], pt[:, :], [B_pt], [B_og])
            store(og[:, :], B_og, d[f"v_mla{L}"][tok0 + s4 * 128: tok0 + (s4 + 1) * 128, :])
    fw.close_scope()


def host_consts():
    pos = np.arange(S, dtype=np.float32)
    inv_freq = (10000.0 ** (-np.arange(0, ROPE // 2, dtype=np.float32) * np.float32(2.0 / ROPE))).astype(np.float32)
    ang = (pos[None, :] * inv_freq[:, None]).astype(np.float32)
    cos = np.cos(ang).astype(np.float32)
    sin = np.sin(ang).astype(np.float32)
    dr_idx, dc_idx, validf, valids = na_host_index()
    return {
        "c_ident": np.eye(128, dtype=np.float32),
        "c_cos": np.ascontiguousarray(np.concatenate([cos, cos], 0)),
        "c_sin": np.ascontiguousarray(np.concatenate([sin, sin], 0)),
        "c_maskf": np.ascontiguousarray(np.where(validf, 0.0, NEG / NA_SCALE).astype(np.float32).reshape(128, U_F * 64)),
        "c_masks": np.ascontiguousarray(np.where(valids, 0.0, NEG / NA_SCALE).astype(np.float32).reshape(128, U_S * 64)),
    }


def host_na_table(na_rpb):
    dr_idx, dc_idx, validf, valids = na_host_index()
    g = na_rpb[:, :, dr_idx, dc_idx]
    g = np.where(validf[None, None], g, np.float32(0.0)).astype(np.float32)
    return np.ascontiguousarray(g.reshape(DEPTH, NA_H, 128, U_F * 64))


W_SHAPES = {
    "w_in": [DEPTH, D, N_IN], "b_gate": [DEPTH, 2 * D], "q_norm_g": [DEPTH, QL], "w_uq": [DEPTH, QL, 768],
    "kv_norm_g": [DEPTH, KVL], "w_ukv": [DEPTH, KVL, 1024], "w_na_o": [DEPTH, 512, D], "w_mla_o": [DEPTH, 512, D],
    "w_out": [DEPTH, D, D], "ln1_g": [DEPTH, D], "ln1_b": [DEPTH, D],
    "ffn_w1": [1, D, D_FF], "ffn_w3": [1, D, D_FF], "ffn_w2": [1, D_FF, D],
    "moe_w_router": [1, D, NE], "moe_b_router": [1, NE],
    "moe_w1": [1, NE, D, D_FFE], "moe_w3": [1, NE, D, D_FFE], "moe_w2": [1, NE, D_FFE, D],
    "w_ple_gate": [DEPTH, D, D], "w_ple": [DEPTH, PLE, D], "ln2_g": [DEPTH, D], "ln2_b": [DEPTH, D],
}
C_SHAPES = {"c_ident": [128, 128], "c_cos": [32, S], "c_sin": [32, S], "c_maskf": [128, U_F * 64], "c_masks": [128, U_S * 64],
            "c_natab": [DEPTH, NA_H, 128, U_F * 64]}


def declare_io(P):
    P.din("x", [S, D])
    P.din("p", [DEPTH, S, PLE])
    for k, shp in W_SHAPES.items():
        P.din(k, shp)
    for k, shp in C_SHAPES.items():
        P.din(k, shp)
    for L in range(DEPTH):
        P.dscr(f"qT_na{L}", [512, S], BF16)
        P.dscr(f"kT_na{L}", [512, S], BF16)
        P.dscr(f"v_na{L}", [S, 512], BF16)
        P.dscr(f"sgT{L}", [2048, S], BF16)
        P.dscr(f"qT_mla{L}", [8, 96, S], BF16)
        P.dscr(f"krT{L}", [32, S], BF16)
        P.dscr(f"kT_mla{L}", [8, 64, S], BF16)
        P.dscr(f"v_mla{L}", [S, 512], BF16)
        P.dscr(f"o_naT{L}", [512, S], BF16)
        P.dscr(f"o_mlaT{L}", [512, S], BF16)


def na_host_index():
    p = np.arange(128)
    krp, kc = p // 64, p % 64
    u = np.arange(U_F)
    qc = np.arange(64)
    dr = (krp[:, None, None] + 14 - u[None, :, None]) + 0 * qc[None, None, :]
    ws = np.clip(qc - 8, 0, 48)
    colvalid = (kc[:, None, None] >= ws[None, None, :]) & (kc[:, None, None] < ws[None, None, :] + 16)
    colvalid = colvalid & (dr == dr)
    dc = kc[:, None, None] - qc[None, None, :] + 15 + 0 * u[None, :, None]
    validf = (np.abs(dr) <= 7) & colvalid
    dr_idx = np.clip(dr + 7, 0, 14)
    dc_idx = np.clip(dc, 0, 30)
    valids = validf[:, 4:4 + U_S, :] & (dr[:, 4:4 + U_S, :] >= -4) & (dr[:, 4:4 + U_S, :] <= 3)
    return dr_idx, dc_idx, validf, valids


def phase_B(P, L, side_w=None):
    fw, nc, d, c = P.fw, P.nc, P.dram, P.c
    fw.open_scope()
    sb, ps = fw.sb, fw.ps
    NQB = 512

    pS = Ring(fw, "pS", 3, [128, 2, NQB], F32, psum=True)
    pacc = Ring(fw, "pacc", 2, [128, NQB], F32, psum=True)
    Pr = Ring(fw, "Pr", 4, [128, 2, NQB], BF16)
    pending_norm = [None]
    accs = Ring(fw, "accs", 2, [128, NQB], F32)
    rden = Ring(fw, "rden", 2, [128, NQB], F32)
    ost = Ring(fw, "ost", 3, [64, NQB], BF16)
    sqr = Ring(fw, "sqr", 2, [128, NQB], BF16)
    nmx = Ring(fw, "nmx", 2, [128, 34], F32)

    def norm_bound(QT, B_Q, KT, B_K, pb, Kd, scale):
        nm, B_nm = nmx.next()
        for which, (T, B_T) in enumerate(((QT, B_Q), (KT, B_K))):
            for blk in range(S // NQB):
                sq, B_sq = sqr.next()
                fw.op(fw.pool, lambda sq=sq, T=T, blk=blk: nc.gpsimd.tensor_tensor(out=sq[pb:pb + Kd, :], in0=T[pb:pb + Kd, blk * NQB:(blk + 1) * NQB], in1=T[pb:pb + Kd, blk * NQB:(blk + 1) * NQB], op=ALU.mult), [B_T], [B_sq])
                pn, B_pn = pS.next()
                mm(fw, pn[:, 0, :], c["ones_b"][pb:pb + Kd, :], sq[pb:pb + Kd, :], True, True, [c["B_ones_b"], B_sq], [B_pn])
                col = which * 16 + blk
                fw.op(fw.dve, lambda pn=pn, nm=nm, col=col: nc.vector.tensor_reduce(out=nm[:, col:col + 1], in_=pn[:, 0, :], axis=AX.X, op=ALU.max), [B_pn], [B_nm])
        fw.op(fw.dve, lambda nm=nm: nc.vector.tensor_reduce(out=nm[:, 32:33], in_=nm[:, 0:16], axis=AX.X, op=ALU.max), [B_nm], [B_nm])
        fw.op(fw.dve, lambda nm=nm: nc.vector.tensor_reduce(out=nm[:, 33:34], in_=nm[:, 16:32], axis=AX.X, op=ALU.max), [B_nm], [B_nm])
        fw.op(fw.dve, lambda nm=nm: nc.vector.tensor_tensor(out=nm[:, 32:33], in0=nm[:, 32:33], in1=nm[:, 33:34], op=ALU.mult), [B_nm], [B_nm])
        fw.op(fw.act, lambda nm=nm: nc.scalar.activation(nm[:, 33:34], nm[:, 32:33], AF.Sqrt, scale=scale * scale), [B_nm], [B_nm])
        fw.op(fw.dve, lambda nm=nm: nc.vector.tensor_scalar(out=nm[:, 33:34], in0=nm[:, 33:34], scalar1=-1.0, scalar2=None, op0=ALU.mult), [B_nm], [B_nm])
        return nm[:, 33:34], B_nm

    def attn_group(QT, B_Q, pb, Kd, q0, nq, items, scale, negc, B_negc, dst):
        pairs = [items[i:i + 2] for i in range(0, len(items), 2)]
        acc, B_acc = pacc.next()
        sbufs = {}

        def emit_S(pi):
            St, B_S = pS.next()
            for j, (kT, B_k, v, B_v, tab, B_tab) in enumerate(pairs[pi]):
                mm(fw, St[:, j, 0:nq], kT, QT[pb:pb + Kd, q0:q0 + nq], True, tab is None, [B_k, B_Q], [B_S])
                if tab is not None:
                    mm(fw, St[:, j, 0:nq], c["ident_b"][:, :], tab, False, True, [c["B_ident_b"], B_tab], [B_S])
            sbufs[pi] = (St, B_S)

        def emit_PV(pi):
            St, B_S = sbufs.pop(pi)
            n = len(pairs[pi])
            Pt, B_P = Pr.next()
            fw.op(fw.act, lambda: nc.scalar.activation(Pt[:, 0:n, 0:nq], St[:, 0:n, 0:nq], AF.Exp, bias=negc, scale=scale), [B_S, B_negc], [B_P])
            for j, (kT, B_k, v, B_v, tab, B_tab) in enumerate(pairs[pi]):
                first = (pi == 0 and j == 0)
                last = (pi == len(pairs) - 1 and j == n - 1)
                mm(fw, acc[0:65, 0:nq], v, Pt[:, j, 0:nq], first, last, [B_v, B_P], [B_acc])

        LA = 3
        for pi in range(min(LA, len(pairs))):
            emit_S(pi)
        for pi in range(len(pairs)):
            emit_PV(pi)
            if pi + LA < len(pairs):
                emit_S(pi + LA)

        def norm():
            a_s, B_as = accs.next()
            fw.op(fw.dve, lambda: nc.vector.tensor_copy(a_s[0:65, 0:nq], acc[0:65, 0:nq]), [B_acc], [B_as])
            rd, B_rd = rden.next()
            fw.op(fw.dve, lambda: nc.vector.reciprocal(rd[64:65, 0:nq], a_s[64:65, 0:nq]), [B_as], [B_rd])
            bc, B_bc = pS.next()
            mm(fw, bc[0:64, 0, 0:nq], c["ones_f"][64:65, 0:64], rd[64:65, 0:nq], True, True, [c["B_ones_f"], B_rd], [B_bc])
            o, B_o = ost.next()
            fw.op(fw.dve, lambda: nc.vector.tensor_tensor(out=o[0:64, 0:nq], in0=a_s[0:64, 0:nq], in1=bc[0:64, 0, 0:nq], op=ALU.mult), [B_as, B_bc], [B_o])
            fw.dma(fw.sp, dst, o[0:64, 0:nq], reads=[B_o])

        if pending_norm[0] is not None:
            pending_norm[0]()
        pending_norm[0] = norm

    def flush_norm():
        if pending_norm[0] is not None:
            pending_norm[0]()
            pending_norm[0] = None

    if P.cfg.get("do_na", True):
        fw.open_scope()
        mkf = sb("mkf", [128, U_F * 64], F32); B_mkf = Buf("mkf")
        mks = sb("mks", [128, U_S * 64], F32); B_mks = Buf("mks")
        fw.dma(fw.sp, mkf[:, :], d["c_maskf"][:, :], writes=[B_mkf])
        fw.dma(fw.sp, mks[:, :], d["c_masks"][:, :], writes=[B_mks])
        gst = Ring(fw, "gst", 2, [128, U_F * 64], F32)
        ttf = Ring(fw, "ttf", 2, [128, U_F * 64], BF16)
        tts = Ring(fw, "tts", 2, [128, U_S * 64], BF16)
        QTr = Ring(fw, "naQ", 2, [128, S], BF16)
        KTr = Ring(fw, "naK", 2, [128, S], BF16)
        Vr = Ring(fw, "naV", 2, [128, 64, 2, 65], BF16)
        heads = P.cfg.get("na_heads", list(range(NA_H)))
        pair_bufs = {}

        def na_load_pair(hp):
            if hp in pair_bufs:
                return
            QT, B_Q = QTr.next(); KT, B_K = KTr.next(); V, B_V = Vr.next()
            fw.dma(fw.sp, QT[:, :], d[f"qT_na{L}"][hp * 128:(hp + 1) * 128, :], writes=[B_Q])
            fw.dma(fw.sp, KT[:, :], d[f"kT_na{L}"][hp * 128:(hp + 1) * 128, :], writes=[B_K])
            fw.op(fw.pool, lambda V=V: nc.gpsimd.memset(V[:, :, :, :], 1.0), [], [B_V])
            for g in range(8):
                for hh in range(2):
                    fw.dma(fw.sp, V[:, g * 8:(g + 1) * 8, hh, 0:64],
                           d[f"v_na{L}"][g * 1024:(g + 1) * 1024, (hp * 2 + hh) * 64:(hp * 2 + hh + 1) * 64].rearrange("(kt p) e -> p kt e", p=128),
                           writes=[B_V])
            pair_bufs.clear() if len(pair_bufs) >= 2 else None
            pair_bufs[hp] = (QT, B_Q, KT, B_K, V, B_V)

        def na_tables(h):
            g_, B_g = gst.next()
            fw.dma(fw.sp, g_[:, :], d["c_natab"][L, h], writes=[B_g])
            tf, B_tf = ttf.next(); ts_, B_ts = tts.next()
            fw.op(fw.dve, lambda: nc.vector.scalar_tensor_tensor(out=tf[:, :], in0=g_[:, :], scalar=1.0 / NA_SCALE, in1=mkf[:, :], op0=ALU.mult, op1=ALU.add), [B_g, B_mkf], [B_tf])
            fw.op(fw.dve, lambda: nc.vector.scalar_tensor_tensor(out=ts_[:, :], in0=g_[:, 4 * 64:(4 + U_S) * 64], scalar=1.0 / NA_SCALE, in1=mks[:, :], op0=ALU.mult, op1=ALU.add), [B_g, B_mks], [B_ts])
            return tf, B_tf, ts_, B_ts

        def na_bound(h):
            QT, B_Q, KT, B_K, V, B_V = pair_bufs[h // 2]
            return norm_bound(QT, B_Q, KT, B_K, (h % 2) * 64, 64, NA_SCALE)

        prepped = {}
        na_load_pair(heads[0] // 2)
        prepped[heads[0]] = (na_tables(heads[0]), na_bound(heads[0]))
        for hi, h in enumerate(heads):
            hp, pb = h // 2, (h % 2) * 64
            hn = heads[hi + 1] if hi + 1 < len(heads) else None
            QT, B_Q, KT, B_K, V, B_V = pair_bufs[hp]
            (tf, B_tf, ts_, B_ts), (negc, B_negc) = prepped.pop(h)
            if hn is not None and (h % 2 == 0 or hn // 2 == hp):
                if hn // 2 != hp and len(pair_bufs) < 2:
                    na_load_pair(hn // 2)
            dst_all = d[f"o_naT{L}"][h * 64:(h + 1) * 64, :]

            def item(kt, tab):
                return (KT[pb:pb + 64, kt * 128:(kt + 1) * 128], B_K, V[:, kt, h % 2, :], B_V, tab, None)

            groups = []
            for r in range(4):
                its = [item(i, tf[:, (14 - 2 * i + r) * 64:(14 - 2 * i + r) * 64 + 64]) for i in range(4)]
                groups.append((r * 64, 64, its, B_tf))
            its = [item(i - 2, ts_[:, (2 * (7 - i) + 4) * 64:(2 * (7 - i) + 4) * 64 + 256]) for i in range(2, 8)]
            groups.append((256, 256, its, B_ts))
            for j in range(1, 15):
                its = [item(4 * j - 2 + i, ts_[:, 2 * (7 - i) * 64:2 * (7 - i) * 64 + 512]) for i in range(8)]
                groups.append((512 * j, 512, its, B_ts))
            its = [item(58 + i, ts_[:, 2 * (7 - i) * 64:2 * (7 - i) * 64 + 320]) for i in range(6)]
            groups.append((7680, 320, its, B_ts))
            for r in range(125, 128):
                its = [item(60 + i, tf[:, (14 - 2 * i + r - 120) * 64:(14 - 2 * i + r - 120) * 64 + 64]) for i in range(4)]
                groups.append((r * 64, 64, its, B_tf))
            gsel = P.cfg.get("na_groups", None)
            sel = [g for gi, g in enumerate(groups) if gsel is None or gi in gsel]
            for gi, (q0, nq, its, B_tab) in enumerate(sel):
                its = [(a_, b_, v, bv, tab, B_tab) for (a_, b_, v, bv, tab, _) in its]
                attn_group(QT, B_Q, pb, 64, q0, nq, its, NA_SCALE, negc, B_negc, dst_all[:, q0:q0 + nq])
                if hn is not None and gi == (len(sel) * 2) // 3:
                    if hn // 2 not in pair_bufs:
                        if len(pair_bufs) >= 2:
                            pair_bufs.pop(min(pair_bufs))
                        na_load_pair(hn // 2)
                    prepped[hn] = (na_tables(hn), na_bound(hn))
            if hn is not None and hn not in prepped:
                if hn // 2 not in pair_bufs:
                    if len(pair_bufs) >= 2:
                        pair_bufs.pop(min(pair_bufs))
                    na_load_pair(hn // 2)
                prepped[hn] = (na_tables(hn), na_bound(hn))
        flush_norm()
        fw.close_scope()

    if P.cfg.get("do_mla", True):
        fw.open_scope()
        QTr = Ring(fw, "mQ", 2, [96, S], BF16)
        KTr = Ring(fw, "mK", 2, [96, S], BF16)
        Vr = Ring(fw, "mV", 2, [128, 64, 65], BF16)
        heads = P.cfg.get("mla_heads", list(range(MLA_H)))
        qblocks = P.cfg.get("mla_qblocks", list(range(S // NQB)))
        wgen, w_per_group = None, 0
        if side_w is not None:
            wc = WConv(P, [fw.pool])
            wc.alloc(nbig=1)
            wgen = wc.units(side_w)
            w_per_group = 7

        def step_w(n):
            nonlocal wgen
            for _ in range(n):
                if wgen is None:
                    return
                try:
                    next(wgen)
                except StopIteration:
                    wgen = None

        def mla_load(h):
            QT, B_Q = QTr.next(); KT, B_K = KTr.next(); V, B_V = Vr.next()
            fw.dma(fw.sp, QT[:, :], d[f"qT_mla{L}"][h], writes=[B_Q])
            fw.dma(fw.sp, KT[0:64, :], d[f"kT_mla{L}"][h], writes=[B_K])
            fw.dma(fw.sp, KT[64:96, :], d[f"krT{L}"][:, :], writes=[B_K])
            fw.op(fw.pool, lambda V=V: nc.gpsimd.memset(V[:, :, :], 1.0), [], [B_V])
            for g in range(8):
                fw.dma(fw.sp, V[:, g * 8:(g + 1) * 8, 0:64],
                       d[f"v_mla{L}"][g * 1024:(g + 1) * 1024, h * 64:(h + 1) * 64].rearrange("(kt p) e -> p kt e", p=128),
                       writes=[B_V])
            return QT, B_Q, KT, B_K, V, B_V

        cur = mla_load(heads[0])
        cur_neg = norm_bound(cur[0], cur[1], cur[2], cur[3], 0, 96, MLA_SCALE)
        for hi, h in enumerate(heads):
            QT, B_Q, KT, B_K, V, B_V = cur
            negc, B_negc = cur_neg
            nxt = mla_load(heads[hi + 1]) if hi + 1 < len(heads) else None
            nxt_neg = None
            for qi, qb in enumerate(qblocks):
                its = [(KT[0:96, kt * 128:(kt + 1) * 128], B_K, V[:, kt, :], B_V, None, None) for kt in range(64)]
                attn_group(QT, B_Q, 0, 96, qb * NQB, NQB, its, MLA_SCALE, negc, B_negc, d[f"o_mlaT{L}"][h * 64:(h + 1) * 64, qb * NQB:(qb + 1) * NQB])
                step_w(w_per_group)
                if nxt is not None and qi == max(0, len(qblocks) - 4):
                    nxt_neg = norm_bound(nxt[0], nxt[1], nxt[2], nxt[3], 0, 96, MLA_SCALE)
            if nxt is not None and nxt_neg is None:
                nxt_neg = norm_bound(nxt[0], nxt[1], nxt[2], nxt[3], 0, 96, MLA_SCALE)
            cur, cur_neg = nxt, nxt_neg
        flush_norm()
        step_w(1 << 30)
        fw.close_scope()
    fw.close_scope()


def ffn_dims(L):
    if L % 2 == 0:
        return 1, D_FF, "ffn_w1", "ffn_w3", "ffn_w2"
    return NE, D_FFE, "moe_w1", "moe_w3", "moe_w2"


class WConv:
    def __init__(self, P, engs):
        self.P, self.engs, self.cnt = P, engs, 0
        self.pending_store = None

    def alloc(self, nbig=1):
        fw = self.P.fw
        self.stg = Ring(fw, "wstg", 3, [128, 2048], F32)
        self.cvt = Ring(fw, "wcvt", 3, [128, 2048], BF16)
        self.HC = 7
        self.big = Ring(fw, "w13b", nbig, [128, self.HC, 2, 8, 128], BF16)
        self.st2 = Ring(fw, "wst2", 2, [128, self.HC * 128], F32)

    def convert(self, out_ap, in_ap, reads, writes):
        fw, nc = self.P.fw, self.P.nc
        e = self.engs[self.cnt % len(self.engs)]
        self.cnt += 1
        if e is fw.act:
            fw.op(e, lambda: nc.scalar.copy(out_ap, in_ap), reads, writes)
        else:
            fw.op(e, lambda: e.h.tensor_copy(out_ap, in_ap), reads, writes)

    def flush_store(self):
        if self.pending_store is not None:
            dst, src, B_c = self.pending_store
            self.P.fw.dma(self.P.fw.sp, dst, src, reads=[B_c])
            self.pending_store = None

    def conv_rows(self, src, dst, ncols):
        fw = self.P.fw
        for c0 in range(0, ncols, 2048):
            w = min(2048, ncols - c0)
            s_, B_s = self.stg.next()
            fw.dma(fw.sp, s_[:, 0:w], src[:, c0:c0 + w], writes=[B_s])
            c_, B_c = self.cvt.next()
            self.convert(c_[:, 0:w], s_[:, 0:w], [B_s], [B_c])
            self.flush_store()
            self.pending_store = (dst[:, c0:c0 + w], c_[:, 0:w], B_c)

    def units(self, layers):
        P, fw, d = self.P, self.P.fw, self.P.dram
        HC = self.HC
        for L in layers:
            for name, K_ in (("w_na_o", 512), ("w_mla_o", 512), ("w_out", D), ("w_ple_gate", D), ("w_ple", PLE)):
                for k in range(K_ // 128):
                    self.conv_rows(d[name][L, k * 128:(k + 1) * 128, :], d[f"{name}_s{L}"][:, k, :], D)
                    yield
            ne, dff, n1, n3, n2 = ffn_dims(L)
            j = L // 2
            nch = dff // 128
            w1 = d[n1][j] if ne > 1 else d[n1]
            w3 = d[n3][j] if ne > 1 else d[n3]
            w2 = d[n2][j] if ne > 1 else d[n2]
            for e in range(ne):
                for c_ in range(nch):
                    self.conv_rows(w2[e, c_ * 128:(c_ + 1) * 128, :], d[f"w2s{L}"][e, c_], D)
                    yield
            self.flush_store()
            for e in range(ne):
                for c0 in range(0, nch, HC):
                    gn = min(HC, nch - c0)
                    bt, B_b = self.big.next()
                    for i, wsrc in enumerate((w1, w3)):
                        for k in range(8):
                            s_, B_s = self.st2.next()
                            fw.dma(fw.sp, s_[:, 0:gn * 128], wsrc[e, k * 128:(k + 1) * 128, c0 * 128:(c0 + gn) * 128], writes=[B_s])
                            self.convert(bt[:, 0:gn, i, k, :], s_[:, 0:gn * 128].rearrange("p (c j) -> p c j", j=128), [B_s], [B_b])
                            yield
                    for cc in range(gn):
                        fw.dma(fw.sp, d[f"w13s{L}"][e, c0 + cc], bt[:, cc, :, :, :].rearrange("p i k j -> p (i k j)"), reads=[B_b])
                    yield


def phase_W(P, layers):
    fw = P.fw
    fw.open_scope()
    wc = WConv(P, [fw.dve, fw.pool, fw.act])
    wc.alloc(nbig=2)
    for _ in wc.units(layers):
        pass
    fw.close_scope()


def declare_scratch_C(P):
    for L in range(DEPTH):
        P.dscr(f"w_na_o_s{L}", [128, 4, D], BF16)
        P.dscr(f"w_mla_o_s{L}", [128, 4, D], BF16)
        P.dscr(f"w_out_s{L}", [128, 8, D], BF16)
        P.dscr(f"w_ple_gate_s{L}", [128, 8, D], BF16)
        P.dscr(f"w_ple_s{L}", [128, 2, D], BF16)
        ne, dff, _, _, _ = ffn_dims(L)
        P.dscr(f"w13s{L}", [ne, dff // 128, 128, 2 * 8 * 128], BF16)
        P.dscr(f"w2s{L}", [ne, dff // 128, 128, D], BF16)
    P.dscr("x_mid", [S, D], F32)


def phase_C(P, L, x_src, x_dst):
    fw, nc, d, c = P.fw, P.nc, P.dram, P.c
    moe = (L % 2 == 1)
    ne, dff, _, _, _ = ffn_dims(L)
    nch = dff // 128
    TT = 1024
    NS = TT // 128
    NT = P.cfg.get("ntiles_C", S // TT)
    G = 4
    fw.open_scope()
    sb = fw.sb

    lnp = sb("lnp", [128, 4, D], F32); B_lnp = Buf("lnp")
    for i, nm in enumerate(("ln1_g", "ln1_b", "ln2_g", "ln2_b")):
        fw.dma(fw.sp, lnp[:, i, :], d[nm][L:L + 1, :].broadcast_to([128, D]), writes=[B_lnp])
    cst = sb("cst", [128, 4], F32); B_cst = Buf("cst")
    fw.op(fw.dve, lambda: nc.vector.memset(cst[:, 0:1], LN_EPS), [], [B_cst])
    fw.op(fw.dve, lambda: nc.vector.memset(cst[:, 1:2], -0.5), [], [B_cst])
    if moe:
        wr = sb("wr", [128, 8, NE], F32); B_wr = Buf("wr")
        fw.dma(fw.sp, wr[:, :, :], d["moe_w_router"][L // 2].rearrange("(k p) e -> p k e", p=128), writes=[B_wr])
        br = sb("br", [128, NE], F32); B_br = Buf("br")
        fw.dma(fw.sp, br[:, :], d["moe_b_router"][L // 2:L // 2 + 1, :].broadcast_to([128, NE]), writes=[B_br])
    gates = sb("gates", [128, NS, NE], F32); B_gates = Buf("gates")

    xacc = sb("xacc", [128, NS, D], F32)
    B_xa = [Buf(f"xacc{s}") for s in range(NS)]
    x1T = sb("x1T", [128, 8, TT], BF16); B_x1T = Buf("x1T")
    oT = Ring(fw, "oT", 2, [128, 4, 512], BF16)
    sgr = Ring(fw, "sgr", 4, [128, 512], BF16)
    mrg = Ring(fw, "mrg", 1, [128, 8, 512], BF16)
    tmp = Ring(fw, "tmpf", 3, [128, 512], F32)
    wring = Ring(fw, "wring", 4, [128, 4, D], BF16)
    x1Tf = Ring(fw, "x1Tf", 2, [128, 8, 128], F32)
    pfr = Ring(fw, "pfr", 2, [128, PLE], F32)
    pT = sb("pT", [128, 2, TT], BF16); B_pT = Buf("pT")
    w13r = Ring(fw, "w13r", 3, [128, 2, 8, 128], BF16)
    w2r = Ring(fw, "w2r", 2, [128, G, D], BF16)
    hTr = Ring(fw, "hTr", 2, [128, G, TT], BF16)
    silr = Ring(fw, "silr", 2, [128, 512], F32)
    stat = Ring(fw, "stat", 2, [128, 16], F32)
    rlg = Ring(fw, "rlg", 2, [128, 32], F32)
    pmm = Ring(fw, "pmmC", 6, [128, 512], F32, psum=True)
    ptr = Ring(fw, "ptrC", 2, [128, 4, 128], F32, psum=True)

    def load_w(name, k0, nk):
        wt, B_w = wring.next()
        fw.dma(fw.sp, wt[:, 0:nk, :], d[f"{name}_s{L}"][:, k0:k0 + nk, :], writes=[B_w])
        return wt, B_w

    def layer_norm(s, gi):
        st, B_st = stat.next()
        xs = xacc[:, s, :]
        for hh in range(2):
            fw.op(fw.dve, lambda hh=hh: nc.vector.bn_stats(st[:, hh * 6:(hh + 1) * 6], xacc[:, s, hh * 512:(hh + 1) * 512]), [B_xa[s]], [B_st])
        fw.op(fw.dve, lambda: nc.vector.bn_aggr(st[:, 12:14], st[:, 0:12]), [B_st], [B_st])
        fw.op(fw.pool, lambda: nc.gpsimd.tensor_scalar(out=st[:, 14:15], in0=st[:, 13:14], scalar1=cst[:, 0:1], scalar2=None, op0=ALU.add), [B_st, B_cst], [B_st])
        fw.op(fw.pool, lambda: nc.gpsimd.tensor_tensor(out=st[:, 14:15], in0=st[:, 14:15], in1=cst[:, 1:2], op=ALU.pow), [B_st, B_cst], [B_st])
        fw.op(fw.dve, lambda: nc.vector.tensor_scalar(out=xs, in0=xs, scalar1=st[:, 12:13], scalar2=st[:, 14:15], op0=ALU.subtract, op1=ALU.mult), [B_st], [B_xa[s]])
        fw.op(fw.dve, lambda: nc.vector.tensor_tensor(out=xs, in0=xs, in1=lnp[:, gi, :], op=ALU.mult), [B_lnp], [B_xa[s]])
        fw.op(fw.pool, lambda: nc.gpsimd.tensor_tensor(out=xs, in0=xs, in1=lnp[:, gi + 1, :], op=ALU.add), [B_lnp], [B_xa[s]])

    wbr = {}
    pre = {}
    preloaded = False

    def c1a(t, th):
        if t not in wbr:
            wbr.clear()
            wbr[t] = (load_w("w_na_o", 0, 4), load_w("w_mla_o", 0, 4))
        (wna, B_wna), (wml, B_wml) = wbr[t]
        t0 = t * TT + th * 512
        ona, B_ona = oT.next()
        oml, B_oml = oT.next()
        fw.dma(fw.sp, ona[:, :, :], d[f"o_naT{L}"][:, t0:t0 + 512].rearrange("(k p) t -> p k t", p=128), writes=[B_ona])
        fw.dma(fw.sp, oml[:, :, :], d[f"o_mlaT{L}"][:, t0:t0 + 512].rearrange("(k p) t -> p k t", p=128), writes=[B_oml])
        mg, B_mg = mrg.next()
        for fc in range(8):
            sgn, B_sgn = sgr.next()
            sgm, B_sgm = sgr.next()
            fw.dma(fw.sp, sgn[:, :], d[f"sgT{L}"][fc * 128:(fc + 1) * 128, t0:t0 + 512], writes=[B_sgn])
            fw.dma(fw.sp, sgm[:, :], d[f"sgT{L}"][D + fc * 128:D + (fc + 1) * 128, t0:t0 + 512], writes=[B_sgm])
            pn, B_pn = pmm.next()
            for k in range(4):
                mm(fw, pn[:, :], wna[:, k, fc * 128:(fc + 1) * 128], ona[:, k, :], k == 0, k == 3, [B_wna, B_ona], [B_pn])
            pm, B_pm = pmm.next()
            for k in range(4):
                mm(fw, pm[:, :], wml[:, k, fc * 128:(fc + 1) * 128], oml[:, k, :], k == 0, k == 3, [B_wml, B_oml], [B_pm])
            t1, B_t1 = tmp.next()
            t2, B_t2 = tmp.next()
            fw.op(fw.dve, lambda: nc.vector.tensor_tensor(out=t1[:, :], in0=pn[:, :], in1=sgn[:, :], op=ALU.mult), [B_pn, B_sgn], [B_t1])
            fw.op(fw.dve, lambda: nc.vector.tensor_tensor(out=t2[:, :], in0=pm[:, :], in1=sgm[:, :], op=ALU.mult), [B_pm, B_sgm], [B_t2])
            fw.op(fw.pool, lambda: nc.gpsimd.tensor_tensor(out=mg[:, fc, :], in0=t1[:, :], in1=t2[:, :], op=ALU.add), [B_t1, B_t2], [B_mg])
        return mg, B_mg

    for t in range(NT):
        tok0 = t * TT
        if not preloaded:
            for s in range(NS):
                fw.dma(fw.sp, xacc[:, s, :], x_src[tok0 + s * 128: tok0 + (s + 1) * 128, :], writes=[B_xa[s]])
        preloaded = False
        for th in range(TT // 512):
            if (t, th) in pre:
                mg, B_mg = pre.pop((t, th))
            else:
                mg, B_mg = c1a(t, th)
            if th == 0:
                wo = [load_w("w_out", 0, 4), load_w("w_out", 4, 4)]
            for s4 in range(4):
                s = th * 4 + s4
                for half in range(2):
                    pp, B_pp = pmm.next()
                    for k in range(8):
                        wt, B_w = wo[k // 4]
                        mm(fw, pp[:, :], mg[:, k, s4 * 128:(s4 + 1) * 128], wt[:, k % 4, half * 512:(half + 1) * 512], k == 0, k == 7, [B_mg, B_w], [B_pp])
                    xh = xacc[:, s, half * 512:(half + 1) * 512]
                    fw.op(fw.dve, lambda: nc.vector.scalar_tensor_tensor(out=xh, in0=xh, scalar=ALPHA, in1=pp[:, :], op0=ALU.mult, op1=ALU.add), [B_pp], [B_xa[s]])
                layer_norm(s, 0)
        for s in range(NS):
            xf, B_xf = x1Tf.next()
            for hf in range(2):
                pt_, B_pt = ptr.next()
                for j in range(4):
                    fc = hf * 4 + j
                    fw.op(fw.pe, lambda fc=fc, j=j: nc.tensor.transpose(pt_[:, j, :], xacc[:, s, fc * 128:(fc + 1) * 128], c["ident_f"][:]),
                          [B_xa[s], c["B_ident_f"]], [B_pt], inc=(j == 3))
                fw.op(fw.dve, lambda: nc.vector.tensor_copy(x1T[:, hf * 4:(hf + 1) * 4, s * 128:(s + 1) * 128], pt_[:, :, :]), [B_pt], [B_x1T])
                if moe:
                    fw.op(fw.dve, lambda: nc.vector.tensor_copy(xf[:, hf * 4:(hf + 1) * 4, :], pt_[:, :, :]), [B_pt], [B_xf])
            if moe:
                pr_, B_pr = pmm.next()
                for k in range(8):
                    mm(fw, pr_[:, 0:NE], xf[:, k, :], wr[:, k, :], k == 0, k == 7, [B_xf, B_wr], [B_pr])
                lg, B_lg = rlg.next()
                fw.op(fw.dve, lambda: nc.vector.tensor_tensor(out=lg[:, 0:8], in0=pr_[:, 0:NE], in1=br[:, :], op=ALU.add), [B_pr, B_br], [B_lg])
                fw.op(fw.dve, lambda: nc.vector.max(out=lg[:, 8:16], in_=lg[:, 0:8]), [B_lg], [B_lg])
                fw.op(fw.dve, lambda: nc.vector.tensor_tensor(out=lg[:, 16:17], in0=lg[:, 8:9], in1=lg[:, 9:10], op=ALU.add), [B_lg], [B_lg])
                fw.op(fw.dve, lambda: nc.vector.tensor_scalar(out=lg[:, 24:32], in0=lg[:, 0:8], scalar1=2.0, scalar2=lg[:, 16:17], op0=ALU.mult, op1=ALU.subtract), [B_lg], [B_lg])
                fw.op(fw.act, lambda: nc.scalar.activation(lg[:, 24:32], lg[:, 24:32], AF.Sigmoid), [B_lg], [B_lg])
                fw.op(fw.dve, lambda: nc.vector.tensor_scalar(out=lg[:, 16:24], in0=lg[:, 0:8], scalar1=lg[:, 9:10], scalar2=None, op0=ALU.is_ge), [B_lg], [B_lg])
                fw.op(fw.dve, lambda: nc.vector.tensor_tensor(out=gates[:, s, :], in0=lg[:, 16:24], in1=lg[:, 24:32], op=ALU.mult), [B_lg], [B_gates])
            fw.op(fw.act, lambda: nc.scalar.mul(xacc[:, s, :], xacc[:, s, :], ALPHA), [], [B_xa[s]])
        for s in range(NS):
            pf, B_pf = pfr.next()
            fw.dma(fw.sp, pf[:, :], d["p"][L, tok0 + s * 128: tok0 + (s + 1) * 128, :], writes=[B_pf])
            pt_, B_pt = ptr.next()
            for k in range(2):
                fw.op(fw.pe, lambda k=k: nc.tensor.transpose(pt_[:, k, :], pf[:, k * 128:(k + 1) * 128], c["ident_f"][:]),
                      [B_pf, c["B_ident_f"]], [B_pt], inc=(k == 1))
            fw.op(fw.dve, lambda: nc.vector.tensor_copy(pT[:, :, s * 128:(s + 1) * 128], pt_[:, 0:2, :]), [B_pt], [B_pT])
        wpg = [load_w("w_ple_gate", 0, 4), load_w("w_ple_gate", 4, 4)]
        wp, B_wp = load_w("w_ple", 0, 2)
        for s in range(NS):
            for half in range(2):
                pg, B_pg = pmm.next()
                for k in range(8):
                    wt, B_w = wpg[k // 4]
                    mm(fw, pg[:, :], x1T[:, k, s * 128:(s + 1) * 128], wt[:, k % 4, half * 512:(half + 1) * 512], k == 0, k == 7, [B_x1T, B_w], [B_pg])
                pq, B_pq = pmm.next()
                for k in range(2):
                    mm(fw, pq[:, :], pT[:, k, s * 128:(s + 1) * 128], wp[:, k, half * 512:(half + 1) * 512], k == 0, k == 1, [B_pT, B_wp], [B_pq])
                sg_, B_sg = silr.next()
                fw.op(fw.act, lambda: nc.scalar.activation(sg_[:, :], pg[:, :], AF.Sigmoid), [B_pg], [B_sg])
                t1, B_t1 = tmp.next()
                fw.op(fw.dve, lambda: nc.vector.tensor_tensor(out=t1[:, :], in0=sg_[:, :], in1=pq[:, :], op=ALU.mult), [B_sg, B_pq], [B_t1])
                xh = xacc[:, s, half * 512:(half + 1) * 512]
                fw.op(fw.pool, lambda: nc.gpsimd.tensor_tensor(out=xh, in0=xh, in1=t1[:, :], op=ALU.add), [B_t1], [B_xa[s]])
        exps = P.cfg.get("experts", list(range(ne)))
        for e in exps:
            for c0 in range(0, nch, G):
                gn = min(G, nch - c0)
                w2t, B_w2 = w2r.next()
                fw.dma(fw.sp, w2t[:, 0:gn, :], d[f"w2s{L}"][e, c0:c0 + gn].rearrange("c p n -> p c n"), writes=[B_w2])
                hT, B_hT = hTr.next()
                for ci in range(gn):
                    w13, B_w13 = w13r.next()
                    fw.dma(fw.sp, w13[:, :, :, :], d[f"w13s{L}"][e, c0 + ci].rearrange("p (i k j) -> p i k j", i=2, k=8), writes=[B_w13])
                    for th in range(TT // 512):
                        pa, B_pa = pmm.next()
                        for k in range(8):
                            mm(fw, pa[:, :], w13[:, 0, k, :], x1T[:, k, th * 512:(th + 1) * 512], k == 0, k == 7, [B_w13, B_x1T], [B_pa])
                        pb_, B_pb_ = pmm.next()
                        for k in range(8):
                            mm(fw, pb_[:, :], w13[:, 1, k, :], x1T[:, k, th * 512:(th + 1) * 512], k == 0, k == 7, [B_w13, B_x1T], [B_pb_])
                        sl, B_sl = silr.next()
                        fw.op(fw.act, lambda: nc.scalar.activation(sl[:, :], pa[:, :], AF.Silu), [B_pa], [B_sl])
                        fw.op(fw.dve, lambda: nc.vector.tensor_tensor(out=hT[:, ci, th * 512:(th + 1) * 512], in0=sl[:, :], in1=pb_[:, :], op=ALU.mult), [B_sl, B_pb_], [B_hT])
                is_last = (e == exps[-1] and c0 + G >= nch)
                if is_last and t + 1 < NT:
                    pre[(t + 1, 0)] = c1a(t + 1, 0)
                for s in range(NS):
                    for half in range(2):
                        pd, B_pd = pmm.next()
                        for ci in range(gn):
                            mm(fw, pd[:, :], hT[:, ci, s * 128:(s + 1) * 128], w2t[:, ci, half * 512:(half + 1) * 512], ci == 0, ci == gn - 1, [B_hT, B_w2], [B_pd])
                        xh = xacc[:, s, half * 512:(half + 1) * 512]
                        if moe:
                            fw.op(fw.dve, lambda: nc.vector.scalar_tensor_tensor(out=xh, in0=pd[:, :], scalar=gates[:, s, e:e + 1], in1=xh, op0=ALU.mult, op1=ALU.add), [B_pd, B_gates], [B_xa[s]])
                        else:
                            fw.op(fw.dve, lambda: nc.vector.tensor_tensor(out=xh, in0=xh, in1=pd[:, :], op=ALU.add), [B_pd], [B_xa[s]])
                    if is_last:
                        layer_norm(s, 2)
                        fw.dma(fw.sp, x_dst[tok0 + s * 128: tok0 + (s + 1) * 128, :], xacc[:, s, :], reads=[B_xa[s]])
                        if t + 1 < NT:
                            fw.dma(fw.sp, xacc[:, s, :], x_src[tok0 + TT + s * 128: tok0 + TT + (s + 1) * 128, :], writes=[B_xa[s]])
                            preloaded = True
    fw.close_scope()


def build_program(cfg=None):
    cfg = dict(cfg or {})
    P = Prog(cfg)
    declare_io(P)
    declare_scratch_C(P)
    out = P.dout("out", [S, D])
    load_consts(P)
    layers = cfg.get("layers", list(range(DEPTH)))
    overlap = cfg.get("overlap_w", True) and len(layers) > 1 and cfg.get("do_B", True) and cfg.get("do_mla", True)
    phase_W(P, layers[:1] if overlap else layers)
    for L in layers:
        x_src = P.dram["x"] if L == layers[0] else P.dram["x_mid"]
        x_dst = out if L == layers[-1] else P.dram["x_mid"]
        if cfg.get("do_A", True):
            phase_A(P, L, x_src)
        if cfg.get("do_B", True):
            phase_B(P, L, side_w=(layers[1:] if (overlap and L == layers[0]) else None))
        if cfg.get("do_C", True):
            phase_C(P, L, x_src, x_dst)
    P.fw.barrier()
    P.es.close()
    return P


def make_in_maps(inputs, cores):
    consts = host_consts()
    consts["c_natab"] = host_na_table(np.asarray(inputs["na_rpb"], dtype=np.float32))
    shared = {k: np.ascontiguousarray(np.asarray(inputs[k], dtype=np.float32)) for k in W_SHAPES}
    shared.update(consts)
    maps = []
    for b in cores:
        m = dict(shared)
        m["x"] = np.ascontiguousarray(np.asarray(inputs["x"][b], dtype=np.float32))
        m["p"] = np.ascontiguousarray(np.asarray(inputs["p"][:, b], dtype=np.float32))
        maps.append(m)
    return maps


def kernel(**inputs):
    P = build_program()
    maps = make_in_maps(inputs, list(range(NCORES)))
    res = run_bass_kernel_spmd(P.nc, maps, core_ids=list(range(NCORES)))
    return np.stack([np.asarray(r["out"], dtype=np.float32) for r in res.results], axis=0)
```
